# Optimizing a Trainium2 kernel written in Bass

```python
import math
import jax, jax.numpy as jnp
from jax import lax
import numpy as np

D_MODEL = 1024
BATCH = 8
SEQ = 2048
DEPTH = 4

MLA_HEADS = 8
MLA_NOPE = 64
MLA_ROPE = 32
MLA_V = 64
MLA_Q_RANK = 256
MLA_KV_RANK = 128
ROPE_BASE = 10000.0
Q_BLOCK = 128
POS_MAX_OFFSET = 4096

MLSTM_HEADS = 4
MLSTM_DH = 64
MLSTM_CHUNK = 64
M_INIT = -1e30

SSD_HEADS = 4
SSD_HEADDIM = 64
SSD_GROUPS = 2
SSD_HPG = SSD_HEADS // SSD_GROUPS
SSD_STATE = 128
SSD_CONV = 5
SSD_CHUNK = 128

MLA_OUT = MLA_HEADS * MLA_V
MLSTM_W = MLSTM_HEADS * MLSTM_DH
SSD_INNER = SSD_HEADS * SSD_HEADDIM
MIX_WIDTH = MLA_OUT + MLSTM_W + SSD_INNER
SSD_CONV_DIM = SSD_INNER + 2 * SSD_GROUPS * SSD_STATE
IN_SIZES = (MLA_Q_RANK, MLA_KV_RANK, MLA_ROPE,
            MLSTM_W, MLSTM_W, MLSTM_W, MLSTM_W, 4 * MLSTM_HEADS,
            SSD_INNER, SSD_CONV_DIM, 2 * SSD_HEADS)
IN_WIDTH = sum(IN_SIZES)

N_GROUPS = 4
EXPERTS_PER_GROUP = 4
N_EXPERTS = N_GROUPS * EXPERTS_PER_GROUP
TOP_K = 2
D_EXPERT = 256

DEEPNORM_ALPHA = (2 * DEPTH) ** 0.25
DEEPNORM_BETA = (8 * DEPTH) ** -0.25

kernel_name = "hymba_mla_mlstm_ssd_hmoe_deepnorm"

F32 = jnp.float32


def _split(t, sizes):
    out, off = [], 0
    for s in sizes:
        out.append(t[..., off:off + s])
        off += s
    return out


def _layernorm(x, g, b, eps=1e-5):
    xf = x.astype(F32)
    mu = jnp.mean(xf, -1, keepdims=True)
    var = jnp.mean(jnp.square(xf - mu), -1, keepdims=True)
    return (xf - mu) * lax.rsqrt(var + eps) * g + b


def _rmsnorm(x, g, eps=1e-6):
    xf = x.astype(F32)
    return xf * lax.rsqrt(jnp.mean(xf * xf, -1, keepdims=True) + eps) * g


def _rope_table(positions):
    inv = ROPE_BASE ** (-jnp.arange(0, MLA_ROPE, 2, dtype=F32) / MLA_ROPE)
    ang = positions.astype(F32)[..., None] * inv
    return jnp.cos(ang), jnp.sin(ang)


def _apply_rope(t, cos, sin):
    half = t.shape[-1] // 2
    t1, t2 = t[..., :half].astype(F32), t[..., half:].astype(F32)
    return jnp.concatenate([t1 * cos - t2 * sin, t2 * cos + t1 * sin], -1)


def _mla(c_q, c_kv, k_r, cos, sin, q_norm, kv_norm, w_uq, w_ukv):
    Bsz, S, _ = c_q.shape
    q = (_rmsnorm(c_q, q_norm).astype(c_q.dtype) @ w_uq).reshape(Bsz, S, MLA_HEADS, MLA_NOPE + MLA_ROPE)
    q_nope = q[..., :MLA_NOPE]
    q_rope = _apply_rope(q[..., MLA_NOPE:], cos[:, :, None], sin[:, :, None]).astype(q.dtype)
    kv = (_rmsnorm(c_kv, kv_norm).astype(c_kv.dtype) @ w_ukv).reshape(Bsz, S, MLA_HEADS, MLA_NOPE + MLA_V)
    k_nope, v = kv[..., :MLA_NOPE], kv[..., MLA_NOPE:]
    k_rope = _apply_rope(k_r, cos, sin).astype(k_r.dtype)
    scale = (MLA_NOPE + MLA_ROPE) ** -0.5
    nb = S // Q_BLOCK
    qn = q_nope.reshape(Bsz, nb, Q_BLOCK, MLA_HEADS, MLA_NOPE).transpose(1, 0, 2, 3, 4)
    qr = q_rope.reshape(Bsz, nb, Q_BLOCK, MLA_HEADS, MLA_ROPE).transpose(1, 0, 2, 3, 4)

    def block(args):
        qn_b, qr_b = args
        s = (jnp.einsum('bqhd,bkhd->bhqk', qn_b, k_nope)
             + jnp.einsum('bqhr,bkr->bhqk', qr_b, k_rope)).astype(F32) * scale
        p = jax.nn.softmax(s, axis=-1).astype(v.dtype)
        return jnp.einsum('bhqk,bkhd->bqhd', p, v)

    o = lax.map(block, (qn, qr))
    return o.transpose(1, 0, 2, 3, 4).reshape(Bsz, S, MLA_OUT)


def _mlstm_scan(q, k, v, ig, lf):
    Bsz, S, H, Dh = q.shape
    L = MLSTM_CHUNK
    nc = S // L

    def chunks(t):
        t = t.reshape((Bsz, nc, L, H) + t.shape[3:])
        return jnp.moveaxis(jnp.moveaxis(t, 1, 0), 3, 2)

    mask = jnp.tril(jnp.ones((L, L), bool))

    def step(carry, inp):
        C, n, m = carry
        qc, kc, vc, igc, lfc = inp
        b = jnp.cumsum(lfc, axis=-1)
        a = jnp.where(mask, b[..., :, None] - b[..., None, :] + igc[..., None, :], -jnp.inf)
        g = b + m[..., None]
        m_t = jnp.maximum(g, jnp.max(a, -1))
        w_intra = jnp.exp(a - m_t[..., None])
        w_inter = jnp.exp(g - m_t)
        qk = jnp.einsum('bhtd,bhsd->bhts', qc, kc).astype(F32) * w_intra
        num = (jnp.einsum('bhts,bhsd->bhtd', qk, vc)
               + w_inter[..., None] * jnp.einsum('bhtd,bhde->bhte', qc, C))
        den = jnp.sum(qk, -1) + w_inter * jnp.einsum('bhtd,bhd->bht', qc, n)
        h = num / jnp.maximum(jnp.abs(den), jnp.exp(-m_t))[..., None]
        bL = b[..., -1]
        dec = bL[..., None] - b + igc
        m_new = jnp.maximum(bL + m, jnp.max(dec, -1))
        w_s = jnp.exp(dec - m_new[..., None])
        w_c = jnp.exp(bL + m - m_new)
        C = w_c[..., None, None] * C + jnp.einsum('bhs,bhsd,bhse->bhde', w_s, kc, vc)
        n = w_c[..., None] * n + jnp.einsum('bhs,bhsd->bhd', w_s, kc)
        return (C, n, m_new), h

    init = (jnp.zeros((Bsz, H, Dh, Dh), F32), jnp.zeros((Bsz, H, Dh), F32),
            jnp.full((Bsz, H), M_INIT, F32))
    _, hs = lax.scan(step, init, (chunks(q), chunks(k), chunks(v), chunks(ig), chunks(lf)))
    hs = jnp.moveaxis(jnp.moveaxis(hs, 2, 3), 0, 1)
    return hs.reshape(Bsz, S, H, Dh)


def _mlstm(q, k, v, o, gates, gate_bias, norm_g):
    Bsz, S, _ = q.shape
    shp = (Bsz, S, MLSTM_HEADS, MLSTM_DH)
    q, k, v = q.reshape(shp), k.reshape(shp) * (MLSTM_DH ** -0.5), v.reshape(shp)
    gt = gates.reshape(Bsz, S, 4, MLSTM_HEADS).astype(F32) + gate_bias
    ig_f, lf_f = gt[:, :, 0], jax.nn.log_sigmoid(gt[:, :, 1])
    ig_b, lf_b = gt[:, :, 2], jax.nn.log_sigmoid(gt[:, :, 3])
    flip = lambda t: jnp.flip(t, axis=1)
    h = (_mlstm_scan(q, k, v, ig_f, lf_f)
         + flip(_mlstm_scan(flip(q), flip(k), flip(v), flip(ig_b), flip(lf_b))))
    mu = jnp.mean(h, -1, keepdims=True)
    var = jnp.mean(jnp.square(h - mu), -1, keepdims=True)
    hn = (h - mu) * lax.rsqrt(var + 1e-5) * norm_g.reshape(MLSTM_HEADS, MLSTM_DH)
    y = jax.nn.sigmoid(o.reshape(shp).astype(F32)) * hn
    return y.reshape(Bsz, S, MLSTM_W)


def _ssd_scan(x, dt, A, Bm, Cm):
    Bsz, S, G, E, P = x.shape
    N = Bm.shape[-1]
    L = SSD_CHUNK
    nc = S // L

    def chunks(t):
        return jnp.moveaxis(t.reshape((Bsz, nc, L) + t.shape[2:]), 1, 0)

    mask = jnp.tril(jnp.ones((L, L), bool))[None, :, :, None, None]

    def step(state, inp):
        xc, dtc, Bc, Cc = inp
        cs = jnp.cumsum(dtc * A, axis=1)
        Lm = jnp.exp(jnp.where(mask, cs[:, :, None] - cs[:, None, :], -jnp.inf))
        CB = jnp.einsum('btgn,bsgn->btsg', Cc, Bc)
        xdt = xc * dtc[..., None]
        y = jnp.einsum('btsg,btsge,bsgep->btgep', CB, Lm, xdt)
        y = y + jnp.einsum('btgn,bgepn->btgep', Cc, state) * jnp.exp(cs)[..., None]
        decay_end = jnp.exp(cs[:, -1:] - cs)
        state = (jnp.exp(cs[:, -1])[..., None, None] * state
                 + jnp.einsum('bsgn,bsge,bsgep->bgepn', Bc, decay_end, xdt))
        return state, y

    init = jnp.zeros((Bsz, G, E, P, N), F32)
    _, ys = lax.scan(step, init, (chunks(x), chunks(dt), chunks(Bm), chunks(Cm)))
    return jnp.moveaxis(ys, 0, 1).reshape(Bsz, S, G, E, P)


def _ssd(z, xbc, dt_raw, conv_w, conv_b, dt_bias, a_log, d_skip, norm_g):
    Bsz, S, Cdim = xbc.shape
    pad = SSD_CONV // 2
    xbc = lax.conv_general_dilated(xbc, conv_w[:, None, :], window_strides=(1,),
                                   padding=((pad, pad),), dimension_numbers=('NWC', 'WIO', 'NWC'),
                                   feature_group_count=Cdim) + conv_b
    xbc = jax.nn.silu(xbc)
    xs, Bm, Cm = _split(xbc, (SSD_INNER, SSD_GROUPS * SSD_STATE, SSD_GROUPS * SSD_STATE))
    x = xs.reshape(Bsz, S, SSD_GROUPS, SSD_HPG, SSD_HEADDIM)
    Bm = Bm.reshape(Bsz, S, SSD_GROUPS, SSD_STATE)
    Cm = Cm.reshape(Bsz, S, SSD_GROUPS, SSD_STATE)
    dt = jax.nn.softplus(dt_raw.reshape(Bsz, S, 2, SSD_GROUPS, SSD_HPG).astype(F32)
                         + dt_bias.reshape(2, SSD_GROUPS, SSD_HPG))
    A = -jnp.exp(a_log.astype(F32)).reshape(2, SSD_GROUPS, SSD_HPG)
    flip = lambda t: jnp.flip(t, axis=1)
    y = (_ssd_scan(x, dt[:, :, 0], A[0], Bm, Cm)
         + flip(_ssd_scan(flip(x), flip(dt[:, :, 1]), A[1], flip(Bm), flip(Cm)))
         + x * d_skip.reshape(SSD_GROUPS, SSD_HPG)[..., None])
    y = y.reshape(Bsz, S, SSD_INNER) * jax.nn.silu(z.astype(F32))
    y = _rmsnorm(y.reshape(Bsz, S, SSD_GROUPS, SSD_INNER // SSD_GROUPS),
                 norm_g.reshape(SSD_GROUPS, SSD_INNER // SSD_GROUPS))
    return y.reshape(Bsz, S, SSD_INNER)


def _hier_moe(x, wg, bg, we, be, w_gate, w_up, w_down):
    glog = (jnp.einsum('bsd,dg->bsg', x, wg) + bg).astype(F32)
    gp, gi = lax.top_k(jax.nn.softmax(glog, -1), 1)
    elog = (jnp.einsum('bsd,gde->bsge', x, we) + be).astype(F32)
    elog = jnp.einsum('bsge,bsg->bse', elog, jax.nn.one_hot(gi[..., 0], N_GROUPS, dtype=F32))
    ep, ei = lax.top_k(jax.nn.softmax(elog, -1), TOP_K)
    ep = ep / jnp.sum(ep, -1, keepdims=True)
    eid = gi * EXPERTS_PER_GROUP + ei
    combine = jnp.sum(jax.nn.one_hot(eid, N_EXPERTS, dtype=F32) * (gp * ep)[..., None], axis=2)
    h = jax.nn.silu(jnp.einsum('bsd,edf->bsef', x, w_gate)) * jnp.einsum('bsd,edf->bsef', x, w_up)
    h = h * combine[..., None].astype(h.dtype)
    return jnp.einsum('bsef,efd->bsd', h, w_down)


def setup_inputs(seed: int = 0) -> dict:
    key = jax.random.key(seed)
    ks = jax.random.split(key, 32)
    nrm = lambda k, shape, s: jax.random.normal(k, shape, F32) * s
    gain = lambda k, shape: 1.0 + 0.02 * jax.random.normal(k, shape, F32)

    x = jax.random.normal(ks[0], (BATCH, SEQ, D_MODEL), F32)
    positions = (jax.random.randint(ks[1], (BATCH, 1), 0, POS_MAX_OFFSET, dtype=jnp.int32)
                 + jnp.arange(SEQ, dtype=jnp.int32)[None, :])

    w_in = nrm(ks[2], (DEPTH, D_MODEL, IN_WIDTH), D_MODEL ** -0.5)
    mla_q_norm = gain(ks[3], (DEPTH, MLA_Q_RANK))
    mla_kv_norm = gain(ks[4], (DEPTH, MLA_KV_RANK))
    mla_w_uq = nrm(ks[5], (DEPTH, MLA_Q_RANK, MLA_HEADS * (MLA_NOPE + MLA_ROPE)), MLA_Q_RANK ** -0.5)
    mla_w_ukv = nrm(ks[6], (DEPTH, MLA_KV_RANK, MLA_HEADS * (MLA_NOPE + MLA_V)), MLA_KV_RANK ** -0.5)

    lin = jnp.linspace(3.0, 6.0, MLSTM_HEADS, dtype=F32)
    zer = jnp.zeros((MLSTM_HEADS,), F32)
    mlstm_gate_bias = jnp.stack([zer, lin, zer, lin])[None] + nrm(ks[7], (DEPTH, 4, MLSTM_HEADS), 0.1)
    mlstm_norm = gain(ks[8], (DEPTH, MLSTM_W))

    ssd_conv_w = nrm(ks[9], (DEPTH, SSD_CONV, SSD_CONV_DIM), SSD_CONV ** -0.5)
    ssd_conv_b = nrm(ks[10], (DEPTH, SSD_CONV_DIM), 0.01)
    dt0 = jnp.exp(jax.random.uniform(ks[11], (DEPTH, 2, SSD_HEADS), F32, math.log(1e-3), math.log(1e-1)))
    ssd_dt_bias = dt0 + jnp.log(-jnp.expm1(-dt0))
    ssd_a_log = jnp.log(jax.random.uniform(ks[12], (DEPTH, 2, SSD_HEADS), F32, 1.0, 16.0))
    ssd_d = gain(ks[13], (DEPTH, SSD_HEADS))
    ssd_norm = gain(ks[14], (DEPTH, SSD_INNER))

    w_out = nrm(ks[15], (DEPTH, MIX_WIDTH, D_MODEL), MIX_WIDTH ** -0.5 * DEEPNORM_BETA)
    ln1_g = gain(ks[16], (DEPTH, D_MODEL))
    ln1_b = nrm(ks[17], (DEPTH, D_MODEL), 0.02)

    router_group_w = nrm(ks[18], (DEPTH, D_MODEL, N_GROUPS), D_MODEL ** -0.5)
    router_group_b = nrm(ks[19], (DEPTH, N_GROUPS), 0.01)
    router_expert_w = nrm(ks[20], (DEPTH, N_GROUPS, D_MODEL, EXPERTS_PER_GROUP), D_MODEL ** -0.5)
    router_expert_b = nrm(ks[21], (DEPTH, N_GROUPS, EXPERTS_PER_GROUP), 0.01)
    expert_w_gate = nrm(ks[22], (DEPTH, N_EXPERTS, D_MODEL, D_EXPERT), D_MODEL ** -0.5)
    expert_w_up = nrm(ks[23], (DEPTH, N_EXPERTS, D_MODEL, D_EXPERT), D_MODEL ** -0.5)
    expert_w_down = nrm(ks[24], (DEPTH, N_EXPERTS, D_EXPERT, D_MODEL), D_EXPERT ** -0.5 * DEEPNORM_BETA)
    ln2_g = gain(ks[25], (DEPTH, D_MODEL))
    ln2_b = nrm(ks[26], (DEPTH, D_MODEL), 0.02)

    return {"x": x, "positions": positions, "w_in": w_in,
            "mla_q_norm": mla_q_norm, "mla_kv_norm": mla_kv_norm,
            "mla_w_uq": mla_w_uq, "mla_w_ukv": mla_w_ukv,
            "mlstm_gate_bias": mlstm_gate_bias, "mlstm_norm": mlstm_norm,
            "ssd_conv_w": ssd_conv_w, "ssd_conv_b": ssd_conv_b, "ssd_dt_bias": ssd_dt_bias,
            "ssd_a_log": ssd_a_log, "ssd_d": ssd_d, "ssd_norm": ssd_norm,
            "w_out": w_out, "ln1_g": ln1_g, "ln1_b": ln1_b,
            "router_group_w": router_group_w, "router_group_b": router_group_b,
            "router_expert_w": router_expert_w, "router_expert_b": router_expert_b,
            "expert_w_gate": expert_w_gate, "expert_w_up": expert_w_up, "expert_w_down": expert_w_down,
            "ln2_g": ln2_g, "ln2_b": ln2_b}


def reference(x, positions, w_in, mla_q_norm, mla_kv_norm, mla_w_uq, mla_w_ukv,
              mlstm_gate_bias, mlstm_norm, ssd_conv_w, ssd_conv_b, ssd_dt_bias,
              ssd_a_log, ssd_d, ssd_norm, w_out, ln1_g, ln1_b,
              router_group_w, router_group_b, router_expert_w, router_expert_b,
              expert_w_gate, expert_w_up, expert_w_down, ln2_g, ln2_b):
    dtype = x.dtype
    cos, sin = _rope_table(positions)
    for l in range(DEPTH):
        p = jnp.einsum('bsd,dc->bsc', x, w_in[l])
        (c_q, c_kv, k_r, m_q, m_k, m_v, m_o, m_g, s_z, s_xbc, s_dt) = _split(p, IN_SIZES)
        y_a = _mla(c_q, c_kv, k_r, cos, sin, mla_q_norm[l], mla_kv_norm[l], mla_w_uq[l], mla_w_ukv[l])
        y_b = _mlstm(m_q, m_k, m_v, m_o, m_g, mlstm_gate_bias[l], mlstm_norm[l])
        y_c = _ssd(s_z, s_xbc, s_dt, ssd_conv_w[l], ssd_conv_b[l], ssd_dt_bias[l],
                   ssd_a_log[l], ssd_d[l], ssd_norm[l])
        heads = jnp.concatenate([y_a.astype(dtype), y_b.astype(dtype), y_c.astype(dtype)], -1)
        mix = jnp.einsum('bsc,cd->bsd', heads, w_out[l])
        x = _layernorm(DEEPNORM_ALPHA * x + mix, ln1_g[l], ln1_b[l]).astype(dtype)
        ffn = _hier_moe(x, router_group_w[l], router_group_b[l], router_expert_w[l], router_expert_b[l],
                        expert_w_gate[l], expert_w_up[l], expert_w_down[l]).astype(dtype)
        x = _layernorm(DEEPNORM_ALPHA * x + ffn, ln2_g[l], ln2_b[l]).astype(dtype)
    return x
```

```python
import math
import numpy as np
import concourse.bass as bass
import concourse.mybir as mybir
from concourse.bass_utils import run_bass_kernel_spmd

F32 = mybir.dt.float32
BF16 = mybir.dt.bfloat16
I32 = mybir.dt.int32
AF = mybir.ActivationFunctionType
ALU = mybir.AluOpType
AX = mybir.AxisListType

S = 2048
D = 1024
NT = 16
DEPTH = 4
ALPHA = (2 * DEPTH) ** 0.25
IN_W = 2488
PI = math.pi
NEG = -30000.0

P_GQ, P_GKV, P_GB, P_MN, P_CW, P_CB, P_DTB, P_AL, P_SD, P_SN, P_RB, P_RW = (
    0, 2, 3, 19, 275, 305, 311, 319, 327, 331, 587, 607)
PW = 767
C_ID, C_U, C_L, C_MF, C_MB, C_ONE, C_IF = 0, 128, 256, 384, 512, 640, 768
CW = 784

WSLOT = 6144
NSLOT = 2
ARENA_BYTES = 74 * 1024


class Sem:
    def __init__(self, nc, name, dma=False):
        self.h = nc.alloc_semaphore(name)
        self.val = 0
        self.dma = dma
        self.name = name


class Buf:
    __slots__ = ("name", "w", "r", "excl")

    def __init__(self, name, excl=False):
        self.name = name
        self.w = None
        self.r = {}
        self.excl = excl


class K:
    def __init__(self, nc):
        self.nc = nc
        self.eng = {}
        for n, a in (("pe", "tensor"), ("dve", "vector"), ("act", "scalar"),
                     ("pool", "gpsimd"), ("sp", "sync")):
            self.eng[n] = (getattr(nc, a), Sem(nc, "e_" + n))
        self.known = {n: {} for n in self.eng}
        self.dsems = []
        self.ninst = 0

    def dsem(self, name):
        s = Sem(self.nc, name, dma=True)
        self.dsems.append(s)
        return s

    def _wait(self, e, tickets):
        h, own = self.eng[e]
        kn = self.known[e]
        need = {}
        for (s, v) in tickets:
            if s.dma:
                v = s.val
            if v > kn.get(s, 0) and v > need.get(s, 0):
                need[s] = v
        for s, v in need.items():
            assert s.val >= v, "wait on unsignalled ticket %s %d>%d (eng %s)" % (s.name, v, s.val, e)
            h.wait_ge(s.h, v)
            kn[s] = v
            self.ninst += 1

    def _tickets(self, reads, writes):
        tk = []
        for b in reads:
            if b.w is not None:
                tk.append(b.w)
        for b in writes:
            if b.w is not None:
                tk.append(b.w)
            for s, v in b.r.items():
                tk.append((s, v))
        return tk

    def _commit(self, t, reads, writes):
        for b in writes:
            b.w = t
            b.r = {}
        for b in reads:
            if b not in writes:
                if t[1] > b.r.get(t[0], 0):
                    b.r[t[0]] = t[1]

    def op(self, e, fn, reads=(), writes=(), signal=True, keep_self=False):
        h, sem = self.eng[e]
        ex = [b for b in reads if b.excl and b not in writes]
        if ex:
            writes = list(writes) + ex
            reads = [b for b in reads if not b.excl]
        tk = self._tickets(reads, writes)
        if e == "pe" and not keep_self:
            tk = [(s, v) for (s, v) in tk if s is not sem]
        self._wait(e, tk)
        inst = fn(h)
        self.ninst += 1
        if signal:
            sem.val += 1
            inst.then_inc(sem.h, 1)
            t = (sem, sem.val)
        else:
            t = (sem, sem.val + 1)
        self._commit(t, reads, writes)
        return inst

    def dma(self, q, out, in_, dsem, reads=(), writes=(), **kw):
        h, _ = self.eng[q]
        self._wait(q, self._tickets(reads, writes))
        inst = h.dma_start(out=out, in_=in_, **kw)
        self.ninst += 1
        dsem.val += 16
        inst.then_inc(dsem.h, 16)
        self._commit((dsem, dsem.val), reads, writes)

    def barrier(self):
        allt = [(s, s.val) for (_, s) in self.eng.values() if s.val > 0]
        allt += [(s, s.val) for s in self.dsems if s.val > 0]
        for e in self.eng:
            self._wait(e, allt)


class Rot:
    def __init__(self, items):
        self.items = list(items)
        self.i = 0

    def __call__(self):
        x = self.items[self.i % len(self.items)]
        self.i += 1
        return x


class Arena:
    def __init__(self, nc, nbytes):
        self.t = nc.alloc_sbuf_tensor("arena", [128, nbytes // 2], BF16)
        self.nbytes = nbytes
        self.off = 0
        self.peak = 0

    def reset(self):
        self.off = 0

    def alloc(self, shape, dtype, name="a"):
        esz = 4 if dtype in (F32, I32) else 2
        n = 1
        for s in shape:
            n *= s
        nb = (n * esz + 63) // 64 * 64
        assert self.off + nb <= self.nbytes, "arena overflow %s need %d have %d" % (
            name, nb, self.nbytes - self.off)
        ap = self.t[:, self.off // 2:(self.off + n * esz) // 2]
        if esz == 4:
            ap = ap.bitcast(dtype)
        if len(shape) == 2:
            ap = ap.rearrange("p (a b) -> p a b", a=shape[0])
        elif len(shape) == 3:
            ap = ap.rearrange("p (a b c) -> p a b c", a=shape[0], b=shape[1])
        elif len(shape) == 4:
            ap = ap.rearrange("p (a b c d) -> p a b c d", a=shape[0], b=shape[1], c=shape[2])
        self.off += nb
        self.peak = max(self.peak, self.off)
        return ap


def bc(ap, shape):
    return ap.to_broadcast(list(shape))


def build(NL, taps=(), stop_after=None):
    nc = bass.Bass("TRN2", target_bir_lowering=False)
    k = K(nc)

    def din(name, shape, dt=F32):
        return nc.dram_tensor(name, list(shape), dt, kind="ExternalInput").ap()

    dx = din("x", [S, D])
    dpos = din("pos", [128, NT], I32)
    dcst = din("cst", [128, CW])
    dprm = din("prm", [NL, 128, PW])
    w_in = din("w_in", [NL, D, IN_W])
    w_uq = din("w_uq", [NL, 256, 768])
    w_ukv = din("w_ukv", [NL, 128, 1024])
    w_out = din("w_out", [NL, D, D])
    dlnp = din("lnp", [NL, 4, D])
    d_wg = din("wg", [NL, 16, D, 256])
    d_wu = din("wu", [NL, 16, D, 256])
    d_wd = din("wd", [NL, 16, 256, D])
    dy = nc.dram_tensor("y", [S, D], F32, kind="ExternalOutput").ap()
    tap_out = {}

    X = nc.alloc_sbuf_tensor("X", [128, NT, D], F32)
    Xb = [Buf("X%d" % i) for i in range(NT)]
    XT = nc.alloc_sbuf_tensor("XT", [128, 8, S], BF16)
    XTb = [Buf("XT%d" % g) for g in range(4)]
    WS = [nc.alloc_sbuf_tensor("ws%d" % i, [128, WSLOT], BF16) for i in range(NSLOT)]
    WSb = [Buf("ws%d" % i) for i in range(NSLOT)]
    WSs = [k.dsem("ws%d" % i) for i in range(NSLOT)]
    CF = nc.alloc_sbuf_tensor("cf", [128, CW], F32)
    CFb = Buf("cf")
    CB = nc.alloc_sbuf_tensor("cb", [128, CW], BF16)
    CBb = Buf("cb")
    PRM = nc.alloc_sbuf_tensor("prm_s", [128, PW], F32)
    PRMb = Buf("prm")
    SCT = nc.alloc_sbuf_tensor("sc", [128, 1024], F32)
    sc_off = [0]

    def sc_reset():
        sc_off[0] = 0

    def sc_alloc(n):
        o = sc_off[0]
        assert o + n <= 1024, "scalar pool overflow"
        sc_off[0] = o + n
        return SCT[:, o:o + n]
    COS = nc.alloc_sbuf_tensor("cos", [128, NT, 16], F32)
    SIN = nc.alloc_sbuf_tensor("sin", [128, NT, 16], F32)
    CSb = Buf("cossin")
    arena = Arena(nc, ARENA_BYTES)
    banks = [nc.alloc_psum_tensor("pb%d" % i, [128, 512], F32) for i in range(8)]
    bankb = [Buf("pb%d" % i, excl=True) for i in range(8)]

    def pool(idx):
        return Rot([(banks[i], bankb[i]) for i in idx])

    s_c = k.dsem("cst")
    s_x = k.dsem("xld")
    s_p = k.dsem("prm")
    s_l = k.dsem("lnp")
    s_y = k.dsem("yst")
    s_t = k.dsem("tap")

    wd_sems = [k.dsem("wd%d" % i) for i in range(8)]
    identF = CF[:, C_ID:C_ID + 128]
    identB = CB[:, C_ID:C_ID + 128]
    slot_ctr = [0]

    def wslot():
        i = slot_ctr[0] % NSLOT
        slot_ctr[0] += 1
        return WS[i], WSb[i], WSs[i]

    def tap(name, ap, reads):
        if name not in taps:
            return
        shp = list(ap.shape)
        n = 1
        for s_ in shp[1:]:
            n *= s_
        cnt = sum(1 for t_ in tap_out if t_.startswith(name))
        nm = name if cnt == 0 else "%s_%d" % (name, cnt)
        dt_ = nc.dram_tensor("tap_" + nm, shp, ap.dtype, kind="ExternalOutput").ap()
        tap_out[nm] = shp
        k.dma("sp", dt_, ap, s_t, reads=reads)

    def mm(out, lhsT, rhs, start, stop, reads, writes, signal, serial=False):
        k.op("pe", lambda h: h.matmul(out, lhsT, rhs, start=start, stop=stop),
             reads=reads, writes=writes, signal=(signal or serial), keep_self=serial)

    def act(out, in_, func, reads, writes, **kw):
        k.op("act", lambda h: h.activation(out=out, in_=in_, func=func, **kw), reads=reads, writes=writes)

    def tt(out, in0, in1, op, reads, writes, e="dve"):
        k.op(e, lambda h: h.tensor_tensor(out=out, in0=in0, in1=in1, op=op), reads=reads, writes=writes)

    def ts(out, in0, s1, s2, op0, op1, reads, writes, e="dve"):
        if s2 is None:
            k.op(e, lambda h: h.tensor_scalar(out=out, in0=in0, scalar1=s1, scalar2=None, op0=op0),
                 reads=reads, writes=writes)
        else:
            k.op(e, lambda h: h.tensor_scalar(out=out, in0=in0, scalar1=s1, scalar2=s2, op0=op0, op1=op1),
                 reads=reads, writes=writes)

    def stt(out, in0, scalar, in1, op0, op1, reads, writes):
        k.op("dve", lambda h: h.scalar_tensor_tensor(out=out, in0=in0, scalar=scalar, in1=in1, op0=op0, op1=op1),
             reads=reads, writes=writes)

    def cp(out, in_, reads, writes, e="dve"):
        k.op(e, lambda h: h.tensor_copy(out=out, in_=in_), reads=reads, writes=writes)

    def red(out, in_, op, reads, writes):
        k.op("dve", lambda h: h.tensor_reduce(out=out, in_=in_, axis=AX.X, op=op), reads=reads, writes=writes)

    def recip(out, in_, reads, writes):
        k.op("dve", lambda h: h.reciprocal(out=out, in_=in_), reads=reads, writes=writes)

    def memset(ap, val, writes, e="dve"):
        k.op(e, lambda h: h.memset(ap, val), writes=writes)

    def tr(out, in_, ident, reads, writes, signal):
        k.op("pe", lambda h: h.transpose(out, in_, ident), reads=reads, writes=writes, signal=signal)

    k.dma("sp", CF[:, :], dcst[:, :], s_c, writes=[CFb])
    s_cb = k.dsem("cstb")
    k.dma("pool", CB[:, :], dcst[:, :], s_cb, writes=[CBb])
    dxv = dx.rearrange("(i p) d -> p i d", p=128)
    for q in range(4):
        k.dma("sp", X[:, 4 * q:4 * q + 4, :], dxv[:, 4 * q:4 * q + 4, :], s_x, writes=Xb[4 * q:4 * q + 4])

    arena.reset()
    tb = Buf("setup_tmp")
    posi = arena.alloc([NT, 1], I32, "posi")
    posf = arena.alloc([NT, 1], F32, "posf")
    ang = arena.alloc([NT, 16], F32, "ang")
    kf = arena.alloc([NT, 16], F32, "kf")
    ki = arena.alloc([NT, 16], I32, "ki")
    mt_ = arena.alloc([NT, 16], F32, "mt")
    k.dma("sp", posi[:, :, 0], dpos[:, :], s_c, writes=[tb])
    cp(posf, posi, [tb], [tb])
    invf = CF[:, C_IF:C_IF + 16].rearrange("p (a b) -> p a b", a=1)
    tt(ang, bc(posf, [128, NT, 16]), bc(invf, [128, NT, 16]), ALU.mult, [tb, CFb], [tb])
    ts(kf, ang, 1.0 / (2 * PI), None, ALU.mult, None, [tb], [tb])
    cp(ki, kf, [tb], [tb])
    cp(kf, ki, [tb], [tb])
    stt(ang, kf, -2 * PI, ang, ALU.mult, ALU.add, [tb], [tb])

    def wrap(r):
        ts(mt_, r, PI, -2 * PI, ALU.is_gt, ALU.mult, [tb], [tb])
        tt(r, r, mt_, ALU.add, [tb], [tb])
        ts(mt_, r, -PI, 2 * PI, ALU.is_lt, ALU.mult, [tb], [tb])
        tt(r, r, mt_, ALU.add, [tb], [tb])

    wrap(ang)
    act(SIN[:, :, :], ang, AF.Sin, [tb], [CSb])
    ts(ang, ang, PI / 2, None, ALU.add, None, [tb, CSb], [tb])
    wrap(ang)
    act(COS[:, :, :], ang, AF.Sin, [tb], [CSb])

    PT4 = pool([6, 7])

    def transpose_x_tile(i, src_tile_ap, src_reads, dst32=None, dst32b=None):
        for c0 in (0, 4):
            bk, bb = PT4()
            for j in range(4):
                c = c0 + j
                tr(bk[:, j * 128:(j + 1) * 128], src_tile_ap[:, c * 128:(c + 1) * 128], identF,
                   src_reads + [CFb], [bb], j == 3)
            bv = bk[:, :].rearrange("p (a b) -> p a b", a=4)
            act(XT[:, c0:c0 + 4, i * 128:(i + 1) * 128], bv, AF.Copy, [bb], [XTb[i // 4]])
            if dst32 is not None:
                cp(dst32[:, c0:c0 + 4, :], bv, [bb], [dst32b])

    for i in range(NT):
        transpose_x_tile(i, X[:, i, :], [Xb[i]])
    k.barrier()

    st = dict(nc=nc, k=k, arena=arena, X=X, Xb=Xb, XT=XT, XTb=XTb, CF=CF, CFb=CFb, CB=CB, CBb=CBb,
              PRM=PRM, PRMb=PRMb, sc_alloc=sc_alloc, sc_reset=sc_reset, COS=COS, SIN=SIN, CSb=CSb, pool=pool,
              wslot=wslot, tap=tap, mm=mm, act=act, tt=tt, ts=ts, stt=stt, cp=cp, red=red, recip=recip,
              memset=memset, tr=tr, identF=identF, identB=identB, w_in=w_in, w_uq=w_uq, w_ukv=w_ukv,
              w_out=w_out, dlnp=dlnp, d_wg=d_wg, d_wu=d_wu, d_wd=d_wd, dprm=dprm, s_p=s_p, s_l=s_l,
              transpose_x_tile=transpose_x_tile, stop_after=stop_after, wd_sems=wd_sems, pre={})

    st = NS(st)
    for l in range(NL):
        k.dma("sp", PRM[:, :], dprm[l], s_p, writes=[PRMb])
        if stop_after == "setup":
            break
        phase_mla(st, l)
        if stop_after is not None and stop_after.startswith("mla"):
            break
        phase_mlstm(st, l)
        if stop_after is not None and stop_after.startswith("mlstm"):
            break
        phase_ssd(st, l)
        if stop_after is not None and stop_after.startswith("ssd"):
            break
        phase_ln_router_moe(st, l, last=(l == NL - 1))

    k.barrier()
    dyv = dy.rearrange("(i p) d -> p i d", p=128)
    for q in range(4):
        k.dma("sp", dyv[:, 4 * q:4 * q + 4, :], X[:, 4 * q:4 * q + 4, :], s_y, reads=Xb[4 * q:4 * q + 4])
    k._wait("sp", [(s_y, s_y.val), (s_t, s_t.val)] if s_t.val else [(s_y, s_y.val)])
    return nc, tap_out, k


class NS:
    def __init__(self, d):
        self.__dict__.update(d)


def rope_tm(st, src, dst, tmpa, tmpb, sb, db):
    s = st
    t1, t2 = src[:, :, 0:16], src[:, :, 16:32]
    C, Sn = s.COS[:, :, :], s.SIN[:, :, :]
    tb_ = Buf("rope_tmp")
    s.tt(tmpa, t1, C, ALU.mult, [sb, s.CSb], [tb_])
    s.tt(tmpb, t2, Sn, ALU.mult, [sb, s.CSb, tb_], [tb_])
    s.tt(dst[:, :, 0:16], tmpa, tmpb, ALU.subtract, [tb_], [db])
    s.tt(tmpa, t2, C, ALU.mult, [sb, s.CSb, db], [tb_])
    s.tt(tmpb, t1, Sn, ALU.mult, [sb, s.CSb, tb_], [tb_])
    s.tt(dst[:, :, 16:32], tmpa, tmpb, ALU.add, [tb_], [db])


def outproj_load(st, l, nchunk, row0):
    s = st
    W, Wb, Wsm = s.wslot()
    Wv = W[:, 0:nchunk * 1024].rearrange("p (c n) -> p c n", c=nchunk)
    src = s.w_out[l, row0:row0 + nchunk * 128, :].rearrange("(c p) n -> p c n", p=128)
    s.k.dma("pool", Wv, src, Wsm, writes=[Wb])
    return Wv, Wb


def outproj_partial(st, l, YT, YTb, nchunk, row0, first, pre=None):
    s = st
    Wv, Wb = pre if pre is not None else outproj_load(s, l, nchunk, row0)
    PA = s.pool([0, 1, 2, 3])
    for i in range(NT):
        for hf in range(2):
            bk, bb = PA()
            for c in range(nchunk):
                s.mm(bk[:, :], YT[:, c, i * 128:(i + 1) * 128], Wv[:, c, hf * 512:(hf + 1) * 512],
                     c == 0, c == nchunk - 1, [YTb, Wb], [bb], c == nchunk - 1)
            xs = s.X[:, i, hf * 512:(hf + 1) * 512]
            if first:
                s.stt(xs, xs, ALPHA, bk[:, :], ALU.mult, ALU.add, [s.Xb[i], bb], [s.Xb[i]])
            else:
                s.tt(xs, xs, bk[:, :], ALU.add, [s.Xb[i], bb], [s.Xb[i]])


def preload_mla(s, l):
    k = s.k
    W0, W0b, W0s = s.wslot()
    W0v = W0[:, 0:8 * 416].rearrange("p (k c) -> p k c", k=8)
    k.dma("pool", W0v, s.w_in[l].rearrange("(k p) c -> p k c", p=128)[:, :, 0:416], W0s, writes=[W0b])
    W1, W1b, W1s = s.wslot()
    Wuq = W1[:, 0:1536].rearrange("p (c n) -> p c n", c=2)
    k.dma("pool", Wuq, s.w_uq[l].rearrange("(c p) n -> p c n", p=128), W1s, writes=[W1b])
    Wkv = W1[:, 1536:2560]
    k.dma("pool", Wkv, s.w_ukv[l], W1s, writes=[W1b])
    s.pre["mla"] = (W0v, W0b, Wuq, Wkv, W1b)


def preload_mlstm_a(s, l):
    Wa, Wab, Was = s.wslot()
    Wav = Wa[:, 0:8 * 512].rearrange("p (k c) -> p k c", k=8)
    wsrc = s.w_in[l].rearrange("(k p) c -> p k c", p=128)
    s.k.dma("pool", Wav, wsrc[:, :, 416:928], Was, writes=[Wab])
    s.pre["mlstm_a"] = (Wav, Wab)


def preload_ssd_x(s, l):
    Wx, Wxb, Wxs = s.wslot()
    Wxv = Wx[:, 0:6144].rearrange("p (k c) -> p k c", k=8)
    wsrc = s.w_in[l].rearrange("(k p) c -> p k c", p=128)
    s.k.dma("pool", Wxv, wsrc[:, :, 1712:2480], Wxs, writes=[Wxb])
    s.pre["ssd_x"] = (Wxv, Wxb)


def phase_mla(st, l):
    s = NS(st) if isinstance(st, dict) else st
    k, ar = s.k, s.arena
    ar.reset()
    PA = s.pool([0, 1, 2, 3])
    PB = s.pool([4, 5])
    PC = s.pool([6, 7])
    if "mla" not in s.pre:
        preload_mla(s, l)
    W0v, W0b, Wuq, Wkv, W1b = s.pre.pop("mla")
    for c in range(2):
        s.ts(Wuq[:, c, :], Wuq[:, c, :], s.PRM[:, P_GQ + c:P_GQ + c + 1], None, ALU.mult, None,
             [s.PRMb, W1b], [W1b])
    s.ts(Wkv, Wkv, s.PRM[:, P_GKV:P_GKV + 1], None, ALU.mult, None, [s.PRMb, W1b], [W1b])

    if s.stop_after == "mla_w":
        return
    cT = ar.alloc([3, S], BF16, "cT"); cTb = Buf("cT")
    YA = ar.alloc([4, S], BF16, "YA"); YAb = Buf("YA")
    krr = ar.alloc([NT, 32], F32, "krr"); krrb = Buf("krr")
    krb = ar.alloc([NT, 32], BF16, "krb"); krbb = Buf("krb")
    sq = ar.alloc([384], F32, "sq"); sqb = Buf("sq")
    s.sc_reset()
    ssq = s.sc_alloc(NT); sskv = s.sc_alloc(NT); ssb = Buf("ss")
    rq = s.sc_alloc(NT); rkv = s.sc_alloc(NT); rb_ = Buf("r")
    ta = ar.alloc([NT, 16], F32, "ta"); tb2 = ar.alloc([NT, 16], F32, "tb")
    qhb = ar.alloc([NT, 96], BF16, "qhb"); qhbb = Buf("qhb")
    qr = ar.alloc([NT, 32], F32, "qr"); qrb = Buf("qr")
    khb = ar.alloc([NT, 96], BF16, "khb"); khbb = Buf("khb")
    VA = [ar.alloc([NT, 65], BF16, "VA%d" % i) for i in range(2)]; VAb = [Buf("VA0"), Buf("VA1")]
    QT = [ar.alloc([S], BF16, "QT%d" % i) for i in range(2)]; QTb = [Buf("QT0"), Buf("QT1")]
    KT = [ar.alloc([S], BF16, "KT%d" % i) for i in range(2)]; KTb = [Buf("KT0"), Buf("KT1")]
    PTs = [(ar.alloc([512], BF16, "PT%d" % i), Buf("PT%d" % i)) for i in range(5)]
    PTr = Rot(PTs)
    OSs = [(ar.alloc([512], F32, "OS%d" % i), Buf("OS%d" % i)) for i in range(2)]
    OSr = Rot(OSs)

    for i in range(NT):
        bk, bb = PA()
        for kk in range(8):
            s.mm(bk[:, 0:416], s.XT[:, kk, i * 128:(i + 1) * 128], W0v[:, kk, :], kk == 0, kk == 7,
                 [s.XTb[i // 4], W0b], [bb], kk == 7)
        s.act(sq[:, 0:384], bk[:, 0:384], AF.Square, [bb], [sqb])
        s.red(ssq[:, i:i + 1], sq[:, 0:256], ALU.add, [sqb], [ssb])
        s.red(sskv[:, i:i + 1], sq[:, 256:384], ALU.add, [sqb], [ssb])
        s.cp(krr[:, i, :], bk[:, 384:416], [bb], [krrb])
    if s.stop_after == "mla_tm":
        return
    for j in range(3):
        for g in range(4):
            bk, bb = PA()
            for kk in range(8):
                s.mm(bk[:, :], W0v[:, kk, j * 128:(j + 1) * 128], s.XT[:, kk, g * 512:(g + 1) * 512],
                     kk == 0, kk == 7, [s.XTb[g], W0b], [bb], kk == 7)
            s.act(cT[:, j, g * 512:(g + 1) * 512], bk[:, :], AF.Copy, [bb], [cTb])
    if s.stop_after == "mla_fm":
        return
    s.act(rq, ssq, AF.Sqrt, [ssb], [rb_], scale=96.0 / 256.0, bias=96e-6)
    s.act(rkv, sskv, AF.Sqrt, [ssb], [rb_], scale=1.0 / 128.0, bias=1e-6)
    s.recip(rq, rq, [rb_], [rb_])
    s.recip(rkv, rkv, [rb_], [rb_])
    rope_tm(s, krr, krb, ta, tb2, krrb, krbb)
    for v_ in range(2):
        s.memset(VA[v_][:, :, 64:65], 1.0, [VAb[v_]])

    if s.stop_after == "mla_r":
        return

    def prep_chunks(h):
        p = h % 2
        ch = []

        def q_group(i4):
            bk, bb = PC()
            for j in range(4):
                i = i4 * 4 + j
                for c in range(2):
                    s.mm(bk[:, j * 96:(j + 1) * 96], cT[:, c, i * 128:(i + 1) * 128],
                         Wuq[:, c, h * 96:(h + 1) * 96], c == 0, c == 1, [cTb, W1b], [bb],
                         j == 3 and c == 1)
            bkv = bk[:, 0:384].rearrange("p (a b) -> p a b", a=4)
            rq4 = rq[:, i4 * 4:(i4 + 1) * 4].rearrange("p (a b) -> p a b", b=1)
            s.tt(qhb[:, i4 * 4:(i4 + 1) * 4, 0:64], bkv[:, :, 0:64], bc(rq4, [128, 4, 64]), ALU.mult,
                 [bb, rb_], [qhbb])
            s.tt(qr[:, i4 * 4:(i4 + 1) * 4, :], bkv[:, :, 64:96], bc(rq4, [128, 4, 32]), ALU.mult,
                 [bb, rb_], [qrb])

        def kv_group(i4):
            bk, bb = PC()
            for j in range(4):
                i = i4 * 4 + j
                s.mm(bk[:, j * 128:(j + 1) * 128], cT[:, 2, i * 128:(i + 1) * 128],
                     Wkv[:, h * 128:(h + 1) * 128], True, True, [cTb, W1b], [bb], j == 3)
            bkv = bk[:, :].rearrange("p (a b) -> p a b", a=4)
            r4 = rkv[:, i4 * 4:(i4 + 1) * 4].rearrange("p (a b) -> p a b", b=1)
            s.tt(khb[:, i4 * 4:(i4 + 1) * 4, 0:64], bkv[:, :, 0:64], bc(r4, [128, 4, 64]), ALU.mult,
                 [bb, rb_], [khbb])
            s.tt(VA[p][:, i4 * 4:(i4 + 1) * 4, 0:64], bkv[:, :, 64:128], bc(r4, [128, 4, 64]), ALU.mult,
                 [bb, rb_], [VAb[p]])

        def tr_group(src, srcb, dst, dstb, i4):
            bk, bb = PC()
            pb = bk[:, 0:256].bitcast(BF16)
            for j in range(4):
                i = i4 * 4 + j
                s.tr(pb[0:96, j * 128:(j + 1) * 128], src[:, i, :], s.identB, [srcb, s.CBb], [bb], j == 3)
            s.cp(dst[0:96, i4 * 512:(i4 + 1) * 512], pb[0:96, :], [bb], [dstb])

        for i4 in range(4):
            ch.append(lambda i4=i4: q_group(i4))
        ch.append(lambda: rope_tm(s, qr, qhb[:, :, 64:96], ta, tb2, qrb, qhbb))
        for i4 in range(4):
            ch.append(lambda i4=i4: kv_group(i4))
        ch.append(lambda: s.cp(khb[:, :, 64:96], krb, [krbb], [khbb]))
        for i4 in range(4):
            ch.append(lambda i4=i4: tr_group(qhb, qhbb, QT[p], QTb[p], i4))
        for i4 in range(4):
            ch.append(lambda i4=i4: tr_group(khb, khbb, KT[p], KTb[p], i4))
        return ch

    LOOK = 3
    pend = []

    fin_q = []

    def fin_tick(flush=False):
        for it in fin_q:
            it[0] -= 1
        while fin_q and (flush or fin_q[0][0] <= 0):
            fin_q.pop(0)[1]()

    def attn_finish(h, qc, ob, obb):
        os_, osb = OSr()
        s.cp(os_[0:65, :], ob[0:65, :], [obb], [osb])
        s.act(os_[64:65, :], os_[64:65, :], AF.Ln, [osb], [osb])
        s.act(os_[64:65, :], os_[64:65, :], AF.Exp, [osb], [osb], scale=-1.0)
        fin_q.append([5, lambda: attn_finish2(h, qc, os_, osb)])

    def attn_finish2(h, qc, os_, osb):
        rbk, rbb = PC()
        s.mm(rbk[0:64, :], s.CF[64:65, C_ONE:C_ONE + 64], os_[64:65, :], True, True,
             [osb, s.CFb], [rbb], True)
        r0 = (h % 2) * 64
        s.tt(YA[r0:r0 + 64, h // 2, qc * 512:(qc + 1) * 512], os_[0:64, :], rbk[0:64, :], ALU.mult,
             [osb, rbb], [YAb])

    def pv_step(item):
        (h, qc, kt, pt, ptb, ob, obb) = item
        p = h % 2
        s.mm(ob[0:65, :], VA[p][:, kt, :], pt, kt == 0, kt == 15, [ptb, VAb[p]], [obb], kt == 15)
        if kt == 15:
            attn_finish(h, qc, ob, obb)

    def attn(h, chunks):
        p = h % 2
        n = 0
        for qc in range(4):
            ob, obb = PB()
            for kt in range(16):
                n += 1
                if chunks and n >= 5 and n % 2 == 0:
                    chunks.pop(0)()
                sb_, sbb = PA()
                s.mm(sb_[:, :], KT[p][0:96, kt * 128:(kt + 1) * 128], QT[p][0:96, qc * 512:(qc + 1) * 512],
                     True, True, [KTb[p], QTb[p]], [sbb], True)
                pt, ptb = PTr()
                s.act(pt, sb_[:, :], AF.Exp, [sbb], [ptb])
                pend.append((h, qc, kt, pt, ptb, ob, obb))
                if len(pend) > LOOK:
                    pv_step(pend.pop(0))
                fin_tick()

    for c_ in prep_chunks(0):
        c_()
    wo_pre = outproj_load(s, l, 4, 0)
    for h in range(8):
        chunks = prep_chunks(h + 1) if h + 1 < 8 else []
        attn(h, chunks)
        while chunks:
            chunks.pop(0)()
    while pend:
        pv_step(pend.pop(0))
    fin_tick(flush=True)
    preload_mlstm_a(s, l)
    s.tap("ya", YA, [YAb])
    outproj_partial(s, l, YA, YAb, 4, 0, True, pre=wo_pre)


def token_decay_arrays(s, a_all, u_all, ab):
    ar = s.arena
    cs = ar.alloc([NT, 2, 4], F32, "cs")
    nb = ar.alloc([NT, 2, 4], F32, "nb")
    ecs = ar.alloc([NT, 2, 4], F32, "ecs")
    ws = ar.alloc([NT, 2, 4], F32, "ws")
    gst = ar.alloc([NT, 2, 4], F32, "gst")
    P1 = s.pool([0, 1])
    U = s.CF[:, C_U:C_U + 128]
    L = s.CF[:, C_L:C_L + 128]
    ones = s.CF[:, C_ONE:C_ONE + 128]
    bk, bb = P1()
    for d, M in ((0, U), (1, L)):
        s.mm(bk[:, d * 64:(d + 1) * 64], M, a_all[:, :, d, :], True, True, [ab, s.CFb], [bb], d == 1)
    for d in range(2):
        s.cp(cs[:, :, d, :], bk[:, d * 64:(d + 1) * 64].rearrange("p (a b) -> p a b", a=NT), [bb], [ab])
    bk2, bb2 = P1()
    s.mm(bk2[:, 0:128], ones, a_all.rearrange("p a b c -> p (a b c)"), True, True, [ab, s.CFb], [bb2], True)
    tot = bk2[:, 0:128].rearrange("p (a b c) -> p a b c", a=NT, b=2)
    s.act(gst, tot, AF.Exp, [bb2], [ab])
    s.tt(nb, u_all, cs, ALU.subtract, [ab], [ab])
    s.tt(ws, tot, nb, ALU.add, [bb2, ab], [ab])
    s.act(ws, ws, AF.Exp, [ab], [ab])
    s.act(ecs, cs, AF.Exp, [ab], [ab])
    return dict(cs=cs, nb=nb, ecs=ecs, ws=ws, gst=gst)


def decay_E(s, i, a_all, nb, ab, Ebuf, Ebb, banks2):
    U = s.CF[:, C_U:C_U + 128]
    L = s.CF[:, C_L:C_L + 128]
    for d in range(2):
        bk, bb = banks2[d]
        M = U if d == 0 else L
        mk = s.CB[:, C_MF:C_MF + 128] if d == 0 else s.CB[:, C_MB:C_MB + 128]
        for h in range(4):
            s.mm(bk[:, h * 128:(h + 1) * 128], bc(a_all[:, i, d, h:h + 1], [128, 128]), M, True, False,
                 [ab, s.CFb], [bb], False)
            s.mm(bk[:, h * 128:(h + 1) * 128], s.identB, mk, False, True, [s.CBb], [bb], h == 3)
        for h in range(4):
            s.act(Ebuf[:, d, h, :], bk[:, h * 128:(h + 1) * 128], AF.Exp, [bb, ab], [Ebb],
                  bias=nb[:, i, d, h:h + 1])


def phase_mlstm(st, l):
    s = NS(st) if isinstance(st, dict) else st
    k, ar = s.k, s.arena
    k.barrier()
    ar.reset()
    s.sc_reset()
    PA = s.pool([0, 1, 2, 3])
    wsrc = s.w_in[l].rearrange("(k p) c -> p k c", p=128)
    if "mlstm_a" not in s.pre:
        preload_mlstm_a(s, l)
    Wav, Wab = s.pre.pop("mlstm_a")
    Wb, Wbb, Wbs = s.wslot()
    Wbv = Wb[:, 0:8 * 528].rearrange("p (k c) -> p k c", k=8)
    k.dma("pool", Wbv, wsrc[:, :, 928:1456], Wbs, writes=[Wbb])
    mqT = ar.alloc([2, S], BF16, "mqT"); mkT = ar.alloc([2, S], BF16, "mkT"); fmb = Buf("mfm")
    mkTM = ar.alloc([NT, 4, 64], BF16, "mkTM"); mvA = ar.alloc([NT, 4, 65], BF16, "mvA"); tmb = Buf("mtm")
    YB = ar.alloc([2, S], BF16, "YB"); YBb = Buf("YB")
    gts = ar.alloc([NT, 2, 2, 4], F32, "gts")
    a_all = ar.alloc([NT, 2, 4], F32, "a_all"); u_all = ar.alloc([NT, 2, 4], F32, "u_all"); ab = Buf("mtok")
    Fst = ar.alloc([NT, 2, 2, 65], BF16, "F"); Fb = Buf("F")
    Sst = ar.alloc([2, 2, 65], F32, "S"); Sb = Buf("S")
    Kw = [ar.alloc([4, 64], BF16, "Kw%d" % i) for i in range(2)]; Kwb = [Buf("Kw0"), Buf("Kw1")]
    Es = [ar.alloc([2, 4, 128], BF16, "E%d" % i) for i in range(2)]; Ebs = [Buf("E0"), Buf("E1")]
    MTs = [ar.alloc([2, 4, 128], BF16, "MT%d" % i) for i in range(2)]; MTbs = [Buf("MT0"), Buf("MT1")]
    Rall = ar.alloc([2, 4, 65], F32, "Rall"); Rt = [Rall[:, 0, :, :], Rall[:, 1, :, :]]; Rb = Buf("R")
    prod = ar.alloc([2, 4, 64], F32, "prod")
    cen, sq_ = prod[:, 0, :, :], prod[:, 1, :, :]
    tmp = ar.alloc([4, 65], F32, "tmp")
    den = s.sc_alloc(8); hs = ar.alloc([4, 64], F32, "hs"); hb_ = Buf("hs")

    st4 = s.sc_alloc(4); st4b = s.sc_alloc(4)
    sgos = [ar.alloc([256], F32, "sgo%d" % i) for i in range(2)]; sgobs = [Buf("sgo0"), Buf("sgo1")]
    ybs = [ar.alloc([256], BF16, "yb%d" % i) for i in range(3)]; ybbs = [Buf("yb%d" % i) for i in range(3)]
    s.memset(mvA[:, :, :, 64:65], 1.0, [tmb])
    for i in range(NT):
        bk, bb = PA()
        for kk in range(8):
            s.mm(bk[:, 0:256], s.XT[:, kk, i * 128:(i + 1) * 128], Wav[:, kk, 256:512], kk == 0, kk == 7,
                 [s.XTb[i // 4], Wab], [bb], kk == 7)
        s.act(mkTM[:, i, :, :], bk[:, 0:256].rearrange("p (a b) -> p a b", a=4), AF.Copy, [bb], [tmb], scale=0.125)
        bk, bb = PA()
        for kk in range(8):
            s.mm(bk[:, 0:256], s.XT[:, kk, i * 128:(i + 1) * 128], Wbv[:, kk, 0:256], kk == 0, kk == 7,
                 [s.XTb[i // 4], Wbb], [bb], False)
        for kk in range(8):
            s.mm(bk[:, 256:272], s.XT[:, kk, i * 128:(i + 1) * 128], Wbv[:, kk, 512:528], kk == 0, kk == 7,
                 [s.XTb[i // 4], Wbb], [bb], kk == 7)
        s.act(mvA[:, i, :, 0:64], bk[:, 0:256].rearrange("p (a b) -> p a b", a=4), AF.Copy, [bb], [tmb])
        s.tt(gts[:, i, :, :, :].rearrange("p a b c -> p (a b c)"), bk[:, 256:272], s.PRM[:, P_GB:P_GB + 16],
             ALU.add, [bb, s.PRMb], [ab])
    for j in range(4):
        for g in range(4):
            bk, bb = PA()
            for kk in range(8):
                s.mm(bk[:, :], Wav[:, kk, j * 128:(j + 1) * 128], s.XT[:, kk, g * 512:(g + 1) * 512],
                     kk == 0, kk == 7, [s.XTb[g], Wab], [bb], kk == 7)
            if j < 2:
                s.act(mqT[:, j, g * 512:(g + 1) * 512], bk[:, :], AF.Copy, [bb], [fmb])
            else:
                s.act(mkT[:, j - 2, g * 512:(g + 1) * 512], bk[:, :], AF.Copy, [bb], [fmb], scale=0.125)
    wo_pre = outproj_load(s, l, 2, 512)
    s.cp(u_all, gts[:, :, :, 0, :], [ab], [ab])
    s.act(a_all, gts[:, :, :, 1, :], AF.Exp, [ab], [ab], scale=-1.0)
    s.act(a_all, a_all, AF.Ln, [ab], [ab], bias=1.0)
    s.ts(a_all, a_all, -1.0, None, ALU.mult, None, [ab], [ab])
    if s.stop_after == "mlstm_a":
        return
    T = token_decay_arrays(s, a_all, u_all, ab)
    cs, nb, ecs, ws, gst = T["cs"], T["nb"], T["ecs"], T["ws"], T["gst"]
    if s.stop_after == "mlstm_b":
        return
    s.memset(Sst, 0.0, [Sb])
    P23 = [s.pool([2, 4]), s.pool([3, 5])]
    for c in range(NT):
        for d in range(2):
            t_ = c if d == 0 else NT - 1 - c
            s.tt(Kw[d], mkTM[:, t_, :, :], bc(ws[:, t_, d, :].rearrange("p (a b) -> p a b", b=1), [128, 4, 64]),
                 ALU.mult, [tmb, ab], [Kwb[d]])
            bk, bb = P23[d]()
            for c2 in range(2):
                s.mm(bk[:, c2 * 130:(c2 + 1) * 130], Kw[d][:, 2 * c2:2 * c2 + 2, :].rearrange("p a b -> p (a b)"),
                     mvA[:, t_, 2 * c2:2 * c2 + 2, :].rearrange("p a b -> p (a b)"), True, True,
                     [Kwb[d], tmb], [bb], c2 == 1)
            s.act(Fst[:, t_, d, :, :], Sst[:, d, :, :], AF.Copy, [Sb], [Fb])
            bv = bk[:, 0:260].rearrange("p (a b) -> p a b", a=2)
            for hp in range(2):
                r0, r1 = hp * 64, hp * 64 + 64
                gsel = gst[r0:r1, t_, d, :].rearrange("p (a b) -> p a b", b=2)[:, :, hp:hp + 1]
                s.tt(Sst[r0:r1, d, :, :], Sst[r0:r1, d, :, :], bc(gsel, [64, 2, 65]), ALU.mult, [Sb, ab], [Sb])
                s.tt(Sst[r0:r1, d, :, :], Sst[r0:r1, d, :, :], bv[r0:r1, :, hp * 65:(hp + 1) * 65], ALU.add,
                     [Sb, bb], [Sb])
    if s.stop_after == "mlstm_c":
        return
    bE = [(s.pool([0])()), (s.pool([1])())]
    bS = s.pool([2])()
    bI = [s.pool([3])(), s.pool([4])()]
    bJ = [s.pool([5])(), s.pool([6])()]
    bT = s.pool([7])()
    def stage_a(i):
        sl = slice(i * 128, (i + 1) * 128)
        E, Eb, MT, MTb, sgo, sgob = Es[i % 2], Ebs[i % 2], MTs[i % 2], MTbs[i % 2], sgos[i % 2], sgobs[i % 2]
        decay_E(s, i, a_all, nb, ab, E, Eb, bE)
        for h in range(4):
            r0 = (h % 2) * 64
            s.mm(bS[0][:, h * 128:(h + 1) * 128], mkT[r0:r0 + 64, h // 2, sl], mqT[r0:r0 + 64, h // 2, sl],
                 True, True, [fmb], [bS[1]], True, serial=True)
        for d in range(2):
            s.tt(MT[:, d, :, :], bS[0][:, :].rearrange("p (a b) -> p a b", a=4), E[:, d, :, :], ALU.mult,
                 [bS[1], Eb], [MTb])
        bk, bb = bT
        for kk in range(8):
            s.mm(bk[:, 0:256], s.XT[:, kk, sl], Wbv[:, kk, 256:512], kk == 0, kk == 7,
                 [s.XTb[i // 4], Wbb], [bb], kk == 7)
        s.act(sgo, bk[:, 0:256], AF.Sigmoid, [bb], [sgob])

    def stage_b(i):
        sl = slice(i * 128, (i + 1) * 128)
        yb, ybb = ybs[i % 3], ybbs[i % 3]
        E, Eb, MT, MTb, sgo, sgob = Es[i % 2], Ebs[i % 2], MTs[i % 2], MTbs[i % 2], sgos[i % 2], sgobs[i % 2]
        for d in range(2):
            for h in range(4):
                r0 = (h % 2) * 64
                s.mm(bJ[d][0][:, h * 65:(h + 1) * 65], mqT[r0:r0 + 64, h // 2, sl], Fst[r0:r0 + 64, i, d, h // 2, :],
                     True, True, [fmb, Fb], [bJ[d][1]], True, serial=True)
            for h in range(4):
                s.mm(bI[d][0][:, h * 65:(h + 1) * 65], MT[:, d, h, :], mvA[:, i, h, :], True, True,
                     [MTb, tmb], [bI[d][1]], h == 3)
            s.tt(tmp, bJ[d][0][:, 0:260].rearrange("p (a b) -> p a b", a=4),
                 bc(ecs[:, i, d, :].rearrange("p (a b) -> p a b", b=1), [128, 4, 65]), ALU.mult,
                 [bJ[d][1], ab], [Rb])
            s.tt(Rt[d], bI[d][0][:, 0:260].rearrange("p (a b) -> p a b", a=4), tmp, ALU.add, [bI[d][1], Rb], [Rb])
        d8 = den.rearrange("p (a b) -> p a b", a=2)
        s.ts(d8, Rall[:, :, :, 64], -1.0, None, ALU.mult, None, [Rb], [Rb])
        s.tt(d8, d8, Rall[:, :, :, 64], ALU.max, [Rb], [Rb])
        s.ts(d8, d8, 1.0, None, ALU.max, None, [Rb], [Rb])
        s.recip(den, den, [Rb], [Rb])
        s.tt(prod, Rall[:, :, :, 0:64], bc(den.rearrange("p (a b c) -> p a b c", a=2, c=1), [128, 2, 4, 64]),
             ALU.mult, [Rb], [hb_])
        s.tt(hs, prod[:, 0, :, :], prod[:, 1, :, :], ALU.add, [hb_], [hb_])
        s.red(st4, hs, ALU.add, [hb_], [hb_])
        s.ts(st4, st4, 1.0 / 64.0, None, ALU.mult, None, [hb_], [hb_])
        s.tt(cen, hs, bc(st4.rearrange("p (a b) -> p a b", b=1), [128, 4, 64]), ALU.subtract, [hb_], [hb_])
        s.tt(sq_, cen, cen, ALU.mult, [hb_], [hb_])
        s.red(st4b, sq_, ALU.add, [hb_], [hb_])
        s.act(st4b, st4b, AF.Sqrt, [hb_], [hb_], scale=1.0 / 64.0, bias=1e-5)
        s.recip(st4b, st4b, [hb_], [hb_])
        s.tt(cen, cen, bc(st4b.rearrange("p (a b) -> p a b", b=1), [128, 4, 64]), ALU.mult, [hb_], [hb_])
        cf = cen.rearrange("p a b -> p (a b)")
        s.tt(cf, cf, s.PRM[:, P_MN:P_MN + 256], ALU.mult, [hb_, s.PRMb], [hb_])
        s.tt(yb, cf, sgo, ALU.mult, [hb_, sgob], [ybb])

    def stage_c(i):
        sl = slice(i * 128, (i + 1) * 128)
        yb, ybb = ybs[i % 3], ybbs[i % 3]
        bk, bb = bT
        pb = bk[:, 0:256].bitcast(BF16)
        for c in range(2):
            s.tr(pb[:, c * 128:(c + 1) * 128], yb[:, c * 128:(c + 1) * 128], s.identB, [ybb, s.CBb], [bb], c == 1)
        s.act(YB[:, :, sl], pb[:, 0:256].rearrange("p (a b) -> p a b", a=2), AF.Copy, [bb], [YBb])

    stage_a(0)
    for i in range(NT + 1):
        if i + 1 < NT:
            stage_a(i + 1)
        if i < NT:
            stage_b(i)
        if i >= 1:
            stage_c(i - 1)
    s.tap("yb", YB, [YBb])
    preload_ssd_x(s, l)
    outproj_partial(s, l, YB, YBb, 2, 512, False, pre=wo_pre)


def phase_ssd(st, l):
    s = NS(st) if isinstance(st, dict) else st
    k, ar = s.k, s.arena
    k.barrier()
    ar.reset()
    s.sc_reset()
    PA = s.pool([0, 1, 2, 3])
    PT = s.pool([6, 7])
    wsrc = s.w_in[l].rearrange("(k p) c -> p k c", p=128)
    if "ssd_x" not in s.pre:
        preload_ssd_x(s, l)
    Wxv, Wxb = s.pre.pop("ssd_x")
    Wz, Wzb, Wzs = s.wslot()
    Wzv = Wz[:, 0:8 * 264].rearrange("p (k c) -> p k c", k=8)
    k.dma("pool", Wzv[:, :, 0:256], wsrc[:, :, 1456:1712], Wzs, writes=[Wzb])
    k.dma("pool", Wzv[:, :, 256:264], wsrc[:, :, 2480:2488], Wzs, writes=[Wzb])
    BCT = ar.alloc([4, S], BF16, "BCT"); bcb = Buf("BCT")
    BTM = ar.alloc([NT, 2, 128], BF16, "BTM"); btb = Buf("BTM")
    xTM = ar.alloc([NT, 4, 64], BF16, "xTM"); tmb = Buf("stm")
    YC = BTM.rearrange("p a b c -> p (a b c)").rearrange("p (a b) -> p a b", a=2); YCb = btb
    dtr = ar.alloc([NT, 2, 4], F32, "dtr")
    a_all = ar.alloc([NT, 2, 4], F32, "a_all"); u_all = ar.alloc([NT, 2, 4], F32, "u_all"); ab = Buf("stok")
    aneg = s.sc_alloc(8)
    T = None
    mark = ar.off
    bk, bb = PA()
    for i in range(NT):
        for kk in range(8):
            s.mm(bk[:, i * 8:(i + 1) * 8], s.XT[:, kk, i * 128:(i + 1) * 128], Wzv[:, kk, 256:264], kk == 0, kk == 7,
                 [s.XTb[i // 4], Wzb], [bb], i == NT - 1 and kk == 7)
    dflat = dtr.rearrange("p a b c -> p a (b c)")
    s.tt(dflat, bk[:, 0:128].rearrange("p (a b) -> p a b", a=NT),
         bc(s.PRM[:, P_DTB:P_DTB + 8].rearrange("p (a b) -> p a b", a=1), [128, NT, 8]), ALU.add,
         [bb, s.PRMb], [ab])
    s.act(dtr, dtr, AF.Exp, [ab], [ab])
    s.act(dtr, dtr, AF.Ln, [ab], [ab], bias=1.0)
    s.act(u_all, dtr, AF.Ln, [ab], [ab])
    s.act(aneg, s.PRM[:, P_AL:P_AL + 8], AF.Exp, [s.PRMb], [ab])
    s.ts(aneg, aneg, -1.0, None, ALU.mult, None, [ab], [ab])
    s.tt(a_all.rearrange("p a b c -> p a (b c)"), dflat,
         bc(aneg.rearrange("p (a b) -> p a b", a=1), [128, NT, 8]), ALU.mult, [ab], [ab])
    cin = [ar.alloc([2052], BF16, "cin%d" % i) for i in range(2)]; cinb = [Buf("cin0"), Buf("cin1")]
    acc = [ar.alloc([512], F32, "acc%d" % i) for i in range(2)]; accb = [Buf("acc0"), Buf("acc1")]
    xfm = [ar.alloc([S], BF16, "xfm%d" % i) for i in range(2)]; xfmb = [Buf("xfm0"), Buf("xfm1")]
    for p_ in range(2):
        s.memset(cin[p_][:, 0:2], 0.0, [cinb[p_]])
        s.memset(cin[p_][:, 2050:2052], 0.0, [cinb[p_]])
    accr = Rot([0, 1])
    for j in range(6):
        p_ = j % 2
        for g in range(4):
            bk, bb = PA()
            for kk in range(8):
                s.mm(bk[:, :], Wxv[:, kk, j * 128:(j + 1) * 128], s.XT[:, kk, g * 512:(g + 1) * 512],
                     kk == 0, kk == 7, [s.XTb[g], Wxb], [bb], kk == 7)
            s.act(cin[p_][:, 2 + g * 512:2 + (g + 1) * 512], bk[:, :], AF.Copy, [bb], [cinb[p_]])
        if j < 2:
            dst, dstb = xfm[p_], xfmb[p_]
        else:
            dst, dstb = BCT[:, j - 2, :], bcb
        for g in range(4):
            a_ = accr()
            cw = lambda jj: s.PRM[:, P_CW + j * 5 + jj:P_CW + j * 5 + jj + 1]
            s.ts(acc[a_], cin[p_][:, g * 512:g * 512 + 512], cw(0), None, ALU.mult, None,
                 [cinb[p_], s.PRMb], [accb[a_]])
            for jj in range(1, 5):
                s.stt(acc[a_], cin[p_][:, g * 512 + jj:g * 512 + jj + 512], cw(jj), acc[a_], ALU.mult, ALU.add,
                      [cinb[p_], s.PRMb, accb[a_]], [accb[a_]])
            s.act(dst[:, g * 512:(g + 1) * 512], acc[a_], AF.Silu, [accb[a_], s.PRMb], [dstb],
                  bias=s.PRM[:, P_CB + j:P_CB + j + 1])
        if j < 4:
            src = xfm[p_] if j < 2 else BCT[:, j - 2, :]
            srcb = xfmb[p_] if j < 2 else bcb
            for i4 in range(4):
                bk, bb = PT()
                pb = bk[:, 0:256].bitcast(BF16)
                for q in range(4):
                    i = i4 * 4 + q
                    s.tr(pb[:, q * 128:(q + 1) * 128], src[:, i * 128:(i + 1) * 128], s.identB, [srcb, s.CBb], [bb], q == 3)
                if j < 2:
                    s.act(xTM[:, i4 * 4:(i4 + 1) * 4, 2 * j:2 * j + 2, :],
                          pb[:, :].rearrange("p (a b c) -> p a b c", a=4, b=2), AF.Copy, [bb], [tmb])
                else:
                    s.act(BTM[:, i4 * 4:(i4 + 1) * 4, j - 2, :], pb[:, :].rearrange("p (a b) -> p a b", a=4),
                          AF.Copy, [bb], [btb])
    s.tap("bct", BCT, [bcb])
    wo_pre = outproj_load(s, l, 2, 768)
    k.barrier()
    ar.off = mark
    T = token_decay_arrays(s, a_all, u_all, ab)
    cs, nb, ecs, ws, gst = T["cs"], T["nb"], T["ecs"], T["ws"], T["gst"]
    Fst = ar.alloc([NT, 2, 4, 64], BF16, "F"); Fb = Buf("F")
    Sst = ar.alloc([2, 4, 64], F32, "S"); Sb = Buf("S")
    Kw = [ar.alloc([4, 128], BF16, "Kw%d" % i) for i in range(2)]; Kwb = [Buf("Kw0"), Buf("Kw1")]
    Es = [ar.alloc([2, 4, 128], BF16, "E%d" % i) for i in range(2)]; Ebs = [Buf("E0"), Buf("E1")]
    MTs = [ar.alloc([2, 4, 128], BF16, "MT%d" % i) for i in range(2)]; MTbs = [Buf("MT0"), Buf("MT1")]
    Rt = [ar.alloc([4, 64], F32, "R%d" % i) for i in range(2)]; Rb = Buf("R")
    tmp = ar.alloc([4, 64], F32, "tmp")
    yt = ar.alloc([4, 64], F32, "yt"); ytb = Buf("yt")
    sq_ = tmp
    zss = [ar.alloc([256], F32, "zs%d" % i) for i in range(2)]; zsbs = [Buf("zs0"), Buf("zs1")]
    ybs = [ar.alloc([256], BF16, "yb%d" % i) for i in range(3)]; ybbs = [Buf("yb%d" % i) for i in range(3)]
    ss2 = s.sc_alloc(2)
    s.memset(Sst, 0.0, [Sb])
    P23 = [s.pool([2, 4]), s.pool([3, 5])]
    for c in range(NT):
        for d in range(2):
            t_ = c if d == 0 else NT - 1 - c
            s.tt(Kw[d].rearrange("p (g e) n -> p g e n", g=2),
                 bc(BTM[:, t_, :, :].rearrange("p g (o n) -> p g o n", o=1), [128, 2, 2, 128]),
                 bc(ws[:, t_, d, :].rearrange("p (g e o) -> p g e o", g=2, o=1), [128, 2, 2, 128]),
                 ALU.mult, [btb, ab], [Kwb[d]])
            bk, bb = P23[d]()
            for h in range(4):
                s.mm(bk[:, h * 64:(h + 1) * 64], Kw[d][:, h, :], xTM[:, t_, h, :], True, True,
                     [Kwb[d], tmb], [bb], h == 3)
            s.act(Fst[:, t_, d, :, :], Sst[:, d, :, :], AF.Copy, [Sb], [Fb])
            s.tt(Sst[:, d, :, :], Sst[:, d, :, :],
                 bc(gst[:, t_, d, :].rearrange("p (a b) -> p a b", b=1), [128, 4, 64]), ALU.mult, [Sb, ab], [Sb])
            s.tt(Sst[:, d, :, :], Sst[:, d, :, :], bk[:, 0:256].rearrange("p (a b) -> p a b", a=4), ALU.add,
                 [Sb, bb], [Sb])
    bE = [(s.pool([0])()), (s.pool([1])())]
    bS = s.pool([2])()
    bI = [s.pool([3])(), s.pool([4])()]
    bJ = [s.pool([5])(), s.pool([6])()]
    bT = s.pool([7])()
    def stage_a(i):
        sl = slice(i * 128, (i + 1) * 128)
        E, Eb, MT, MTb, zs, zsb = Es[i % 2], Ebs[i % 2], MTs[i % 2], MTbs[i % 2], zss[i % 2], zsbs[i % 2]
        decay_E(s, i, a_all, nb, ab, E, Eb, bE)
        for g in range(2):
            s.mm(bS[0][:, g * 128:(g + 1) * 128], BCT[:, g, sl], BCT[:, 2 + g, sl], True, True, [bcb], [bS[1]], g == 1)
        for d in range(2):
            s.tt(MT[:, d, :, :].rearrange("p (g e) n -> p g e n", g=2),
                 bc(bS[0][:, 0:256].rearrange("p (g o n) -> p g o n", g=2, o=1), [128, 2, 2, 128]),
                 E[:, d, :, :].rearrange("p (g e) n -> p g e n", g=2), ALU.mult, [bS[1], Eb], [MTb])
        bk, bb = bT
        for kk in range(8):
            s.mm(bk[:, 0:256], s.XT[:, kk, sl], Wzv[:, kk, 0:256], kk == 0, kk == 7,
                 [s.XTb[i // 4], Wzb], [bb], kk == 7)
        s.act(zs, bk[:, 0:256], AF.Silu, [bb], [zsb])

    def stage_b(i):
        sl = slice(i * 128, (i + 1) * 128)
        yb, ybb = ybs[i % 3], ybbs[i % 3]
        E, Eb, MT, MTb, zs, zsb = Es[i % 2], Ebs[i % 2], MTs[i % 2], MTbs[i % 2], zss[i % 2], zsbs[i % 2]
        for d in range(2):
            for h in range(4):
                s.mm(bJ[d][0][:, h * 64:(h + 1) * 64], BCT[:, 2 + h // 2, sl], Fst[:, i, d, h, :], True, True,
                     [bcb, Fb], [bJ[d][1]], h == 3)
            for h in range(4):
                s.mm(bI[d][0][:, h * 64:(h + 1) * 64], MT[:, d, h, :], xTM[:, i, h, :], True, True,
                     [MTb, tmb], [bI[d][1]], h == 3)
            s.tt(tmp, bJ[d][0][:, 0:256].rearrange("p (a b) -> p a b", a=4),
                 bc(ecs[:, i, d, :].rearrange("p (a b) -> p a b", b=1), [128, 4, 64]), ALU.mult,
                 [bJ[d][1], ab], [Rb])
            s.tt(Rt[d], bI[d][0][:, 0:256].rearrange("p (a b) -> p a b", a=4), tmp, ALU.add, [bI[d][1], Rb], [Rb])
        s.tt(yt, Rt[0], Rt[1], ALU.add, [Rb], [ytb])
        s.tt(sq_, xTM[:, i, :, :], bc(s.PRM[:, P_SD:P_SD + 4].rearrange("p (a b) -> p a b", b=1), [128, 4, 64]),
             ALU.mult, [tmb, s.PRMb], [ytb, Rb])
        s.tt(yt, yt, sq_, ALU.add, [ytb, Rb], [ytb])
        yf = yt.rearrange("p a b -> p (a b)")
        s.tt(yf, yf, zs, ALU.mult, [ytb, zsb], [ytb])
        s.tt(sq_, yt, yt, ALU.mult, [ytb], [ytb, Rb])
        s.red(ss2, sq_.rearrange("p (g e) n -> p g (e n)", g=2), ALU.add, [ytb, Rb], [ytb])
        s.act(ss2, ss2, AF.Sqrt, [ytb], [ytb], scale=1.0 / 128.0, bias=1e-6)
        s.recip(ss2, ss2, [ytb], [ytb])
        s.tt(yt.rearrange("p (g e) n -> p g (e n)", g=2), yt.rearrange("p (g e) n -> p g (e n)", g=2),
             bc(ss2.rearrange("p (a b) -> p a b", b=1), [128, 2, 128]), ALU.mult, [ytb], [ytb])
        s.tt(yb, yf, s.PRM[:, P_SN:P_SN + 256], ALU.mult, [ytb, s.PRMb], [ybb])

    def stage_c(i):
        sl = slice(i * 128, (i + 1) * 128)
        yb, ybb = ybs[i % 3], ybbs[i % 3]
        bk, bb = bT
        pb = bk[:, 0:256].bitcast(BF16)
        for c in range(2):
            s.tr(pb[:, c * 128:(c + 1) * 128], yb[:, c * 128:(c + 1) * 128], s.identB, [ybb, s.CBb], [bb], c == 1)
        s.act(YC[:, :, sl], pb[:, 0:256].rearrange("p (a b) -> p a b", a=2), AF.Copy, [bb], [YCb])

    stage_a(0)
    for i in range(NT + 1):
        if i + 1 < NT:
            stage_a(i + 1)
        if i < NT:
            stage_b(i)
        if i >= 1:
            stage_c(i - 1)
    s.tap("yc", YC, [YCb])
    outproj_partial(s, l, YC, YCb, 2, 768, False, pre=wo_pre)


def layernorm_tiles(s, l, which, T1s, T1bs, LNP, LNPb, post):
    st6 = s.arena.alloc([2, 6], F32, "st6")
    mv = s.sc_alloc(2)
    sd = s.sc_alloc(1)
    lb = Buf("lnstat")
    def front(i):
        T1, T1b = T1s[i % 2], T1bs[i % 2]
        for hf in range(2):
            s.k.op("dve", lambda h: h.bn_stats(out=st6[:, hf, :], in_=s.X[:, i, hf * 512:(hf + 1) * 512]),
                   reads=[s.Xb[i]], writes=[lb])
        s.k.op("dve", lambda h: h.bn_aggr(out=mv, in_=st6.rearrange("p a b -> p (a b)")), reads=[lb], writes=[lb])
        s.act(sd, mv[:, 1:2], AF.Sqrt, [lb], [lb], scale=1.0, bias=1e-5)
        s.recip(sd, sd, [lb], [lb])
        s.ts(T1, s.X[:, i, :], mv[:, 0:1], sd, ALU.subtract, ALU.mult, [s.Xb[i], lb], [T1b])
        s.tt(T1, T1, LNP[:, 0, :], ALU.mult, [T1b, LNPb], [T1b])
        s.tt(T1, T1, LNP[:, 1, :], ALU.add, [T1b, LNPb], [T1b])

    front(0)
    for i in range(NT):
        if i + 1 < NT:
            front(i + 1)
        post(i, T1s[i % 2], T1bs[i % 2])


def phase_ln_router_moe(st, l, last):
    s = NS(st) if isinstance(st, dict) else st
    k, ar = s.k, s.arena
    k.barrier()
    ar.reset()
    s.sc_reset()
    s_l = s.s_l
    comb = ar.alloc([NT, 4, 4], F32, "comb"); combb = Buf("comb")
    mark = ar.off

    def load_gu(e):
        W, Wb, Ws = s.wslot()
        Wv = W[:, 0:4096].rearrange("p (k c) -> p k c", k=8)
        k.dma("pool", Wv[:, :, 0:256], s.d_wg[l, e].rearrange("(k p) f -> p k f", p=128), Ws, writes=[Wb])
        k.dma("pool", Wv[:, :, 256:512], s.d_wu[l, e].rearrange("(k p) f -> p k f", p=128), Ws, writes=[Wb])
        return Wv, Wb

    e0_pre = load_gu(0)
    LNP = ar.alloc([2, D], F32, "LNP"); LNPb = Buf("LNP")
    for q in range(2):
        k.dma("sp", LNP[:, q, :], s.dlnp[l, q:q + 1, :].to_broadcast([128, D]), s_l, writes=[LNPb])
    T1s = [ar.alloc([D], F32, "T1%d" % i) for i in range(2)]; T1bs = [Buf("T10"), Buf("T11")]
    x32s = [ar.alloc([8, 128], F32, "x32%d" % i) for i in range(2)]; x32bs = [Buf("x320"), Buf("x321")]
    RL = ar.alloc([NT, 20], F32, "RL"); RLb = Buf("RL")
    PR = s.pool([4, 5])

    def post1(i, T1, T1b):
        x32, x32b = x32s[i % 2], x32bs[i % 2]
        s.transpose_x_tile(i, T1, [T1b], dst32=x32, dst32b=x32b)
        s.act(s.X[:, i, :], T1, AF.Copy, [T1b], [s.Xb[i]], scale=ALPHA)
        bk, bb = PR()
        for c in range(8):
            s.mm(bk[:, 0:20], x32[:, c, :], s.PRM[:, P_RW + c * 20:P_RW + (c + 1) * 20], c == 0, c == 7,
                 [x32b, s.PRMb], [bb], c == 7)
        s.tt(RL[:, i, :], bk[:, 0:20], s.PRM[:, P_RB:P_RB + 20], ALU.add, [bb, s.PRMb], [RLb])

    layernorm_tiles(s, l, 0, T1s, T1bs, LNP, LNPb, post1)
    s.tap("x1", s.X[:, :, :], s.Xb)
    gl = RL[:, :, 0:4]
    el = RL[:, :, 4:20].rearrange("p t (g e) -> p t g e", g=4)
    A1 = lambda n: ar.alloc([NT, n], F32, "r%d" % n)
    gmax, gsum, gp, emax1, emax2, ssum, rg = A1(1), A1(1), A1(1), A1(1), A1(1), A1(1), A1(1)
    gsh, ohg, esel, esel2, m1, m2, ee = A1(4), A1(4), A1(4), A1(4), A1(4), A1(4), A1(4)
    t16 = ar.alloc([NT, 4, 4], F32, "t16")
    rb_ = Buf("route")
    b4 = lambda a: bc(a, [128, NT, 4])
    R_, W_ = [RLb, rb_], [rb_]
    s.red(gmax, gl, ALU.max, R_, W_)
    s.tt(gsh, gl, b4(gmax), ALU.subtract, R_, W_)
    s.act(gsh, gsh, AF.Exp, R_, W_)
    s.red(gsum, gsh, ALU.add, R_, W_)
    s.recip(gp, gsum, R_, W_)
    s.tt(ohg, gl, b4(gmax), ALU.is_equal, R_, W_)
    s.tt(t16, el, bc(ohg.rearrange("p t (g o) -> p t g o", o=1), [128, NT, 4, 4]), ALU.mult, R_, W_)
    s.red(esel, t16.rearrange("p t g e -> p t e g"), ALU.add, R_, W_)
    s.red(emax1, esel, ALU.max, R_, W_)
    s.tt(m1, esel, b4(emax1), ALU.is_equal, R_, W_)
    s.stt(esel2, m1, -1e30, esel, ALU.mult, ALU.add, R_, W_)
    s.red(emax2, esel2, ALU.max, R_, W_)
    s.tt(m2, esel2, b4(emax2), ALU.is_equal, R_, W_)
    s.tt(m1, m1, m2, ALU.add, R_, W_)
    s.tt(ee, esel, b4(emax1), ALU.subtract, R_, W_)
    s.act(ee, ee, AF.Exp, R_, W_)
    s.tt(ee, ee, m1, ALU.mult, R_, W_)
    s.red(ssum, ee, ALU.add, R_, W_)
    s.recip(rg, ssum, R_, W_)
    s.tt(rg, rg, gp, ALU.mult, R_, W_)
    s.tt(ee, ee, b4(rg), ALU.mult, R_, W_)
    s.tt(comb, bc(ohg.rearrange("p t (g o) -> p t g o", o=1), [128, NT, 4, 4]),
         bc(ee.rearrange("p t (o e) -> p t o e", o=1), [128, NT, 4, 4]), ALU.mult, R_, [combb])
    s.tap("comb", comb, [combb])
    if s.stop_after == "ln1":
        return
    k.barrier()
    ar.off = mark
    hT = [ar.alloc([2, S], BF16, "hT%d" % i) for i in range(4)]; hTb = [Buf("hT%d" % i) for i in range(4)]
    WD = [[ar.alloc([2, D], BF16, "WD%d_%d" % (g_, e_)) for e_ in range(4)] for g_ in range(2)]
    WDb = [[Buf("WD") for _ in range(4)] for _ in range(2)]
    WDs = s.wd_sems
    NB_ = 4
    sgs = [ar.alloc([256], F32, "sg%d" % i) for i in range(NB_)]; sgb = [Buf("sg%d" % i) for i in range(NB_)]
    hms = [ar.alloc([256], BF16, "hm%d" % i) for i in range(NB_)]; hmb = [Buf("hm%d" % i) for i in range(NB_)]
    trq = []
    PG = s.pool([0, 1, 2])
    PTr = s.pool([3, 4])
    PD = s.pool([5, 6, 7])
    slots = {}

    def load_expert(e):
        if e == 0:
            Wv, Wb = e0_pre
        else:
            Wv, Wb = load_gu(e)
        g_, e_ = (e // 4) % 2, e % 4
        k.dma("pool", WD[g_][e_], s.d_wd[l, e].rearrange("(c p) n -> p c n", p=128), WDs[g_ * 4 + e_],
              writes=[WDb[g_][e_]])
        slots[e] = (Wv, Wb)

    load_expert(0)
    cnt = 0
    for e in range(16):
        if e + 1 < 16:
            load_expert(e + 1)
        Wv, Wb = slots.pop(e)
        e4 = e % 4
        def moe_tr(item):
            (i_, hm_, hmb2, e4_) = item
            sl_ = slice(i_ * 128, (i_ + 1) * 128)
            tk_, tb_ = PTr()
            pb = tk_[:, 0:128].bitcast(BF16)
            for c in range(2):
                s.tr(pb[:, c * 128:(c + 1) * 128], hm_[:, c * 128:(c + 1) * 128], s.identB, [hmb2, s.CBb], [tb_], c == 1)
            s.act(hT[e4_][:, :, sl_], pb[:, 0:256].rearrange("p (a b) -> p a b", a=2), AF.Copy, [tb_], [hTb[e4_]])

        for i in range(NT):
            sl = slice(i * 128, (i + 1) * 128)
            bk, bb = PG()
            for kk in range(8):
                s.mm(bk[:, :], s.XT[:, kk, sl], Wv[:, kk, :], kk == 0, kk == 7, [s.XTb[i // 4], Wb], [bb], kk == 7)
            sg, sgb_ = sgs[cnt % NB_], sgb[cnt % NB_]
            hm, hmb_ = hms[cnt % NB_], hmb[cnt % NB_]
            cnt += 1
            s.act(sg, bk[:, 0:256], AF.Silu, [bb], [sgb_])
            s.stt(hm, bk[:, 256:512], comb[:, i, e // 4, e4:e4 + 1], sg, ALU.mult, ALU.mult,
                  [bb, combb, sgb_], [hmb_])
            trq.append((i, hm, hmb_, e4))
            if len(trq) > 2:
                moe_tr(trq.pop(0))
        if e4 == 3:
            while trq:
                moe_tr(trq.pop(0))
        if e4 == 3:
            g_ = (e // 4) % 2
            for i in range(NT):
                sl = slice(i * 128, (i + 1) * 128)
                for hf in range(2):
                    bk, bb = PD()
                    n = 0
                    for ee_ in range(4):
                        for c in range(2):
                            s.mm(bk[:, :], hT[ee_][:, c, sl], WD[g_][ee_][:, c, hf * 512:(hf + 1) * 512],
                                 n == 0, n == 7, [hTb[ee_], WDb[g_][ee_]], [bb], n == 7)
                            n += 1
                    xs = s.X[:, i, hf * 512:(hf + 1) * 512]
                    s.tt(xs, xs, bk[:, :], ALU.add, [s.Xb[i], bb], [s.Xb[i]])
    if s.stop_after == "moe":
        return
    if not last:
        preload_mla(s, l + 1)
    k.barrier()
    ar.off = mark
    LNP = ar.alloc([2, D], F32, "LNP2"); LNPb = Buf("LNP2")
    for q in range(2):
        k.dma("sp", LNP[:, q, :], s.dlnp[l, 2 + q:3 + q, :].to_broadcast([128, D]), s_l, writes=[LNPb])
    T1s = [ar.alloc([D], F32, "T2%d" % i) for i in range(2)]; T1bs = [Buf("T20"), Buf("T21")]

    def post2(i, T1, T1b):
        if not last:
            s.transpose_x_tile(i, T1, [T1b])
        s.act(s.X[:, i, :], T1, AF.Copy, [T1b], [s.Xb[i]])

    layernorm_tiles(s, l, 1, T1s, T1bs, LNP, LNPb, post2)
    k.barrier()


def _consts():
    c = np.zeros((128, CW), np.float32)
    r = np.arange(128)
    c[:, C_ID:C_ID + 128] = np.eye(128, dtype=np.float32)
    c[:, C_U:C_U + 128] = (r[:, None] <= r[None, :]).astype(np.float32)
    c[:, C_L:C_L + 128] = (r[:, None] >= r[None, :]).astype(np.float32)
    c[:, C_MF:C_MF + 128] = np.where(r[:, None] <= r[None, :], 0.0, NEG).astype(np.float32)
    c[:, C_MB:C_MB + 128] = np.where(r[:, None] >= r[None, :], 0.0, NEG).astype(np.float32)
    c[:, C_ONE:C_ONE + 128] = 1.0
    inv = (10000.0 ** (-np.arange(0, 32, 2, dtype=np.float32) / np.float32(32))).astype(np.float32)
    c[:, C_IF:C_IF + 16] = inv[None, :]
    return c


def _pack_prm(inp, l):
    p = np.zeros((128, PW), np.float32)
    rep = lambda v: np.broadcast_to(np.asarray(v, np.float32).reshape(1, -1), (128, np.asarray(v).size))
    p[:, P_GQ:P_GQ + 2] = inp["mla_q_norm"][l].reshape(2, 128).T
    p[:, P_GKV] = inp["mla_kv_norm"][l]
    p[:, P_GB:P_GB + 16] = rep(inp["mlstm_gate_bias"][l])
    p[:, P_MN:P_MN + 256] = rep(inp["mlstm_norm"][l])
    cw = inp["ssd_conv_w"][l]
    p[:, P_CW:P_CW + 30] = cw.reshape(5, 6, 128).transpose(2, 1, 0).reshape(128, 30)
    p[:, P_CB:P_CB + 6] = inp["ssd_conv_b"][l].reshape(6, 128).T
    p[:, P_DTB:P_DTB + 8] = rep(inp["ssd_dt_bias"][l])
    p[:, P_AL:P_AL + 8] = rep(inp["ssd_a_log"][l])
    p[:, P_SD:P_SD + 4] = rep(inp["ssd_d"][l])
    p[:, P_SN:P_SN + 256] = rep(inp["ssd_norm"][l])
    rb = np.concatenate([inp["router_group_b"][l].reshape(-1), inp["router_expert_b"][l].reshape(-1)])
    p[:, P_RB:P_RB + 20] = rep(rb)
    wr = np.concatenate([inp["router_group_w"][l],
                         inp["router_expert_w"][l].transpose(1, 0, 2).reshape(1024, 16)], axis=1)
    p[:, P_RW:P_RW + 160] = wr.reshape(8, 128, 20).transpose(1, 0, 2).reshape(128, 160)
    return p


def make_in_maps(inp, layers):
    inp = {k_: np.asarray(v) for k_, v in inp.items()}
    L = list(layers)
    sl = lambda a: np.ascontiguousarray(a[L])
    shared = {
        "cst": _consts(),
        "prm": np.stack([_pack_prm(inp, l) for l in L]),
        "w_in": sl(inp["w_in"]), "w_uq": sl(inp["mla_w_uq"]), "w_ukv": sl(inp["mla_w_ukv"]),
        "w_out": sl(inp["w_out"]),
        "lnp": np.stack([np.stack([inp["ln1_g"][l], inp["ln1_b"][l], inp["ln2_g"][l], inp["ln2_b"][l]]) for l in L]),
        "wg": sl(inp["expert_w_gate"]), "wu": sl(inp["expert_w_up"]), "wd": sl(inp["expert_w_down"]),
    }
    return shared


_CACHE = {}


def _prog(NL):
    if NL not in _CACHE:
        _CACHE[NL] = build(NL)[0]
    return _CACHE[NL]


FUSED = True


def kernel(**inputs):
    x = np.asarray(inputs["x"], np.float32)
    pos = np.asarray(inputs["positions"]).astype(np.int32)
    B = x.shape[0]
    posl = [np.ascontiguousarray(pos[b].reshape(NT, 128).T) for b in range(B)]
    groups = [list(range(DEPTH))] if FUSED else [[l] for l in range(DEPTH)]
    cur = [np.ascontiguousarray(x[b]) for b in range(B)]
    for L in groups:
        nc = _prog(len(L))
        shared = make_in_maps(inputs, L)
        in_maps = []
        for b in range(B):
            m = dict(shared)
            m["x"] = cur[b]
            m["pos"] = posl[b]
            in_maps.append(m)
        res = run_bass_kernel_spmd(nc, in_maps, core_ids=list(range(B)))
        cur = [np.ascontiguousarray(np.asarray(r["y"], dtype=np.float32)) for r in res.results]
    return np.stack(cur).astype(np.float32)
```

```python
import math
import numpy as np
import concourse.bass as bass
import concourse.mybir as mybir
from concourse.bass_utils import run_bass_kernel_spmd

F32 = mybir.dt.float32
BF16 = mybir.dt.bfloat16
I32 = mybir.dt.int32
AF = mybir.ActivationFunctionType
ALU = mybir.AluOpType
AX = mybir.AxisListType

S = 2048
D = 1024
NT = 16
DEPTH = 4
ALPHA = (2 * DEPTH) ** 0.25
IN_W = 2488
PI = math.pi
NEG = -30000.0

P_GQ, P_GKV, P_GB, P_MN, P_CW, P_CB, P_DTB, P_AL, P_SD, P_SN, P_RB, P_RW = (
    0, 2, 3, 19, 275, 305, 311, 319, 327, 331, 587, 607)
PW = 767
C_ID, C_U, C_L, C_MF, C_MB, C_ONE, C_IF = 0, 128, 256, 384, 512, 640, 768
CW = 784

WSLOT = 6144
NSLOT = 2
ARENA_BYTES = 74 * 1024


class Sem:
    def __init__(self, nc, name, dma=False):
        self.h = nc.alloc_semaphore(name)
        self.val = 0
        self.dma = dma
        self.name = name


class Buf:
    __slots__ = ("name", "w", "r", "excl")

    def __init__(self, name, excl=False):
        self.name = name
        self.w = None
        self.r = {}
        self.excl = excl


class K:
    def __init__(self, nc):
        self.nc = nc
        self.eng = {}
        for n, a in (("pe", "tensor"), ("dve", "vector"), ("act", "scalar"),
                     ("pool", "gpsimd"), ("sp", "sync")):
            self.eng[n] = (getattr(nc, a), Sem(nc, "e_" + n))
        self.known = {n: {} for n in self.eng}
        self.dsems = []
        self.ninst = 0

    def dsem(self, name):
        s = Sem(self.nc, name, dma=True)
        self.dsems.append(s)
        return s

    def _wait(self, e, tickets):
        h, own = self.eng[e]
        kn = self.known[e]
        need = {}
        for (s, v) in tickets:
            if s.dma:
                v = s.val
            if v > kn.get(s, 0) and v > need.get(s, 0):
                need[s] = v
        for s, v in need.items():
            assert s.val >= v, "wait on unsignalled ticket %s %d>%d (eng %s)" % (s.name, v, s.val, e)
            h.wait_ge(s.h, v)
            kn[s] = v
            self.ninst += 1

    def _tickets(self, reads, writes):
        tk = []
        for b in reads:
            if b.w is not None:
                tk.append(b.w)
        for b in writes:
            if b.w is not None:
                tk.append(b.w)
            for s, v in b.r.items():
                tk.append((s, v))
        return tk

    def _commit(self, t, reads, writes):
        for b in writes:
            b.w = t
            b.r = {}
        for b in reads:
            if b not in writes:
                if t[1] > b.r.get(t[0], 0):
                    b.r[t[0]] = t[1]

    def op(self, e, fn, reads=(), writes=(), signal=True, keep_self=False):
        h, sem = self.eng[e]
        ex = [b for b in reads if b.excl and b not in writes]
        if ex:
            writes = list(writes) + ex
            reads = [b for b in reads if not b.excl]
        tk = self._tickets(reads, writes)
        if e == "pe" and not keep_self:
            tk = [(s, v) for (s, v) in tk if s is not sem]
        self._wait(e, tk)
        inst = fn(h)
        self.ninst += 1
        if signal:
            sem.val += 1
            inst.then_inc(sem.h, 1)
            t = (sem, sem.val)
        else:
            t = (sem, sem.val + 1)
        self._commit(t, reads, writes)
        return inst

    def dma(self, q, out, in_, dsem, reads=(), writes=(), **kw):
        h, _ = self.eng[q]
        self._wait(q, self._tickets(reads, writes))
        inst = h.dma_start(out=out, in_=in_, **kw)
        self.ninst += 1
        dsem.val += 16
        inst.then_inc(dsem.h, 16)
        self._commit((dsem, dsem.val), reads, writes)

    def barrier(self):
        allt = [(s, s.val) for (_, s) in self.eng.values() if s.val > 0]
        allt += [(s, s.val) for s in self.dsems if s.val > 0]
        for e in self.eng:
            self._wait(e, allt)


class Rot:
    def __init__(self, items):
        self.items = list(items)
        self.i = 0

    def __call__(self):
        x = self.items[self.i % len(self.items)]
        self.i += 1
        return x


class Arena:
    def __init__(self, nc, nbytes):
        self.t = nc.alloc_sbuf_tensor("arena", [128, nbytes // 2], BF16)
        self.nbytes = nbytes
        self.off = 0
        self.peak = 0

    def reset(self):
        self.off = 0

    def alloc(self, shape, dtype, name="a"):
        esz = 4 if dtype in (F32, I32) else 2
        n = 1
        for s in shape:
            n *= s
        nb = (n * esz + 63) // 64 * 64
        assert self.off + nb <= self.nbytes, "arena overflow %s need %d have %d" % (
            name, nb, self.nbytes - self.off)
        ap = self.t[:, self.off // 2:(self.off + n * esz) // 2]
        if esz == 4:
            ap = ap.bitcast(dtype)
        if len(shape) == 2:
            ap = ap.rearrange("p (a b) -> p a b", a=shape[0])
        elif len(shape) == 3:
            ap = ap.rearrange("p (a b c) -> p a b c", a=shape[0], b=shape[1])
        elif len(shape) == 4:
            ap = ap.rearrange("p (a b c d) -> p a b c d", a=shape[0], b=shape[1], c=shape[2])
        self.off += nb
        self.peak = max(self.peak, self.off)
        return ap


def bc(ap, shape):
    return ap.to_broadcast(list(shape))


def build(NL, taps=(), stop_after=None):
    nc = bass.Bass("TRN2", target_bir_lowering=False)
    k = K(nc)

    def din(name, shape, dt=F32):
        return nc.dram_tensor(name, list(shape), dt, kind="ExternalInput").ap()

    dx = din("x", [S, D])
    dpos = din("pos", [128, NT], I32)
    dcst = din("cst", [128, CW])
    dprm = din("prm", [NL, 128, PW])
    w_in = din("w_in", [NL, D, IN_W])
    w_uq = din("w_uq", [NL, 256, 768])
    w_ukv = din("w_ukv", [NL, 128, 1024])
    w_out = din("w_out", [NL, D, D])
    dlnp = din("lnp", [NL, 4, D])
    d_wg = din("wg", [NL, 16, D, 256])
    d_wu = din("wu", [NL, 16, D, 256])
    d_wd = din("wd", [NL, 16, 256, D])
    dy = nc.dram_tensor("y", [S, D], F32, kind="ExternalOutput").ap()
    tap_out = {}

    X = nc.alloc_sbuf_tensor("X", [128, NT, D], F32)
    Xb = [Buf("X%d" % i) for i in range(NT)]
    XT = nc.alloc_sbuf_tensor("XT", [128, 8, S], BF16)
    XTb = [Buf("XT%d" % g) for g in range(4)]
    WS = [nc.alloc_sbuf_tensor("ws%d" % i, [128, WSLOT], BF16) for i in range(NSLOT)]
    WSb = [Buf("ws%d" % i) for i in range(NSLOT)]
    WSs = [k.dsem("ws%d" % i) for i in range(NSLOT)]
    CF = nc.alloc_sbuf_tensor("cf", [128, CW], F32)
    CFb = Buf("cf")
    CB = nc.alloc_sbuf_tensor("cb", [128, CW], BF16)
    CBb = Buf("cb")
    PRM = nc.alloc_sbuf_tensor("prm_s", [128, PW], F32)
    PRMb = Buf("prm")
    SCT = nc.alloc_sbuf_tensor("sc", [128, 1024], F32)
    sc_off = [0]

    def sc_reset():
        sc_off[0] = 0

    def sc_alloc(n):
        o = sc_off[0]
        assert o + n <= 1024, "scalar pool overflow"
        sc_off[0] = o + n
        return SCT[:, o:o + n]
    COS = nc.alloc_sbuf_tensor("cos", [128, NT, 16], F32)
    SIN = nc.alloc_sbuf_tensor("sin", [128, NT, 16], F32)
    CSb = Buf("cossin")
    arena = Arena(nc, ARENA_BYTES)
    banks = [nc.alloc_psum_tensor("pb%d" % i, [128, 512], F32) for i in range(8)]
    bankb = [Buf("pb%d" % i, excl=True) for i in range(8)]

    def pool(idx):
        return Rot([(banks[i], bankb[i]) for i in idx])

    s_c = k.dsem("cst")
    s_x = k.dsem("xld")
    s_p = k.dsem("prm")
    s_l = k.dsem("lnp")
    s_y = k.dsem("yst")
    s_t = k.dsem("tap")

    wd_sems = [k.dsem("wd%d" % i) for i in range(8)]
    identF = CF[:, C_ID:C_ID + 128]
    identB = CB[:, C_ID:C_ID + 128]
    slot_ctr = [0]

    def wslot():
        i = slot_ctr[0] % NSLOT
        slot_ctr[0] += 1
        return WS[i], WSb[i], WSs[i]

    def tap(name, ap, reads):
        if name not in taps:
            return
        shp = list(ap.shape)
        n = 1
        for s_ in shp[1:]:
            n *= s_
        cnt = sum(1 for t_ in tap_out if t_.startswith(name))
        nm = name if cnt == 0 else "%s_%d" % (name, cnt)
        dt_ = nc.dram_tensor("tap_" + nm, shp, ap.dtype, kind="ExternalOutput").ap()
        tap_out[nm] = shp
        k.dma("sp", dt_, ap, s_t, reads=reads)

    def mm(out, lhsT, rhs, start, stop, reads, writes, signal, serial=False):
        k.op("pe", lambda h: h.matmul(out, lhsT, rhs, start=start, stop=stop),
             reads=reads, writes=writes, signal=(signal or serial), keep_self=serial)

    def act(out, in_, func, reads, writes, **kw):
        k.op("act", lambda h: h.activation(out=out, in_=in_, func=func, **kw), reads=reads, writes=writes)

    def tt(out, in0, in1, op, reads, writes, e="dve"):
        k.op(e, lambda h: h.tensor_tensor(out=out, in0=in0, in1=in1, op=op), reads=reads, writes=writes)

    def ts(out, in0, s1, s2, op0, op1, reads, writes, e="dve"):
        if s2 is None:
            k.op(e, lambda h: h.tensor_scalar(out=out, in0=in0, scalar1=s1, scalar2=None, op0=op0),
                 reads=reads, writes=writes)
        else:
            k.op(e, lambda h: h.tensor_scalar(out=out, in0=in0, scalar1=s1, scalar2=s2, op0=op0, op1=op1),
                 reads=reads, writes=writes)

    def stt(out, in0, scalar, in1, op0, op1, reads, writes):
        k.op("dve", lambda h: h.scalar_tensor_tensor(out=out, in0=in0, scalar=scalar, in1=in1, op0=op0, op1=op1),
             reads=reads, writes=writes)

    def cp(out, in_, reads, writes, e="dve"):
        k.op(e, lambda h: h.tensor_copy(out=out, in_=in_), reads=reads, writes=writes)

    def red(out, in_, op, reads, writes):
        k.op("dve", lambda h: h.tensor_reduce(out=out, in_=in_, axis=AX.X, op=op), reads=reads, writes=writes)

    def recip(out, in_, reads, writes):
        k.op("dve", lambda h: h.reciprocal(out=out, in_=in_), reads=reads, writes=writes)

    def memset(ap, val, writes, e="dve"):
        k.op(e, lambda h: h.memset(ap, val), writes=writes)

    def tr(out, in_, ident, reads, writes, signal):
        k.op("pe", lambda h: h.transpose(out, in_, ident), reads=reads, writes=writes, signal=signal)

    k.dma("sp", CF[:, :], dcst[:, :], s_c, writes=[CFb])
    s_cb = k.dsem("cstb")
    k.dma("pool", CB[:, :], dcst[:, :], s_cb, writes=[CBb])
    dxv = dx.rearrange("(i p) d -> p i d", p=128)
    for q in range(4):
        k.dma("sp", X[:, 4 * q:4 * q + 4, :], dxv[:, 4 * q:4 * q + 4, :], s_x, writes=Xb[4 * q:4 * q + 4])

    arena.reset()
    tb = Buf("setup_tmp")
    posi = arena.alloc([NT, 1], I32, "posi")
    posf = arena.alloc([NT, 1], F32, "posf")
    ang = arena.alloc([NT, 16], F32, "ang")
    kf = arena.alloc([NT, 16], F32, "kf")
    ki = arena.alloc([NT, 16], I32, "ki")
    mt_ = arena.alloc([NT, 16], F32, "mt")
    k.dma("sp", posi[:, :, 0], dpos[:, :], s_c, writes=[tb])
    cp(posf, posi, [tb], [tb])
    invf = CF[:, C_IF:C_IF + 16].rearrange("p (a b) -> p a b", a=1)
    tt(ang, bc(posf, [128, NT, 16]), bc(invf, [128, NT, 16]), ALU.mult, [tb, CFb], [tb])
    ts(kf, ang, 1.0 / (2 * PI), None, ALU.mult, None, [tb], [tb])
    cp(ki, kf, [tb], [tb])
    cp(kf, ki, [tb], [tb])
    stt(ang, kf, -2 * PI, ang, ALU.mult, ALU.add, [tb], [tb])

    def wrap(r):
        ts(mt_, r, PI, -2 * PI, ALU.is_gt, ALU.mult, [tb], [tb])
        tt(r, r, mt_, ALU.add, [tb], [tb])
        ts(mt_, r, -PI, 2 * PI, ALU.is_lt, ALU.mult, [tb], [tb])
        tt(r, r, mt_, ALU.add, [tb], [tb])

    wrap(ang)
    act(SIN[:, :, :], ang, AF.Sin, [tb], [CSb])
    ts(ang, ang, PI / 2, None, ALU.add, None, [tb, CSb], [tb])
    wrap(ang)
    act(COS[:, :, :], ang, AF.Sin, [tb], [CSb])

    PT4 = pool([6, 7])

    def transpose_x_tile(i, src_tile_ap, src_reads, dst32=None, dst32b=None):
        for c0 in (0, 4):
            bk, bb = PT4()
            for j in range(4):
                c = c0 + j
                tr(bk[:, j * 128:(j + 1) * 128], src_tile_ap[:, c * 128:(c + 1) * 128], identF,
                   src_reads + [CFb], [bb], j == 3)
            bv = bk[:, :].rearrange("p (a b) -> p a b", a=4)
            act(XT[:, c0:c0 + 4, i * 128:(i + 1) * 128], bv, AF.Copy, [bb], [XTb[i // 4]])
            if dst32 is not None:
                cp(dst32[:, c0:c0 + 4, :], bv, [bb], [dst32b])

    for i in range(NT):
        transpose_x_tile(i, X[:, i, :], [Xb[i]])
    k.barrier()

    st = dict(nc=nc, k=k, arena=arena, X=X, Xb=Xb, XT=XT, XTb=XTb, CF=CF, CFb=CFb, CB=CB, CBb=CBb,
              PRM=PRM, PRMb=PRMb, sc_alloc=sc_alloc, sc_reset=sc_reset, COS=COS, SIN=SIN, CSb=CSb, pool=pool,
              wslot=wslot, tap=tap, mm=mm, act=act, tt=tt, ts=ts, stt=stt, cp=cp, red=red, recip=recip,
              memset=memset, tr=tr, identF=identF, identB=identB, w_in=w_in, w_uq=w_uq, w_ukv=w_ukv,
              w_out=w_out, dlnp=dlnp, d_wg=d_wg, d_wu=d_wu, d_wd=d_wd, dprm=dprm, s_p=s_p, s_l=s_l,
              transpose_x_tile=transpose_x_tile, stop_after=stop_after, wd_sems=wd_sems, pre={})

    st = NS(st)
    for l in range(NL):
        k.dma("sp", PRM[:, :], dprm[l], s_p, writes=[PRMb])
        if stop_after == "setup":
            break
        phase_mla(st, l)
        if stop_after is not None and stop_after.startswith("mla"):
            break
        phase_mlstm(st, l)
        if stop_after is not None and stop_after.startswith("mlstm"):
            break
        phase_ssd(st, l)
        if stop_after is not None and stop_after.startswith("ssd"):
            break
        phase_ln_router_moe(st, l, last=(l == NL - 1))

    k.barrier()
    dyv = dy.rearrange("(i p) d -> p i d", p=128)
    for q in range(4):
        k.dma("sp", dyv[:, 4 * q:4 * q + 4, :], X[:, 4 * q:4 * q + 4, :], s_y, reads=Xb[4 * q:4 * q + 4])
    k._wait("sp", [(s_y, s_y.val), (s_t, s_t.val)] if s_t.val else [(s_y, s_y.val)])
    return nc, tap_out, k


class NS:
    def __init__(self, d):
        self.__dict__.update(d)


def rope_tm(st, src, dst, tmpa, tmpb, sb, db):
    s = st
    t1, t2 = src[:, :, 0:16], src[:, :, 16:32]
    C, Sn = s.COS[:, :, :], s.SIN[:, :, :]
    tb_ = Buf("rope_tmp")
    s.tt(tmpa, t1, C, ALU.mult, [sb, s.CSb], [tb_])
    s.tt(tmpb, t2, Sn, ALU.mult, [sb, s.CSb, tb_], [tb_])
    s.tt(dst[:, :, 0:16], tmpa, tmpb, ALU.subtract, [tb_], [db])
    s.tt(tmpa, t2, C, ALU.mult, [sb, s.CSb, db], [tb_])
    s.tt(tmpb, t1, Sn, ALU.mult, [sb, s.CSb, tb_], [tb_])
    s.tt(dst[:, :, 16:32], tmpa, tmpb, ALU.add, [tb_], [db])


def outproj_load(st, l, nchunk, row0):
    s = st
    W, Wb, Wsm = s.wslot()
    Wv = W[:, 0:nchunk * 1024].rearrange("p (c n) -> p c n", c=nchunk)
    src = s.w_out[l, row0:row0 + nchunk * 128, :].rearrange("(c p) n -> p c n", p=128)
    s.k.dma("pool", Wv, src, Wsm, writes=[Wb])
    return Wv, Wb


def outproj_partial(st, l, YT, YTb, nchunk, row0, first, pre=None):
    s = st
    Wv, Wb = pre if pre is not None else outproj_load(s, l, nchunk, row0)
    PA = s.pool([0, 1, 2, 3])
    for i in range(NT):
        for hf in range(2):
            bk, bb = PA()
            for c in range(nchunk):
                s.mm(bk[:, :], YT[:, c, i * 128:(i + 1) * 128], Wv[:, c, hf * 512:(hf + 1) * 512],
                     c == 0, c == nchunk - 1, [YTb, Wb], [bb], c == nchunk - 1)
            xs = s.X[:, i, hf * 512:(hf + 1) * 512]
            if first:
                s.stt(xs, xs, ALPHA, bk[:, :], ALU.mult, ALU.add, [s.Xb[i], bb], [s.Xb[i]])
            else:
                s.tt(xs, xs, bk[:, :], ALU.add, [s.Xb[i], bb], [s.Xb[i]])


def preload_mla(s, l):
    k = s.k
    W0, W0b, W0s = s.wslot()
    W0v = W0[:, 0:8 * 416].rearrange("p (k c) -> p k c", k=8)
    k.dma("pool", W0v, s.w_in[l].rearrange("(k p) c -> p k c", p=128)[:, :, 0:416], W0s, writes=[W0b])
    W1, W1b, W1s = s.wslot()
    Wuq = W1[:, 0:1536].rearrange("p (c n) -> p c n", c=2)
    k.dma("pool", Wuq, s.w_uq[l].rearrange("(c p) n -> p c n", p=128), W1s, writes=[W1b])
    Wkv = W1[:, 1536:2560]
    k.dma("pool", Wkv, s.w_ukv[l], W1s, writes=[W1b])
    s.pre["mla"] = (W0v, W0b, Wuq, Wkv, W1b)


def preload_mlstm_a(s, l):
    Wa, Wab, Was = s.wslot()
    Wav = Wa[:, 0:8 * 512].rearrange("p (k c) -> p k c", k=8)
    wsrc = s.w_in[l].rearrange("(k p) c -> p k c", p=128)
    s.k.dma("pool", Wav, wsrc[:, :, 416:928], Was, writes=[Wab])
    s.pre["mlstm_a"] = (Wav, Wab)


def preload_ssd_x(s, l):
    Wx, Wxb, Wxs = s.wslot()
    Wxv = Wx[:, 0:6144].rearrange("p (k c) -> p k c", k=8)
    wsrc = s.w_in[l].rearrange("(k p) c -> p k c", p=128)
    s.k.dma("pool", Wxv, wsrc[:, :, 1712:2480], Wxs, writes=[Wxb])
    s.pre["ssd_x"] = (Wxv, Wxb)


def phase_mla(st, l):
    s = NS(st) if isinstance(st, dict) else st
    k, ar = s.k, s.arena
    ar.reset()
    PA = s.pool([0, 1, 2, 3])
    PB = s.pool([4, 5])
    PC = s.pool([6, 7])
    if "mla" not in s.pre:
        preload_mla(s, l)
    W0v, W0b, Wuq, Wkv, W1b = s.pre.pop("mla")
    for c in range(2):
        s.ts(Wuq[:, c, :], Wuq[:, c, :], s.PRM[:, P_GQ + c:P_GQ + c + 1], None, ALU.mult, None,
             [s.PRMb, W1b], [W1b])
    s.ts(Wkv, Wkv, s.PRM[:, P_GKV:P_GKV + 1], None, ALU.mult, None, [s.PRMb, W1b], [W1b])

    if s.stop_after == "mla_w":
        return
    cT = ar.alloc([3, S], BF16, "cT"); cTb = Buf("cT")
    YA = ar.alloc([4, S], BF16, "YA"); YAb = Buf("YA")
    krr = ar.alloc([NT, 32], F32, "krr"); krrb = Buf("krr")
    krb = ar.alloc([NT, 32], BF16, "krb"); krbb = Buf("krb")
    sq = ar.alloc([384], F32, "sq"); sqb = Buf("sq")
    s.sc_reset()
    ssq = s.sc_alloc(NT); sskv = s.sc_alloc(NT); ssb = Buf("ss")
    rq = s.sc_alloc(NT); rkv = s.sc_alloc(NT); rb_ = Buf("r")
    ta = ar.alloc([NT, 16], F32, "ta"); tb2 = ar.alloc([NT, 16], F32, "tb")
    qhb = ar.alloc([NT, 96], BF16, "qhb"); qhbb = Buf("qhb")
    qr = ar.alloc([NT, 32], F32, "qr"); qrb = Buf("qr")
    khb = ar.alloc([NT, 96], BF16, "khb"); khbb = Buf("khb")
    VA = [ar.alloc([NT, 65], BF16, "VA%d" % i) for i in range(2)]; VAb = [Buf("VA0"), Buf("VA1")]
    QT = [ar.alloc([S], BF16, "QT%d" % i) for i in range(2)]; QTb = [Buf("QT0"), Buf("QT1")]
    KT = [ar.alloc([S], BF16, "KT%d" % i) for i in range(2)]; KTb = [Buf("KT0"), Buf("KT1")]
    PTs = [(ar.alloc([512], BF16, "PT%d" % i), Buf("PT%d" % i)) for i in range(5)]
    PTr = Rot(PTs)
    OSs = [(ar.alloc([512], F32, "OS%d" % i), Buf("OS%d" % i)) for i in range(2)]
    OSr = Rot(OSs)

    for i in range(NT):
        bk, bb = PA()
        for kk in range(8):
            s.mm(bk[:, 0:416], s.XT[:, kk, i * 128:(i + 1) * 128], W0v[:, kk, :], kk == 0, kk == 7,
                 [s.XTb[i // 4], W0b], [bb], kk == 7)
        s.act(sq[:, 0:384], bk[:, 0:384], AF.Square, [bb], [sqb])
        s.red(ssq[:, i:i + 1], sq[:, 0:256], ALU.add, [sqb], [ssb])
        s.red(sskv[:, i:i + 1], sq[:, 256:384], ALU.add, [sqb], [ssb])
        s.cp(krr[:, i, :], bk[:, 384:416], [bb], [krrb])
    if s.stop_after == "mla_tm":
        return
    for j in range(3):
        for g in range(4):
            bk, bb = PA()
            for kk in range(8):
                s.mm(bk[:, :], W0v[:, kk, j * 128:(j + 1) * 128], s.XT[:, kk, g * 512:(g + 1) * 512],
                     kk == 0, kk == 7, [s.XTb[g], W0b], [bb], kk == 7)
            s.act(cT[:, j, g * 512:(g + 1) * 512], bk[:, :], AF.Copy, [bb], [cTb])
    if s.stop_after == "mla_fm":
        return
    s.act(rq, ssq, AF.Sqrt, [ssb], [rb_], scale=96.0 / 256.0, bias=96e-6)
    s.act(rkv, sskv, AF.Sqrt, [ssb], [rb_], scale=1.0 / 128.0, bias=1e-6)
    s.recip(rq, rq, [rb_], [rb_])
    s.recip(rkv, rkv, [rb_], [rb_])
    rope_tm(s, krr, krb, ta, tb2, krrb, krbb)
    for v_ in range(2):
        s.memset(VA[v_][:, :, 64:65], 1.0, [VAb[v_]])

    if s.stop_after == "mla_r":
        return

    def prep_chunks(h):
        p = h % 2
        ch = []

        def q_group(i4):
            bk, bb = PC()
            for j in range(4):
                i = i4 * 4 + j
                for c in range(2):
                    s.mm(bk[:, j * 96:(j + 1) * 96], cT[:, c, i * 128:(i + 1) * 128],
                         Wuq[:, c, h * 96:(h + 1) * 96], c == 0, c == 1, [cTb, W1b], [bb],
                         j == 3 and c == 1)
            bkv = bk[:, 0:384].rearrange("p (a b) -> p a b", a=4)
            rq4 = rq[:, i4 * 4:(i4 + 1) * 4].rearrange("p (a b) -> p a b", b=1)
            s.tt(qhb[:, i4 * 4:(i4 + 1) * 4, 0:64], bkv[:, :, 0:64], bc(rq4, [128, 4, 64]), ALU.mult,
                 [bb, rb_], [qhbb])
            s.tt(qr[:, i4 * 4:(i4 + 1) * 4, :], bkv[:, :, 64:96], bc(rq4, [128, 4, 32]), ALU.mult,
                 [bb, rb_], [qrb])

        def kv_group(i4):
            bk, bb = PC()
            for j in range(4):
                i = i4 * 4 + j
                s.mm(bk[:, j * 128:(j + 1) * 128], cT[:, 2, i * 128:(i + 1) * 128],
                     Wkv[:, h * 128:(h + 1) * 128], True, True, [cTb, W1b], [bb], j == 3)
            bkv = bk[:, :].rearrange("p (a b) -> p a b", a=4)
            r4 = rkv[:, i4 * 4:(i4 + 1) * 4].rearrange("p (a b) -> p a b", b=1)
            s.tt(khb[:, i4 * 4:(i4 + 1) * 4, 0:64], bkv[:, :, 0:64], bc(r4, [128, 4, 64]), ALU.mult,
                 [bb, rb_], [khbb])
            s.tt(VA[p][:, i4 * 4:(i4 + 1) * 4, 0:64], bkv[:, :, 64:128], bc(r4, [128, 4, 64]), ALU.mult,
                 [bb, rb_], [VAb[p]])

        def tr_group(src, srcb, dst, dstb, i4):
            bk, bb = PC()
            pb = bk[:, 0:256].bitcast(BF16)
            for j in range(4):
                i = i4 * 4 + j
                s.tr(pb[0:96, j * 128:(j + 1) * 128], src[:, i, :], s.identB, [srcb, s.CBb], [bb], j == 3)
            s.cp(dst[0:96, i4 * 512:(i4 + 1) * 512], pb[0:96, :], [bb], [dstb])

        for i4 in range(4):
            ch.append(lambda i4=i4: q_group(i4))
        ch.append(lambda: rope_tm(s, qr, qhb[:, :, 64:96], ta, tb2, qrb, qhbb))
        for i4 in range(4):
            ch.append(lambda i4=i4: kv_group(i4))
        ch.append(lambda: s.cp(khb[:, :, 64:96], krb, [krbb], [khbb]))
        for i4 in range(4):
            ch.append(lambda i4=i4: tr_group(qhb, qhbb, QT[p], QTb[p], i4))
        for i4 in range(4):
            ch.append(lambda i4=i4: tr_group(khb, khbb, KT[p], KTb[p], i4))
        return ch

    LOOK = 3
    pend = []

    fin_q = []

    def fin_tick(flush=False):
        for it in fin_q:
            it[0] -= 1
        while fin_q and (flush or fin_q[0][0] <= 0):
            fin_q.pop(0)[1]()

    def attn_finish(h, qc, ob, obb):
        os_, osb = OSr()
        s.cp(os_[0:65, :], ob[0:65, :], [obb], [osb])
        s.act(os_[64:65, :], os_[64:65, :], AF.Ln, [osb], [osb])
        s.act(os_[64:65, :], os_[64:65, :], AF.Exp, [osb], [osb], scale=-1.0)
        fin_q.append([5, lambda: attn_finish2(h, qc, os_, osb)])

    def attn_finish2(h, qc, os_, osb):
        rbk, rbb = PC()
        s.mm(rbk[0:64, :], s.CF[64:65, C_ONE:C_ONE + 64], os_[64:65, :], True, True,
             [osb, s.CFb], [rbb], True)
        r0 = (h % 2) * 64
        s.tt(YA[r0:r0 + 64, h // 2, qc * 512:(qc + 1) * 512], os_[0:64, :], rbk[0:64, :], ALU.mult,
             [osb, rbb], [YAb])

    def pv_step(item):
        (h, qc, kt, pt, ptb, ob, obb) = item
        p = h % 2
        s.mm(ob[0:65, :], VA[p][:, kt, :], pt, kt == 0, kt == 15, [ptb, VAb[p]], [obb], kt == 15)
        if kt == 15:
            attn_finish(h, qc, ob, obb)

    def attn(h, chunks):
        p = h % 2
        n = 0
        for qc in range(4):
            ob, obb = PB()
            for kt in range(16):
                n += 1
                if chunks and n >= 5 and n % 2 == 0:
                    chunks.pop(0)()
                sb_, sbb = PA()
                s.mm(sb_[:, :], KT[p][0:96, kt * 128:(kt + 1) * 128], QT[p][0:96, qc * 512:(qc + 1) * 512],
                     True, True, [KTb[p], QTb[p]], [sbb], True)
                pt, ptb = PTr()
                s.act(pt, sb_[:, :], AF.Exp, [sbb], [ptb])
                pend.append((h, qc, kt, pt, ptb, ob, obb))
                if len(pend) > LOOK:
                    pv_step(pend.pop(0))
                fin_tick()

    for c_ in prep_chunks(0):
        c_()
    wo_pre = outproj_load(s, l, 4, 0)
    for h in range(8):
        chunks = prep_chunks(h + 1) if h + 1 < 8 else []
        attn(h, chunks)
        while chunks:
            chunks.pop(0)()
    while pend:
        pv_step(pend.pop(0))
    fin_tick(flush=True)
    preload_mlstm_a(s, l)
    s.tap("ya", YA, [YAb])
    outproj_partial(s, l, YA, YAb, 4, 0, True, pre=wo_pre)


def token_decay_arrays(s, a_all, u_all, ab):
    ar = s.arena
    cs = ar.alloc([NT, 2, 4], F32, "cs")
    nb = ar.alloc([NT, 2, 4], F32, "nb")
    ecs = ar.alloc([NT, 2, 4], F32, "ecs")
    ws = ar.alloc([NT, 2, 4], F32, "ws")
    gst = ar.alloc([NT, 2, 4], F32, "gst")
    P1 = s.pool([0, 1])
    U = s.CF[:, C_U:C_U + 128]
    L = s.CF[:, C_L:C_L + 128]
    ones = s.CF[:, C_ONE:C_ONE + 128]
    bk, bb = P1()
    for d, M in ((0, U), (1, L)):
        s.mm(bk[:, d * 64:(d + 1) * 64], M, a_all[:, :, d, :], True, True, [ab, s.CFb], [bb], d == 1)
    for d in range(2):
        s.cp(cs[:, :, d, :], bk[:, d * 64:(d + 1) * 64].rearrange("p (a b) -> p a b", a=NT), [bb], [ab])
    bk2, bb2 = P1()
    s.mm(bk2[:, 0:128], ones, a_all.rearrange("p a b c -> p (a b c)"), True, True, [ab, s.CFb], [bb2], True)
    tot = bk2[:, 0:128].rearrange("p (a b c) -> p a b c", a=NT, b=2)
    s.act(gst, tot, AF.Exp, [bb2], [ab])
    s.tt(nb, u_all, cs, ALU.subtract, [ab], [ab])
    s.tt(ws, tot, nb, ALU.add, [bb2, ab], [ab])
    s.act(ws, ws, AF.Exp, [ab], [ab])
    s.act(ecs, cs, AF.Exp, [ab], [ab])
    return dict(cs=cs, nb=nb, ecs=ecs, ws=ws, gst=gst)


def decay_E(s, i, a_all, nb, ab, Ebuf, Ebb, banks2):
    U = s.CF[:, C_U:C_U + 128]
    L = s.CF[:, C_L:C_L + 128]
    for d in range(2):
        bk, bb = banks2[d]
        M = U if d == 0 else L
        mk = s.CB[:, C_MF:C_MF + 128] if d == 0 else s.CB[:, C_MB:C_MB + 128]
        for h in range(4):
            s.mm(bk[:, h * 128:(h + 1) * 128], bc(a_all[:, i, d, h:h + 1], [128, 128]), M, True, False,
                 [ab, s.CFb], [bb], False)
            s.mm(bk[:, h * 128:(h + 1) * 128], s.identB, mk, False, True, [s.CBb], [bb], h == 3)
        for h in range(4):
            s.act(Ebuf[:, d, h, :], bk[:, h * 128:(h + 1) * 128], AF.Exp, [bb, ab], [Ebb],
                  bias=nb[:, i, d, h:h + 1])


def phase_mlstm(st, l):
    s = NS(st) if isinstance(st, dict) else st
    k, ar = s.k, s.arena
    k.barrier()
    ar.reset()
    s.sc_reset()
    PA = s.pool([0, 1, 2, 3])
    wsrc = s.w_in[l].rearrange("(k p) c -> p k c", p=128)
    if "mlstm_a" not in s.pre:
        preload_mlstm_a(s, l)
    Wav, Wab = s.pre.pop("mlstm_a")
    Wb, Wbb, Wbs = s.wslot()
    Wbv = Wb[:, 0:8 * 528].rearrange("p (k c) -> p k c", k=8)
    k.dma("pool", Wbv, wsrc[:, :, 928:1456], Wbs, writes=[Wbb])
    mqT = ar.alloc([2, S], BF16, "mqT"); mkT = ar.alloc([2, S], BF16, "mkT"); fmb = Buf("mfm")
    mkTM = ar.alloc([NT, 4, 64], BF16, "mkTM"); mvA = ar.alloc([NT, 4, 65], BF16, "mvA"); tmb = Buf("mtm")
    YB = ar.alloc([2, S], BF16, "YB"); YBb = Buf("YB")
    gts = ar.alloc([NT, 2, 2, 4], F32, "gts")
    a_all = ar.alloc([NT, 2, 4], F32, "a_all"); u_all = ar.alloc([NT, 2, 4], F32, "u_all"); ab = Buf("mtok")
    Fst = ar.alloc([NT, 2, 2, 65], BF16, "F"); Fb = Buf("F")
    Sst = ar.alloc([2, 2, 65], F32, "S"); Sb = Buf("S")
    Kw = [ar.alloc([4, 64], BF16, "Kw%d" % i) for i in range(2)]; Kwb = [Buf("Kw0"), Buf("Kw1")]
    Es = [ar.alloc([2, 4, 128], BF16, "E%d" % i) for i in range(2)]; Ebs = [Buf("E0"), Buf("E1")]
    MTs = [ar.alloc([2, 4, 128], BF16, "MT%d" % i) for i in range(2)]; MTbs = [Buf("MT0"), Buf("MT1")]
    Rall = ar.alloc([2, 4, 65], F32, "Rall"); Rt = [Rall[:, 0, :, :], Rall[:, 1, :, :]]; Rb = Buf("R")
    prod = ar.alloc([2, 4, 64], F32, "prod")
    cen, sq_ = prod[:, 0, :, :], prod[:, 1, :, :]
    tmp = ar.alloc([4, 65], F32, "tmp")
    den = s.sc_alloc(8); hs = ar.alloc([4, 64], F32, "hs"); hb_ = Buf("hs")

    st4 = s.sc_alloc(4); st4b = s.sc_alloc(4)
    sgos = [ar.alloc([256], F32, "sgo%d" % i) for i in range(2)]; sgobs = [Buf("sgo0"), Buf("sgo1")]
    ybs = [ar.alloc([256], BF16, "yb%d" % i) for i in range(3)]; ybbs = [Buf("yb%d" % i) for i in range(3)]
    s.memset(mvA[:, :, :, 64:65], 1.0, [tmb])
    for i in range(NT):
        bk, bb = PA()
        for kk in range(8):
            s.mm(bk[:, 0:256], s.XT[:, kk, i * 128:(i + 1) * 128], Wav[:, kk, 256:512], kk == 0, kk == 7,
                 [s.XTb[i // 4], Wab], [bb], kk == 7)
        s.act(mkTM[:, i, :, :], bk[:, 0:256].rearrange("p (a b) -> p a b", a=4), AF.Copy, [bb], [tmb], scale=0.125)
        bk, bb = PA()
        for kk in range(8):
            s.mm(bk[:, 0:256], s.XT[:, kk, i * 128:(i + 1) * 128], Wbv[:, kk, 0:256], kk == 0, kk == 7,
                 [s.XTb[i // 4], Wbb], [bb], False)
        for kk in range(8):
            s.mm(bk[:, 256:272], s.XT[:, kk, i * 128:(i + 1) * 128], Wbv[:, kk, 512:528], kk == 0, kk == 7,
                 [s.XTb[i // 4], Wbb], [bb], kk == 7)
        s.act(mvA[:, i, :, 0:64], bk[:, 0:256].rearrange("p (a b) -> p a b", a=4), AF.Copy, [bb], [tmb])
        s.tt(gts[:, i, :, :, :].rearrange("p a b c -> p (a b c)"), bk[:, 256:272], s.PRM[:, P_GB:P_GB + 16],
             ALU.add, [bb, s.PRMb], [ab])
    for j in range(4):
        for g in range(4):
            bk, bb = PA()
            for kk in range(8):
                s.mm(bk[:, :], Wav[:, kk, j * 128:(j + 1) * 128], s.XT[:, kk, g * 512:(g + 1) * 512],
                     kk == 0, kk == 7, [s.XTb[g], Wab], [bb], kk == 7)
            if j < 2:
                s.act(mqT[:, j, g * 512:(g + 1) * 512], bk[:, :], AF.Copy, [bb], [fmb])
            else:
                s.act(mkT[:, j - 2, g * 512:(g + 1) * 512], bk[:, :], AF.Copy, [bb], [fmb], scale=0.125)
    wo_pre = outproj_load(s, l, 2, 512)
    s.cp(u_all, gts[:, :, :, 0, :], [ab], [ab])
    s.act(a_all, gts[:, :, :, 1, :], AF.Exp, [ab], [ab], scale=-1.0)
    s.act(a_all, a_all, AF.Ln, [ab], [ab], bias=1.0)
    s.ts(a_all, a_all, -1.0, None, ALU.mult, None, [ab], [ab])
    if s.stop_after == "mlstm_a":
        return
    T = token_decay_arrays(s, a_all, u_all, ab)
    cs, nb, ecs, ws, gst = T["cs"], T["nb"], T["ecs"], T["ws"], T["gst"]
    if s.stop_after == "mlstm_b":
        return
    s.memset(Sst, 0.0, [Sb])
    P23 = [s.pool([2, 4]), s.pool([3, 5])]
    for c in range(NT):
        for d in range(2):
            t_ = c if d == 0 else NT - 1 - c
            s.tt(Kw[d], mkTM[:, t_, :, :], bc(ws[:, t_, d, :].rearrange("p (a b) -> p a b", b=1), [128, 4, 64]),
                 ALU.mult, [tmb, ab], [Kwb[d]])
            bk, bb = P23[d]()
            for c2 in range(2):
                s.mm(bk[:, c2 * 130:(c2 + 1) * 130], Kw[d][:, 2 * c2:2 * c2 + 2, :].rearrange("p a b -> p (a b)"),
                     mvA[:, t_, 2 * c2:2 * c2 + 2, :].rearrange("p a b -> p (a b)"), True, True,
                     [Kwb[d], tmb], [bb], c2 == 1)
            s.act(Fst[:, t_, d, :, :], Sst[:, d, :, :], AF.Copy, [Sb], [Fb])
            bv = bk[:, 0:260].rearrange("p (a b) -> p a b", a=2)
            for hp in range(2):
                r0, r1 = hp * 64, hp * 64 + 64
                gsel = gst[r0:r1, t_, d, :].rearrange("p (a b) -> p a b", b=2)[:, :, hp:hp + 1]
                s.tt(Sst[r0:r1, d, :, :], Sst[r0:r1, d, :, :], bc(gsel, [64, 2, 65]), ALU.mult, [Sb, ab], [Sb])
                s.tt(Sst[r0:r1, d, :, :], Sst[r0:r1, d, :, :], bv[r0:r1, :, hp * 65:(hp + 1) * 65], ALU.add,
                     [Sb, bb], [Sb])
    if s.stop_after == "mlstm_c":
        return
    bE = [(s.pool([0])()), (s.pool([1])())]
    bS = s.pool([2])()
    bI = [s.pool([3])(), s.pool([4])()]
    bJ = [s.pool([5])(), s.pool([6])()]
    bT = s.pool([7])()
    def stage_a(i):
        sl = slice(i * 128, (i + 1) * 128)
        E, Eb, MT, MTb, sgo, sgob = Es[i % 2], Ebs[i % 2], MTs[i % 2], MTbs[i % 2], sgos[i % 2], sgobs[i % 2]
        decay_E(s, i, a_all, nb, ab, E, Eb, bE)
        for h in range(4):
            r0 = (h % 2) * 64
            s.mm(bS[0][:, h * 128:(h + 1) * 128], mkT[r0:r0 + 64, h // 2, sl], mqT[r0:r0 + 64, h // 2, sl],
                 True, True, [fmb], [bS[1]], True, serial=True)
        for d in range(2):
            s.tt(MT[:, d, :, :], bS[0][:, :].rearrange("p (a b) -> p a b", a=4), E[:, d, :, :], ALU.mult,
                 [bS[1], Eb], [MTb])
        bk, bb = bT
        for kk in range(8):
            s.mm(bk[:, 0:256], s.XT[:, kk, sl], Wbv[:, kk, 256:512], kk == 0, kk == 7,
                 [s.XTb[i // 4], Wbb], [bb], kk == 7)
        s.act(sgo, bk[:, 0:256], AF.Sigmoid, [bb], [sgob])

    def stage_b(i):
        sl = slice(i * 128, (i + 1) * 128)
        yb, ybb = ybs[i % 3], ybbs[i % 3]
        E, Eb, MT, MTb, sgo, sgob = Es[i % 2], Ebs[i % 2], MTs[i % 2], MTbs[i % 2], sgos[i % 2], sgobs[i % 2]
        for d in range(2):
            for h in range(4):
                r0 = (h % 2) * 64
                s.mm(bJ[d][0][:, h * 65:(h + 1) * 65], mqT[r0:r0 + 64, h // 2, sl], Fst[r0:r0 + 64, i, d, h // 2, :],
                     True, True, [fmb, Fb], [bJ[d][1]], True, serial=True)
            for h in range(4):
                s.mm(bI[d][0][:, h * 65:(h + 1) * 65], MT[:, d, h, :], mvA[:, i, h, :], True, True,
                     [MTb, tmb], [bI[d][1]], h == 3)
            s.tt(tmp, bJ[d][0][:, 0:260].rearrange("p (a b) -> p a b", a=4),
                 bc(ecs[:, i, d, :].rearrange("p (a b) -> p a b", b=1), [128, 4, 65]), ALU.mult,
                 [bJ[d][1], ab], [Rb])
            s.tt(Rt[d], bI[d][0][:, 0:260].rearrange("p (a b) -> p a b", a=4), tmp, ALU.add, [bI[d][1], Rb], [Rb])
        d8 = den.rearrange("p (a b) -> p a b", a=2)
        s.ts(d8, Rall[:, :, :, 64], -1.0, None, ALU.mult, None, [Rb], [Rb])
        s.tt(d8, d8, Rall[:, :, :, 64], ALU.max, [Rb], [Rb])
        s.ts(d8, d8, 1.0, None, ALU.max, None, [Rb], [Rb])
        s.recip(den, den, [Rb], [Rb])
        s.tt(prod, Rall[:, :, :, 0:64], bc(den.rearrange("p (a b c) -> p a b c", a=2, c=1), [128, 2, 4, 64]),
             ALU.mult, [Rb], [hb_])
        s.tt(hs, prod[:, 0, :, :], prod[:, 1, :, :], ALU.add, [hb_], [hb_])
        s.red(st4, hs, ALU.add, [hb_], [hb_])
        s.ts(st4, st4, 1.0 / 64.0, None, ALU.mult, None, [hb_], [hb_])
        s.tt(cen, hs, bc(st4.rearrange("p (a b) -> p a b", b=1), [128, 4, 64]), ALU.subtract, [hb_], [hb_])
        s.tt(sq_, cen, cen, ALU.mult, [hb_], [hb_])
        s.red(st4b, sq_, ALU.add, [hb_], [hb_])
        s.act(st4b, st4b, AF.Ln, [hb_], [hb_], scale=1.0 / 64.0, bias=1e-5)
        s.act(st4b, st4b, AF.Exp, [hb_], [hb_], scale=-0.5)
        s.tt(cen, cen, bc(st4b.rearrange("p (a b) -> p a b", b=1), [128, 4, 64]), ALU.mult, [hb_], [hb_])
        cf = cen.rearrange("p a b -> p (a b)")
        s.tt(cf, cf, s.PRM[:, P_MN:P_MN + 256], ALU.mult, [hb_, s.PRMb], [hb_])
        s.tt(yb, cf, sgo, ALU.mult, [hb_, sgob], [ybb])

    def stage_c(i):
        sl = slice(i * 128, (i + 1) * 128)
        yb, ybb = ybs[i % 3], ybbs[i % 3]
        bk, bb = bT
        pb = bk[:, 0:256].bitcast(BF16)
        for c in range(2):
            s.tr(pb[:, c * 128:(c + 1) * 128], yb[:, c * 128:(c + 1) * 128], s.identB, [ybb, s.CBb], [bb], c == 1)
        s.act(YB[:, :, sl], pb[:, 0:256].rearrange("p (a b) -> p a b", a=2), AF.Copy, [bb], [YBb])

    stage_a(0)
    for i in range(NT + 1):
        if i + 1 < NT:
            stage_a(i + 1)
        if i < NT:
            stage_b(i)
        if i >= 1:
            stage_c(i - 1)
    s.tap("yb", YB, [YBb])
    preload_ssd_x(s, l)
    outproj_partial(s, l, YB, YBb, 2, 512, False, pre=wo_pre)


def phase_ssd(st, l):
    s = NS(st) if isinstance(st, dict) else st
    k, ar = s.k, s.arena
    k.barrier()
    ar.reset()
    s.sc_reset()
    PA = s.pool([0, 1, 2, 3])
    PT = s.pool([6, 7])
    wsrc = s.w_in[l].rearrange("(k p) c -> p k c", p=128)
    if "ssd_x" not in s.pre:
        preload_ssd_x(s, l)
    Wxv, Wxb = s.pre.pop("ssd_x")
    Wz, Wzb, Wzs = s.wslot()
    Wzv = Wz[:, 0:8 * 264].rearrange("p (k c) -> p k c", k=8)
    k.dma("pool", Wzv[:, :, 0:256], wsrc[:, :, 1456:1712], Wzs, writes=[Wzb])
    k.dma("pool", Wzv[:, :, 256:264], wsrc[:, :, 2480:2488], Wzs, writes=[Wzb])
    BCT = ar.alloc([4, S], BF16, "BCT"); bcb = Buf("BCT")
    BTM = ar.alloc([NT, 2, 128], BF16, "BTM"); btb = Buf("BTM")
    xTM = ar.alloc([NT, 4, 64], BF16, "xTM"); tmb = Buf("stm")
    YC = BTM.rearrange("p a b c -> p (a b c)").rearrange("p (a b) -> p a b", a=2); YCb = btb
    dtr = ar.alloc([NT, 2, 4], F32, "dtr")
    a_all = ar.alloc([NT, 2, 4], F32, "a_all"); u_all = ar.alloc([NT, 2, 4], F32, "u_all"); ab = Buf("stok")
    aneg = s.sc_alloc(8)
    T = None
    mark = ar.off
    bk, bb = PA()
    for i in range(NT):
        for kk in range(8):
            s.mm(bk[:, i * 8:(i + 1) * 8], s.XT[:, kk, i * 128:(i + 1) * 128], Wzv[:, kk, 256:264], kk == 0, kk == 7,
                 [s.XTb[i // 4], Wzb], [bb], i == NT - 1 and kk == 7)
    dflat = dtr.rearrange("p a b c -> p a (b c)")
    s.tt(dflat, bk[:, 0:128].rearrange("p (a b) -> p a b", a=NT),
         bc(s.PRM[:, P_DTB:P_DTB + 8].rearrange("p (a b) -> p a b", a=1), [128, NT, 8]), ALU.add,
         [bb, s.PRMb], [ab])
    s.act(dtr, dtr, AF.Exp, [ab], [ab])
    s.act(dtr, dtr, AF.Ln, [ab], [ab], bias=1.0)
    s.act(u_all, dtr, AF.Ln, [ab], [ab])
    s.act(aneg, s.PRM[:, P_AL:P_AL + 8], AF.Exp, [s.PRMb], [ab])
    s.ts(aneg, aneg, -1.0, None, ALU.mult, None, [ab], [ab])
    s.tt(a_all.rearrange("p a b c -> p a (b c)"), dflat,
         bc(aneg.rearrange("p (a b) -> p a b", a=1), [128, NT, 8]), ALU.mult, [ab], [ab])
    cin = [ar.alloc([2052], BF16, "cin%d" % i) for i in range(2)]; cinb = [Buf("cin0"), Buf("cin1")]
    acc = [ar.alloc([512], F32, "acc%d" % i) for i in range(2)]; accb = [Buf("acc0"), Buf("acc1")]
    xfm = [ar.alloc([S], BF16, "xfm%d" % i) for i in range(2)]; xfmb = [Buf("xfm0"), Buf("xfm1")]
    for p_ in range(2):
        s.memset(cin[p_][:, 0:2], 0.0, [cinb[p_]])
        s.memset(cin[p_][:, 2050:2052], 0.0, [cinb[p_]])
    accr = Rot([0, 1])
    for j in range(6):
        p_ = j % 2
        for g in range(4):
            bk, bb = PA()
            for kk in range(8):
                s.mm(bk[:, :], Wxv[:, kk, j * 128:(j + 1) * 128], s.XT[:, kk, g * 512:(g + 1) * 512],
                     kk == 0, kk == 7, [s.XTb[g], Wxb], [bb], kk == 7)
            s.act(cin[p_][:, 2 + g * 512:2 + (g + 1) * 512], bk[:, :], AF.Copy, [bb], [cinb[p_]])
        if j < 2:
            dst, dstb = xfm[p_], xfmb[p_]
        else:
            dst, dstb = BCT[:, j - 2, :], bcb
        for g in range(4):
            a_ = accr()
            cw = lambda jj: s.PRM[:, P_CW + j * 5 + jj:P_CW + j * 5 + jj + 1]
            s.ts(acc[a_], cin[p_][:, g * 512:g * 512 + 512], cw(0), None, ALU.mult, None,
                 [cinb[p_], s.PRMb], [accb[a_]])
            for jj in range(1, 5):
                s.stt(acc[a_], cin[p_][:, g * 512 + jj:g * 512 + jj + 512], cw(jj), acc[a_], ALU.mult, ALU.add,
                      [cinb[p_], s.PRMb, accb[a_]], [accb[a_]])
            s.act(dst[:, g * 512:(g + 1) * 512], acc[a_], AF.Silu, [accb[a_], s.PRMb], [dstb],
                  bias=s.PRM[:, P_CB + j:P_CB + j + 1])
        if j < 4:
            src = xfm[p_] if j < 2 else BCT[:, j - 2, :]
            srcb = xfmb[p_] if j < 2 else bcb
            for i4 in range(4):
                bk, bb = PT()
                pb = bk[:, 0:256].bitcast(BF16)
                for q in range(4):
                    i = i4 * 4 + q
                    s.tr(pb[:, q * 128:(q + 1) * 128], src[:, i * 128:(i + 1) * 128], s.identB, [srcb, s.CBb], [bb], q == 3)
                if j < 2:
                    s.act(xTM[:, i4 * 4:(i4 + 1) * 4, 2 * j:2 * j + 2, :],
                          pb[:, :].rearrange("p (a b c) -> p a b c", a=4, b=2), AF.Copy, [bb], [tmb])
                else:
                    s.act(BTM[:, i4 * 4:(i4 + 1) * 4, j - 2, :], pb[:, :].rearrange("p (a b) -> p a b", a=4),
                          AF.Copy, [bb], [btb])
    s.tap("bct", BCT, [bcb])
    wo_pre = outproj_load(s, l, 2, 768)
    k.barrier()
    ar.off = mark
    T = token_decay_arrays(s, a_all, u_all, ab)
    cs, nb, ecs, ws, gst = T["cs"], T["nb"], T["ecs"], T["ws"], T["gst"]
    Fst = ar.alloc([NT, 2, 4, 64], BF16, "F"); Fb = Buf("F")
    Sst = ar.alloc([2, 4, 64], F32, "S"); Sb = Buf("S")
    Kw = [ar.alloc([4, 128], BF16, "Kw%d" % i) for i in range(2)]; Kwb = [Buf("Kw0"), Buf("Kw1")]
    Es = [ar.alloc([2, 4, 128], BF16, "E%d" % i) for i in range(2)]; Ebs = [Buf("E0"), Buf("E1")]
    MTs = [ar.alloc([2, 4, 128], BF16, "MT%d" % i) for i in range(2)]; MTbs = [Buf("MT0"), Buf("MT1")]
    Rt = [ar.alloc([4, 64], F32, "R%d" % i) for i in range(2)]; Rb = Buf("R")
    tmp = ar.alloc([4, 64], F32, "tmp")
    yt = ar.alloc([4, 64], F32, "yt"); ytb = Buf("yt")
    sq_ = tmp
    zss = [ar.alloc([256], F32, "zs%d" % i) for i in range(2)]; zsbs = [Buf("zs0"), Buf("zs1")]
    ybs = [ar.alloc([256], BF16, "yb%d" % i) for i in range(3)]; ybbs = [Buf("yb%d" % i) for i in range(3)]
    ss2 = s.sc_alloc(2)
    s.memset(Sst, 0.0, [Sb])
    P23 = [s.pool([2, 4]), s.pool([3, 5])]
    for c in range(NT):
        for d in range(2):
            t_ = c if d == 0 else NT - 1 - c
            s.tt(Kw[d].rearrange("p (g e) n -> p g e n", g=2),
                 bc(BTM[:, t_, :, :].rearrange("p g (o n) -> p g o n", o=1), [128, 2, 2, 128]),
                 bc(ws[:, t_, d, :].rearrange("p (g e o) -> p g e o", g=2, o=1), [128, 2, 2, 128]),
                 ALU.mult, [btb, ab], [Kwb[d]])
            bk, bb = P23[d]()
            for h in range(4):
                s.mm(bk[:, h * 64:(h + 1) * 64], Kw[d][:, h, :], xTM[:, t_, h, :], True, True,
                     [Kwb[d], tmb], [bb], h == 3)
            s.act(Fst[:, t_, d, :, :], Sst[:, d, :, :], AF.Copy, [Sb], [Fb])
            s.tt(Sst[:, d, :, :], Sst[:, d, :, :],
                 bc(gst[:, t_, d, :].rearrange("p (a b) -> p a b", b=1), [128, 4, 64]), ALU.mult, [Sb, ab], [Sb])
            s.tt(Sst[:, d, :, :], Sst[:, d, :, :], bk[:, 0:256].rearrange("p (a b) -> p a b", a=4), ALU.add,
                 [Sb, bb], [Sb])
    bE = [(s.pool([0])()), (s.pool([1])())]
    bS = s.pool([2])()
    bI = [s.pool([3])(), s.pool([4])()]
    bJ = [s.pool([5])(), s.pool([6])()]
    bT = s.pool([7])()
    def stage_a(i):
        sl = slice(i * 128, (i + 1) * 128)
        E, Eb, MT, MTb, zs, zsb = Es[i % 2], Ebs[i % 2], MTs[i % 2], MTbs[i % 2], zss[i % 2], zsbs[i % 2]
        decay_E(s, i, a_all, nb, ab, E, Eb, bE)
        for g in range(2):
            s.mm(bS[0][:, g * 128:(g + 1) * 128], BCT[:, g, sl], BCT[:, 2 + g, sl], True, True, [bcb], [bS[1]], g == 1)
        for d in range(2):
            s.tt(MT[:, d, :, :].rearrange("p (g e) n -> p g e n", g=2),
                 bc(bS[0][:, 0:256].rearrange("p (g o n) -> p g o n", g=2, o=1), [128, 2, 2, 128]),
                 E[:, d, :, :].rearrange("p (g e) n -> p g e n", g=2), ALU.mult, [bS[1], Eb], [MTb])
        bk, bb = bT
        for kk in range(8):
            s.mm(bk[:, 0:256], s.XT[:, kk, sl], Wzv[:, kk, 0:256], kk == 0, kk == 7,
                 [s.XTb[i // 4], Wzb], [bb], kk == 7)
        s.act(zs, bk[:, 0:256], AF.Silu, [bb], [zsb])

    def stage_b(i):
        sl = slice(i * 128, (i + 1) * 128)
        yb, ybb = ybs[i % 3], ybbs[i % 3]
        E, Eb, MT, MTb, zs, zsb = Es[i % 2], Ebs[i % 2], MTs[i % 2], MTbs[i % 2], zss[i % 2], zsbs[i % 2]
        for d in range(2):
            for h in range(4):
                s.mm(bJ[d][0][:, h * 64:(h + 1) * 64], BCT[:, 2 + h // 2, sl], Fst[:, i, d, h, :], True, True,
                     [bcb, Fb], [bJ[d][1]], h == 3)
            for h in range(4):
                s.mm(bI[d][0][:, h * 64:(h + 1) * 64], MT[:, d, h, :], xTM[:, i, h, :], True, True,
                     [MTb, tmb], [bI[d][1]], h == 3)
            s.tt(tmp, bJ[d][0][:, 0:256].rearrange("p (a b) -> p a b", a=4),
                 bc(ecs[:, i, d, :].rearrange("p (a b) -> p a b", b=1), [128, 4, 64]), ALU.mult,
                 [bJ[d][1], ab], [Rb])
            s.tt(Rt[d], bI[d][0][:, 0:256].rearrange("p (a b) -> p a b", a=4), tmp, ALU.add, [bI[d][1], Rb], [Rb])
        s.tt(yt, Rt[0], Rt[1], ALU.add, [Rb], [ytb])
        s.tt(sq_, xTM[:, i, :, :], bc(s.PRM[:, P_SD:P_SD + 4].rearrange("p (a b) -> p a b", b=1), [128, 4, 64]),
             ALU.mult, [tmb, s.PRMb], [ytb, Rb])
        s.tt(yt, yt, sq_, ALU.add, [ytb, Rb], [ytb])
        yf = yt.rearrange("p a b -> p (a b)")
        s.tt(yf, yf, zs, ALU.mult, [ytb, zsb], [ytb])
        s.tt(sq_, yt, yt, ALU.mult, [ytb], [ytb, Rb])
        s.red(ss2, sq_.rearrange("p (g e) n -> p g (e n)", g=2), ALU.add, [ytb, Rb], [ytb])
        s.act(ss2, ss2, AF.Ln, [ytb], [ytb], scale=1.0 / 128.0, bias=1e-6)
        s.act(ss2, ss2, AF.Exp, [ytb], [ytb], scale=-0.5)
        s.tt(yt.rearrange("p (g e) n -> p g (e n)", g=2), yt.rearrange("p (g e) n -> p g (e n)", g=2),
             bc(ss2.rearrange("p (a b) -> p a b", b=1), [128, 2, 128]), ALU.mult, [ytb], [ytb])
        s.tt(yb, yf, s.PRM[:, P_SN:P_SN + 256], ALU.mult, [ytb, s.PRMb], [ybb])

    def stage_c(i):
        sl = slice(i * 128, (i + 1) * 128)
        yb, ybb = ybs[i % 3], ybbs[i % 3]
        bk, bb = bT
        pb = bk[:, 0:256].bitcast(BF16)
        for c in range(2):
            s.tr(pb[:, c * 128:(c + 1) * 128], yb[:, c * 128:(c + 1) * 128], s.identB, [ybb, s.CBb], [bb], c == 1)
        s.act(YC[:, :, sl], pb[:, 0:256].rearrange("p (a b) -> p a b", a=2), AF.Copy, [bb], [YCb])

    stage_a(0)
    for i in range(NT + 1):
        if i + 1 < NT:
            stage_a(i + 1)
        if i < NT:
            stage_b(i)
        if i >= 1:
            stage_c(i - 1)
    s.tap("yc", YC, [YCb])
    outproj_partial(s, l, YC, YCb, 2, 768, False, pre=wo_pre)


def layernorm_tiles(s, l, which, T1s, T1bs, LNP, LNPb, post):
    st6 = s.arena.alloc([2, 6], F32, "st6")
    mv = s.sc_alloc(2)
    sd = s.sc_alloc(1)
    lb = Buf("lnstat")
    def front(i):
        T1, T1b = T1s[i % 2], T1bs[i % 2]
        for hf in range(2):
            s.k.op("dve", lambda h: h.bn_stats(out=st6[:, hf, :], in_=s.X[:, i, hf * 512:(hf + 1) * 512]),
                   reads=[s.Xb[i]], writes=[lb])
        s.k.op("dve", lambda h: h.bn_aggr(out=mv, in_=st6.rearrange("p a b -> p (a b)")), reads=[lb], writes=[lb])
        s.act(sd, mv[:, 1:2], AF.Sqrt, [lb], [lb], scale=1.0, bias=1e-5)
        s.recip(sd, sd, [lb], [lb])
        s.ts(T1, s.X[:, i, :], mv[:, 0:1], sd, ALU.subtract, ALU.mult, [s.Xb[i], lb], [T1b])
        s.tt(T1, T1, LNP[:, 0, :], ALU.mult, [T1b, LNPb], [T1b])
        s.tt(T1, T1, LNP[:, 1, :], ALU.add, [T1b, LNPb], [T1b])

    front(0)
    for i in range(NT):
        if i + 1 < NT:
            front(i + 1)
        post(i, T1s[i % 2], T1bs[i % 2])


def phase_ln_router_moe(st, l, last):
    s = NS(st) if isinstance(st, dict) else st
    k, ar = s.k, s.arena
    k.barrier()
    ar.reset()
    s.sc_reset()
    s_l = s.s_l
    comb = ar.alloc([NT, 4, 4], F32, "comb"); combb = Buf("comb")
    mark = ar.off

    def load_gu(e):
        W, Wb, Ws = s.wslot()
        Wv = W[:, 0:4096].rearrange("p (k c) -> p k c", k=8)
        k.dma("pool", Wv[:, :, 0:256], s.d_wg[l, e].rearrange("(k p) f -> p k f", p=128), Ws, writes=[Wb])
        k.dma("pool", Wv[:, :, 256:512], s.d_wu[l, e].rearrange("(k p) f -> p k f", p=128), Ws, writes=[Wb])
        return Wv, Wb

    e0_pre = load_gu(0)
    LNP = ar.alloc([2, D], F32, "LNP"); LNPb = Buf("LNP")
    for q in range(2):
        k.dma("sp", LNP[:, q, :], s.dlnp[l, q:q + 1, :].to_broadcast([128, D]), s_l, writes=[LNPb])
    T1s = [ar.alloc([D], F32, "T1%d" % i) for i in range(2)]; T1bs = [Buf("T10"), Buf("T11")]
    x32s = [ar.alloc([8, 128], F32, "x32%d" % i) for i in range(2)]; x32bs = [Buf("x320"), Buf("x321")]
    RL = ar.alloc([NT, 20], F32, "RL"); RLb = Buf("RL")
    PR = s.pool([4, 5])

    def post1(i, T1, T1b):
        x32, x32b = x32s[i % 2], x32bs[i % 2]
        s.transpose_x_tile(i, T1, [T1b], dst32=x32, dst32b=x32b)
        s.act(s.X[:, i, :], T1, AF.Copy, [T1b], [s.Xb[i]], scale=ALPHA)
        bk, bb = PR()
        for c in range(8):
            s.mm(bk[:, 0:20], x32[:, c, :], s.PRM[:, P_RW + c * 20:P_RW + (c + 1) * 20], c == 0, c == 7,
                 [x32b, s.PRMb], [bb], c == 7)
        s.tt(RL[:, i, :], bk[:, 0:20], s.PRM[:, P_RB:P_RB + 20], ALU.add, [bb, s.PRMb], [RLb])

    layernorm_tiles(s, l, 0, T1s, T1bs, LNP, LNPb, post1)
    s.tap("x1", s.X[:, :, :], s.Xb)
    gl = RL[:, :, 0:4]
    el = RL[:, :, 4:20].rearrange("p t (g e) -> p t g e", g=4)
    A1 = lambda n: ar.alloc([NT, n], F32, "r%d" % n)
    gmax, gsum, gp, emax1, emax2, ssum, rg = A1(1), A1(1), A1(1), A1(1), A1(1), A1(1), A1(1)
    gsh, ohg, esel, esel2, m1, m2, ee = A1(4), A1(4), A1(4), A1(4), A1(4), A1(4), A1(4)
    t16 = ar.alloc([NT, 4, 4], F32, "t16")
    rb_ = Buf("route")
    b4 = lambda a: bc(a, [128, NT, 4])
    R_, W_ = [RLb, rb_], [rb_]
    s.red(gmax, gl, ALU.max, R_, W_)
    s.tt(gsh, gl, b4(gmax), ALU.subtract, R_, W_)
    s.act(gsh, gsh, AF.Exp, R_, W_)
    s.red(gsum, gsh, ALU.add, R_, W_)
    s.recip(gp, gsum, R_, W_)
    s.tt(ohg, gl, b4(gmax), ALU.is_equal, R_, W_)
    s.tt(t16, el, bc(ohg.rearrange("p t (g o) -> p t g o", o=1), [128, NT, 4, 4]), ALU.mult, R_, W_)
    s.red(esel, t16.rearrange("p t g e -> p t e g"), ALU.add, R_, W_)
    s.red(emax1, esel, ALU.max, R_, W_)
    s.tt(m1, esel, b4(emax1), ALU.is_equal, R_, W_)
    s.stt(esel2, m1, -1e30, esel, ALU.mult, ALU.add, R_, W_)
    s.red(emax2, esel2, ALU.max, R_, W_)
    s.tt(m2, esel2, b4(emax2), ALU.is_equal, R_, W_)
    s.tt(m1, m1, m2, ALU.add, R_, W_)
    s.tt(ee, esel, b4(emax1), ALU.subtract, R_, W_)
    s.act(ee, ee, AF.Exp, R_, W_)
    s.tt(ee, ee, m1, ALU.mult, R_, W_)
    s.red(ssum, ee, ALU.add, R_, W_)
    s.recip(rg, ssum, R_, W_)
    s.tt(rg, rg, gp, ALU.mult, R_, W_)
    s.tt(ee, ee, b4(rg), ALU.mult, R_, W_)
    s.tt(comb, bc(ohg.rearrange("p t (g o) -> p t g o", o=1), [128, NT, 4, 4]),
         bc(ee.rearrange("p t (o e) -> p t o e", o=1), [128, NT, 4, 4]), ALU.mult, R_, [combb])
    s.tap("comb", comb, [combb])
    if s.stop_after == "ln1":
        return
    k.barrier()
    ar.off = mark
    hT = [ar.alloc([2, S], BF16, "hT%d" % i) for i in range(4)]; hTb = [Buf("hT%d" % i) for i in range(4)]
    WD = [[ar.alloc([2, D], BF16, "WD%d_%d" % (g_, e_)) for e_ in range(4)] for g_ in range(2)]
    WDb = [[Buf("WD") for _ in range(4)] for _ in range(2)]
    WDs = s.wd_sems
    NB_ = 4
    sgs = [ar.alloc([256], F32, "sg%d" % i) for i in range(NB_)]; sgb = [Buf("sg%d" % i) for i in range(NB_)]
    hms = [ar.alloc([256], BF16, "hm%d" % i) for i in range(NB_)]; hmb = [Buf("hm%d" % i) for i in range(NB_)]
    trq = []
    PG = s.pool([0, 1, 2])
    PTr = s.pool([3, 4])
    PD = s.pool([5, 6, 7])
    slots = {}

    def load_expert(e):
        if e == 0:
            Wv, Wb = e0_pre
        else:
            Wv, Wb = load_gu(e)
        g_, e_ = (e // 4) % 2, e % 4
        k.dma("pool", WD[g_][e_], s.d_wd[l, e].rearrange("(c p) n -> p c n", p=128), WDs[g_ * 4 + e_],
              writes=[WDb[g_][e_]])
        slots[e] = (Wv, Wb)

    load_expert(0)
    cnt = 0
    for e in range(16):
        if e + 1 < 16:
            load_expert(e + 1)
        Wv, Wb = slots.pop(e)
        e4 = e % 4
        def moe_tr(item):
            (i_, hm_, hmb2, e4_) = item
            sl_ = slice(i_ * 128, (i_ + 1) * 128)
            tk_, tb_ = PTr()
            pb = tk_[:, 0:128].bitcast(BF16)
            for c in range(2):
                s.tr(pb[:, c * 128:(c + 1) * 128], hm_[:, c * 128:(c + 1) * 128], s.identB, [hmb2, s.CBb], [tb_], c == 1)
            s.act(hT[e4_][:, :, sl_], pb[:, 0:256].rearrange("p (a b) -> p a b", a=2), AF.Copy, [tb_], [hTb[e4_]])

        for i in range(NT):
            sl = slice(i * 128, (i + 1) * 128)
            bk, bb = PG()
            for kk in range(8):
                s.mm(bk[:, :], s.XT[:, kk, sl], Wv[:, kk, :], kk == 0, kk == 7, [s.XTb[i // 4], Wb], [bb], kk == 7)
            sg, sgb_ = sgs[cnt % NB_], sgb[cnt % NB_]
            hm, hmb_ = hms[cnt % NB_], hmb[cnt % NB_]
            cnt += 1
            s.act(sg, bk[:, 0:256], AF.Silu, [bb], [sgb_])
            s.stt(hm, bk[:, 256:512], comb[:, i, e // 4, e4:e4 + 1], sg, ALU.mult, ALU.mult,
                  [bb, combb, sgb_], [hmb_])
            trq.append((i, hm, hmb_, e4))
            if len(trq) > 2:
                moe_tr(trq.pop(0))
        if e4 == 3:
            while trq:
                moe_tr(trq.pop(0))
        if e4 == 3:
            g_ = (e // 4) % 2
            for i in range(NT):
                sl = slice(i * 128, (i + 1) * 128)
                for hf in range(2):
                    bk, bb = PD()
                    n = 0
                    for ee_ in range(4):
                        for c in range(2):
                            s.mm(bk[:, :], hT[ee_][:, c, sl], WD[g_][ee_][:, c, hf * 512:(hf + 1) * 512],
                                 n == 0, n == 7, [hTb[ee_], WDb[g_][ee_]], [bb], n == 7)
                            n += 1
                    xs = s.X[:, i, hf * 512:(hf + 1) * 512]
                    s.tt(xs, xs, bk[:, :], ALU.add, [s.Xb[i], bb], [s.Xb[i]])
    if s.stop_after == "moe":
        return
    if not last:
        preload_mla(s, l + 1)
    k.barrier()
    ar.off = mark
    LNP = ar.alloc([2, D], F32, "LNP2"); LNPb = Buf("LNP2")
    for q in range(2):
        k.dma("sp", LNP[:, q, :], s.dlnp[l, 2 + q:3 + q, :].to_broadcast([128, D]), s_l, writes=[LNPb])
    T1s = [ar.alloc([D], F32, "T2%d" % i) for i in range(2)]; T1bs = [Buf("T20"), Buf("T21")]

    def post2(i, T1, T1b):
        if not last:
            s.transpose_x_tile(i, T1, [T1b])
        s.act(s.X[:, i, :], T1, AF.Copy, [T1b], [s.Xb[i]])

    layernorm_tiles(s, l, 1, T1s, T1bs, LNP, LNPb, post2)
    k.barrier()


def _consts():
    c = np.zeros((128, CW), np.float32)
    r = np.arange(128)
    c[:, C_ID:C_ID + 128] = np.eye(128, dtype=np.float32)
    c[:, C_U:C_U + 128] = (r[:, None] <= r[None, :]).astype(np.float32)
    c[:, C_L:C_L + 128] = (r[:, None] >= r[None, :]).astype(np.float32)
    c[:, C_MF:C_MF + 128] = np.where(r[:, None] <= r[None, :], 0.0, NEG).astype(np.float32)
    c[:, C_MB:C_MB + 128] = np.where(r[:, None] >= r[None, :], 0.0, NEG).astype(np.float32)
    c[:, C_ONE:C_ONE + 128] = 1.0
    inv = (10000.0 ** (-np.arange(0, 32, 2, dtype=np.float32) / np.float32(32))).astype(np.float32)
    c[:, C_IF:C_IF + 16] = inv[None, :]
    return c


def _pack_prm(inp, l):
    p = np.zeros((128, PW), np.float32)
    rep = lambda v: np.broadcast_to(np.asarray(v, np.float32).reshape(1, -1), (128, np.asarray(v).size))
    p[:, P_GQ:P_GQ + 2] = inp["mla_q_norm"][l].reshape(2, 128).T
    p[:, P_GKV] = inp["mla_kv_norm"][l]
    p[:, P_GB:P_GB + 16] = rep(inp["mlstm_gate_bias"][l])
    p[:, P_MN:P_MN + 256] = rep(inp["mlstm_norm"][l])
    cw = inp["ssd_conv_w"][l]
    p[:, P_CW:P_CW + 30] = cw.reshape(5, 6, 128).transpose(2, 1, 0).reshape(128, 30)
    p[:, P_CB:P_CB + 6] = inp["ssd_conv_b"][l].reshape(6, 128).T
    p[:, P_DTB:P_DTB + 8] = rep(inp["ssd_dt_bias"][l])
    p[:, P_AL:P_AL + 8] = rep(inp["ssd_a_log"][l])
    p[:, P_SD:P_SD + 4] = rep(inp["ssd_d"][l])
    p[:, P_SN:P_SN + 256] = rep(inp["ssd_norm"][l])
    rb = np.concatenate([inp["router_group_b"][l].reshape(-1), inp["router_expert_b"][l].reshape(-1)])
    p[:, P_RB:P_RB + 20] = rep(rb)
    wr = np.concatenate([inp["router_group_w"][l],
                         inp["router_expert_w"][l].transpose(1, 0, 2).reshape(1024, 16)], axis=1)
    p[:, P_RW:P_RW + 160] = wr.reshape(8, 128, 20).transpose(1, 0, 2).reshape(128, 160)
    return p


def make_in_maps(inp, layers):
    inp = {k_: np.asarray(v) for k_, v in inp.items()}
    L = list(layers)
    sl = lambda a: np.ascontiguousarray(a[L])
    shared = {
        "cst": _consts(),
        "prm": np.stack([_pack_prm(inp, l) for l in L]),
        "w_in": sl(inp["w_in"]), "w_uq": sl(inp["mla_w_uq"]), "w_ukv": sl(inp["mla_w_ukv"]),
        "w_out": sl(inp["w_out"]),
        "lnp": np.stack([np.stack([inp["ln1_g"][l], inp["ln1_b"][l], inp["ln2_g"][l], inp["ln2_b"][l]]) for l in L]),
        "wg": sl(inp["expert_w_gate"]), "wu": sl(inp["expert_w_up"]), "wd": sl(inp["expert_w_down"]),
    }
    return shared


_CACHE = {}


def _prog(NL):
    if NL not in _CACHE:
        _CACHE[NL] = build(NL)[0]
    return _CACHE[NL]


FUSED = True


def kernel(**inputs):
    x = np.asarray(inputs["x"], np.float32)
    pos = np.asarray(inputs["positions"]).astype(np.int32)
    B = x.shape[0]
    posl = [np.ascontiguousarray(pos[b].reshape(NT, 128).T) for b in range(B)]
    groups = [list(range(DEPTH))] if FUSED else [[l] for l in range(DEPTH)]
    cur = [np.ascontiguousarray(x[b]) for b in range(B)]
    for L in groups:
        nc = _prog(len(L))
        shared = make_in_maps(inputs, L)
        in_maps = []
        for b in range(B):
            m = dict(shared)
            m["x"] = cur[b]
            m["pos"] = posl[b]
            in_maps.append(m)
        res = run_bass_kernel_spmd(nc, in_maps, core_ids=list(range(B)))
        cur = [np.ascontiguousarray(np.asarray(r["y"], dtype=np.float32)) for r in res.results]
    return np.stack(cur).astype(np.float32)
```

```python
import math
import numpy as np
import concourse.bass as bass
import concourse.mybir as mybir
from concourse.bass_utils import run_bass_kernel_spmd

F32 = mybir.dt.float32
BF16 = mybir.dt.bfloat16
I32 = mybir.dt.int32
AF = mybir.ActivationFunctionType
ALU = mybir.AluOpType
AX = mybir.AxisListType

S = 2048
D = 1024
NT = 16
DEPTH = 4
ALPHA = (2 * DEPTH) ** 0.25
IN_W = 2488
PI = math.pi
NEG = -30000.0

P_GQ, P_GKV, P_GB, P_MN, P_CW, P_CB, P_DTB, P_AL, P_SD, P_SN, P_RB, P_RW = (
    0, 2, 3, 19, 275, 305, 311, 319, 327, 331, 587, 607)
PW = 767
C_ID, C_U, C_L, C_MF, C_MB, C_ONE, C_IF = 0, 128, 256, 384, 512, 640, 768
CW = 784

WSLOT = 6144
NSLOT = 2
ARENA_BYTES = 74 * 1024


class Sem:
    def __init__(self, nc, name, dma=False):
        self.h = nc.alloc_semaphore(name)
        self.val = 0
        self.dma = dma
        self.name = name


class Buf:
    __slots__ = ("name", "w", "r", "excl")

    def __init__(self, name, excl=False):
        self.name = name
        self.w = None
        self.r = {}
        self.excl = excl


class K:
    def __init__(self, nc):
        self.nc = nc
        self.eng = {}
        for n, a in (("pe", "tensor"), ("dve", "vector"), ("act", "scalar"),
                     ("pool", "gpsimd"), ("sp", "sync")):
            self.eng[n] = (getattr(nc, a), Sem(nc, "e_" + n))
        self.known = {n: {} for n in self.eng}
        self.dsems = []
        self.ninst = 0

    def dsem(self, name):
        s = Sem(self.nc, name, dma=True)
        self.dsems.append(s)
        return s

    def _wait(self, e, tickets):
        h, own = self.eng[e]
        kn = self.known[e]
        need = {}
        for (s, v) in tickets:
            if s.dma:
                v = s.val
            if v > kn.get(s, 0) and v > need.get(s, 0):
                need[s] = v
        for s, v in need.items():
            assert s.val >= v, "wait on unsignalled ticket %s %d>%d (eng %s)" % (s.name, v, s.val, e)
            h.wait_ge(s.h, v)
            kn[s] = v
            self.ninst += 1

    def _tickets(self, reads, writes):
        tk = []
        for b in reads:
            if b.w is not None:
                tk.append(b.w)
        for b in writes:
            if b.w is not None:
                tk.append(b.w)
            for s, v in b.r.items():
                tk.append((s, v))
        return tk

    def _commit(self, t, reads, writes):
        for b in writes:
            b.w = t
            b.r = {}
        for b in reads:
            if b not in writes:
                if t[1] > b.r.get(t[0], 0):
                    b.r[t[0]] = t[1]

    def op(self, e, fn, reads=(), writes=(), signal=True, keep_self=False):
        h, sem = self.eng[e]
        ex = [b for b in reads if b.excl and b not in writes]
        if ex:
            writes = list(writes) + ex
            reads = [b for b in reads if not b.excl]
        tk = self._tickets(reads, writes)
        if e == "pe" and not keep_self:
            tk = [(s, v) for (s, v) in tk if s is not sem]
        self._wait(e, tk)
        inst = fn(h)
        self.ninst += 1
        if signal:
            sem.val += 1
            inst.then_inc(sem.h, 1)
            t = (sem, sem.val)
        else:
            t = (sem, sem.val + 1)
        self._commit(t, reads, writes)
        return inst

    def dma(self, q, out, in_, dsem, reads=(), writes=(), **kw):
        h, _ = self.eng[q]
        self._wait(q, self._tickets(reads, writes))
        inst = h.dma_start(out=out, in_=in_, **kw)
        self.ninst += 1
        dsem.val += 16
        inst.then_inc(dsem.h, 16)
        self._commit((dsem, dsem.val), reads, writes)

    def barrier(self):
        allt = [(s, s.val) for (_, s) in self.eng.values() if s.val > 0]
        allt += [(s, s.val) for s in self.dsems if s.val > 0]
        for e in self.eng:
            self._wait(e, allt)


class Rot:
    def __init__(self, items):
        self.items = list(items)
        self.i = 0

    def __call__(self):
        x = self.items[self.i % len(self.items)]
        self.i += 1
        return x


class Arena:
    def __init__(self, nc, nbytes):
        self.t = nc.alloc_sbuf_tensor("arena", [128, nbytes // 2], BF16)
        self.nbytes = nbytes
        self.off = 0
        self.peak = 0

    def reset(self):
        self.off = 0

    def alloc(self, shape, dtype, name="a"):
        esz = 4 if dtype in (F32, I32) else 2
        n = 1
        for s in shape:
            n *= s
        nb = (n * esz + 63) // 64 * 64
        assert self.off + nb <= self.nbytes, "arena overflow %s need %d have %d" % (
            name, nb, self.nbytes - self.off)
        ap = self.t[:, self.off // 2:(self.off + n * esz) // 2]
        if esz == 4:
            ap = ap.bitcast(dtype)
        if len(shape) == 2:
            ap = ap.rearrange("p (a b) -> p a b", a=shape[0])
        elif len(shape) == 3:
            ap = ap.rearrange("p (a b c) -> p a b c", a=shape[0], b=shape[1])
        elif len(shape) == 4:
            ap = ap.rearrange("p (a b c d) -> p a b c d", a=shape[0], b=shape[1], c=shape[2])
        self.off += nb
        self.peak = max(self.peak, self.off)
        return ap


def bc(ap, shape):
    return ap.to_broadcast(list(shape))


def build(NL, taps=(), stop_after=None):
    nc = bass.Bass("TRN2", target_bir_lowering=False)
    k = K(nc)

    def din(name, shape, dt=F32):
        return nc.dram_tensor(name, list(shape), dt, kind="ExternalInput").ap()

    dx = din("x", [S, D])
    dpos = din("pos", [128, NT], I32)
    dcst = din("cst", [128, CW])
    dprm = din("prm", [NL, 128, PW])
    w_in = din("w_in", [NL, D, IN_W])
    w_uq = din("w_uq", [NL, 256, 768])
    w_ukv = din("w_ukv", [NL, 128, 1024])
    w_out = din("w_out", [NL, D, D])
    dlnp = din("lnp", [NL, 4, D])
    d_wg = din("wg", [NL, 16, D, 256])
    d_wu = din("wu", [NL, 16, D, 256])
    d_wd = din("wd", [NL, 16, 256, D])
    dy = nc.dram_tensor("y", [S, D], F32, kind="ExternalOutput").ap()
    tap_out = {}

    X = nc.alloc_sbuf_tensor("X", [128, NT, D], F32)
    Xb = [Buf("X%d" % i) for i in range(NT)]
    XT = nc.alloc_sbuf_tensor("XT", [128, 8, S], BF16)
    XTb = [Buf("XT%d" % g) for g in range(4)]
    WS = [nc.alloc_sbuf_tensor("ws%d" % i, [128, WSLOT], BF16) for i in range(NSLOT)]
    WSb = [Buf("ws%d" % i) for i in range(NSLOT)]
    WSs = [k.dsem("ws%d" % i) for i in range(NSLOT)]
    CF = nc.alloc_sbuf_tensor("cf", [128, CW], F32)
    CFb = Buf("cf")
    CB = nc.alloc_sbuf_tensor("cb", [128, CW], BF16)
    CBb = Buf("cb")
    PRM = nc.alloc_sbuf_tensor("prm_s", [128, PW], F32)
    PRMb = Buf("prm")
    SCT = nc.alloc_sbuf_tensor("sc", [128, 1024], F32)
    sc_off = [0]

    def sc_reset():
        sc_off[0] = 0

    def sc_alloc(n):
        o = sc_off[0]
        assert o + n <= 1024, "scalar pool overflow"
        sc_off[0] = o + n
        return SCT[:, o:o + n]
    COS = nc.alloc_sbuf_tensor("cos", [128, NT, 16], F32)
    SIN = nc.alloc_sbuf_tensor("sin", [128, NT, 16], F32)
    CSb = Buf("cossin")
    arena = Arena(nc, ARENA_BYTES)
    banks = [nc.alloc_psum_tensor("pb%d" % i, [128, 512], F32) for i in range(8)]
    bankb = [Buf("pb%d" % i, excl=True) for i in range(8)]

    def pool(idx):
        return Rot([(banks[i], bankb[i]) for i in idx])

    s_c = k.dsem("cst")
    s_x = k.dsem("xld")
    s_p = k.dsem("prm")
    s_l = k.dsem("lnp")
    s_y = k.dsem("yst")
    s_t = k.dsem("tap")

    wd_sems = [k.dsem("wd%d" % i) for i in range(8)]
    identF = CF[:, C_ID:C_ID + 128]
    identB = CB[:, C_ID:C_ID + 128]
    slot_ctr = [0]

    def wslot():
        i = slot_ctr[0] % NSLOT
        slot_ctr[0] += 1
        return WS[i], WSb[i], WSs[i]

    def tap(name, ap, reads):
        if name not in taps:
            return
        shp = list(ap.shape)
        n = 1
        for s_ in shp[1:]:
            n *= s_
        cnt = sum(1 for t_ in tap_out if t_.startswith(name))
        nm = name if cnt == 0 else "%s_%d" % (name, cnt)
        dt_ = nc.dram_tensor("tap_" + nm, shp, ap.dtype, kind="ExternalOutput").ap()
        tap_out[nm] = shp
        k.dma("sp", dt_, ap, s_t, reads=reads)

    def mm(out, lhsT, rhs, start, stop, reads, writes, signal, serial=False):
        k.op("pe", lambda h: h.matmul(out, lhsT, rhs, start=start, stop=stop),
             reads=reads, writes=writes, signal=(signal or serial), keep_self=serial)

    def act(out, in_, func, reads, writes, **kw):
        k.op("act", lambda h: h.activation(out=out, in_=in_, func=func, **kw), reads=reads, writes=writes)

    def tt(out, in0, in1, op, reads, writes, e="dve"):
        k.op(e, lambda h: h.tensor_tensor(out=out, in0=in0, in1=in1, op=op), reads=reads, writes=writes)

    def ts(out, in0, s1, s2, op0, op1, reads, writes, e="dve"):
        if s2 is None:
            k.op(e, lambda h: h.tensor_scalar(out=out, in0=in0, scalar1=s1, scalar2=None, op0=op0),
                 reads=reads, writes=writes)
        else:
            k.op(e, lambda h: h.tensor_scalar(out=out, in0=in0, scalar1=s1, scalar2=s2, op0=op0, op1=op1),
                 reads=reads, writes=writes)

    def stt(out, in0, scalar, in1, op0, op1, reads, writes):
        k.op("dve", lambda h: h.scalar_tensor_tensor(out=out, in0=in0, scalar=scalar, in1=in1, op0=op0, op1=op1),
             reads=reads, writes=writes)

    def cp(out, in_, reads, writes, e="dve"):
        k.op(e, lambda h: h.tensor_copy(out=out, in_=in_), reads=reads, writes=writes)

    def red(out, in_, op, reads, writes):
        k.op("dve", lambda h: h.tensor_reduce(out=out, in_=in_, axis=AX.X, op=op), reads=reads, writes=writes)

    def recip(out, in_, reads, writes):
        k.op("dve", lambda h: h.reciprocal(out=out, in_=in_), reads=reads, writes=writes)

    def memset(ap, val, writes, e="dve"):
        k.op(e, lambda h: h.memset(ap, val), writes=writes)

    def tr(out, in_, ident, reads, writes, signal):
        k.op("pe", lambda h: h.transpose(out, in_, ident), reads=reads, writes=writes, signal=signal)

    k.dma("sp", CF[:, :], dcst[:, :], s_c, writes=[CFb])
    s_cb = k.dsem("cstb")
    k.dma("pool", CB[:, :], dcst[:, :], s_cb, writes=[CBb])
    dxv = dx.rearrange("(i p) d -> p i d", p=128)
    for q in range(4):
        k.dma("sp", X[:, 4 * q:4 * q + 4, :], dxv[:, 4 * q:4 * q + 4, :], s_x, writes=Xb[4 * q:4 * q + 4])

    arena.reset()
    tb = Buf("setup_tmp")
    posi = arena.alloc([NT, 1], I32, "posi")
    posf = arena.alloc([NT, 1], F32, "posf")
    ang = arena.alloc([NT, 16], F32, "ang")
    kf = arena.alloc([NT, 16], F32, "kf")
    ki = arena.alloc([NT, 16], I32, "ki")
    mt_ = arena.alloc([NT, 16], F32, "mt")
    k.dma("sp", posi[:, :, 0], dpos[:, :], s_c, writes=[tb])
    cp(posf, posi, [tb], [tb])
    invf = CF[:, C_IF:C_IF + 16].rearrange("p (a b) -> p a b", a=1)
    tt(ang, bc(posf, [128, NT, 16]), bc(invf, [128, NT, 16]), ALU.mult, [tb, CFb], [tb])
    ts(kf, ang, 1.0 / (2 * PI), None, ALU.mult, None, [tb], [tb])
    cp(ki, kf, [tb], [tb])
    cp(kf, ki, [tb], [tb])
    stt(ang, kf, -2 * PI, ang, ALU.mult, ALU.add, [tb], [tb])

    def wrap(r):
        ts(mt_, r, PI, -2 * PI, ALU.is_gt, ALU.mult, [tb], [tb])
        tt(r, r, mt_, ALU.add, [tb], [tb])
        ts(mt_, r, -PI, 2 * PI, ALU.is_lt, ALU.mult, [tb], [tb])
        tt(r, r, mt_, ALU.add, [tb], [tb])

    wrap(ang)
    act(SIN[:, :, :], ang, AF.Sin, [tb], [CSb])
    ts(ang, ang, PI / 2, None, ALU.add, None, [tb, CSb], [tb])
    wrap(ang)
    act(COS[:, :, :], ang, AF.Sin, [tb], [CSb])

    PT4 = pool([0, 1, 2, 3, 6, 7])

    def transpose_x_tile(i, src_tile_ap, src_reads, dst32=None, dst32b=None):
        for c0 in (0, 4):
            bk, bb = PT4()
            for j in range(4):
                c = c0 + j
                tr(bk[:, j * 128:(j + 1) * 128], src_tile_ap[:, c * 128:(c + 1) * 128], identF,
                   src_reads + [CFb], [bb], j == 3)
            bv = bk[:, :].rearrange("p (a b) -> p a b", a=4)
            act(XT[:, c0:c0 + 4, i * 128:(i + 1) * 128], bv, AF.Copy, [bb], [XTb[i // 4]])
            if dst32 is not None:
                cp(dst32[:, c0:c0 + 4, :], bv, [bb], [dst32b])

    for i in range(NT):
        transpose_x_tile(i, X[:, i, :], [Xb[i]])
    k.barrier()

    st = dict(nc=nc, k=k, arena=arena, X=X, Xb=Xb, XT=XT, XTb=XTb, CF=CF, CFb=CFb, CB=CB, CBb=CBb,
              PRM=PRM, PRMb=PRMb, sc_alloc=sc_alloc, sc_reset=sc_reset, COS=COS, SIN=SIN, CSb=CSb, pool=pool,
              wslot=wslot, tap=tap, mm=mm, act=act, tt=tt, ts=ts, stt=stt, cp=cp, red=red, recip=recip,
              memset=memset, tr=tr, identF=identF, identB=identB, w_in=w_in, w_uq=w_uq, w_ukv=w_ukv,
              w_out=w_out, dlnp=dlnp, d_wg=d_wg, d_wu=d_wu, d_wd=d_wd, dprm=dprm, s_p=s_p, s_l=s_l,
              transpose_x_tile=transpose_x_tile, stop_after=stop_after, wd_sems=wd_sems, pre={})

    st = NS(st)
    for l in range(NL):
        k.dma("sp", PRM[:, :], dprm[l], s_p, writes=[PRMb])
        if stop_after == "setup":
            break
        phase_mla(st, l)
        if stop_after is not None and stop_after.startswith("mla"):
            break
        phase_mlstm(st, l)
        if stop_after is not None and stop_after.startswith("mlstm"):
            break
        phase_ssd(st, l)
        if stop_after is not None and stop_after.startswith("ssd"):
            break
        phase_ln_router_moe(st, l, last=(l == NL - 1))

    k.barrier()
    dyv = dy.rearrange("(i p) d -> p i d", p=128)
    for q in range(4):
        k.dma("sp", dyv[:, 4 * q:4 * q + 4, :], X[:, 4 * q:4 * q + 4, :], s_y, reads=Xb[4 * q:4 * q + 4])
    k._wait("sp", [(s_y, s_y.val), (s_t, s_t.val)] if s_t.val else [(s_y, s_y.val)])
    return nc, tap_out, k


class NS:
    def __init__(self, d):
        self.__dict__.update(d)


def rope_tm(st, src, dst, tmpa, tmpb, sb, db):
    s = st
    t1, t2 = src[:, :, 0:16], src[:, :, 16:32]
    C, Sn = s.COS[:, :, :], s.SIN[:, :, :]
    tb_ = Buf("rope_tmp")
    s.tt(tmpa, t1, C, ALU.mult, [sb, s.CSb], [tb_])
    s.tt(tmpb, t2, Sn, ALU.mult, [sb, s.CSb, tb_], [tb_])
    s.tt(dst[:, :, 0:16], tmpa, tmpb, ALU.subtract, [tb_], [db])
    s.tt(tmpa, t2, C, ALU.mult, [sb, s.CSb, db], [tb_])
    s.tt(tmpb, t1, Sn, ALU.mult, [sb, s.CSb, tb_], [tb_])
    s.tt(dst[:, :, 16:32], tmpa, tmpb, ALU.add, [tb_], [db])


def outproj_load(st, l, nchunk, row0):
    s = st
    W, Wb, Wsm = s.wslot()
    Wv = W[:, 0:nchunk * 1024].rearrange("p (c n) -> p c n", c=nchunk)
    src = s.w_out[l, row0:row0 + nchunk * 128, :].rearrange("(c p) n -> p c n", p=128)
    s.k.dma("pool", Wv, src, Wsm, writes=[Wb])
    return Wv, Wb


def outproj_partial(st, l, YT, YTb, nchunk, row0, first, pre=None):
    s = st
    Wv, Wb = pre if pre is not None else outproj_load(s, l, nchunk, row0)
    PA = s.pool([0, 1, 2, 3])
    for i in range(NT):
        for hf in range(2):
            bk, bb = PA()
            for c in range(nchunk):
                s.mm(bk[:, :], YT[:, c, i * 128:(i + 1) * 128], Wv[:, c, hf * 512:(hf + 1) * 512],
                     c == 0, c == nchunk - 1, [YTb, Wb], [bb], c == nchunk - 1)
            xs = s.X[:, i, hf * 512:(hf + 1) * 512]
            if first:
                s.stt(xs, xs, ALPHA, bk[:, :], ALU.mult, ALU.add, [s.Xb[i], bb], [s.Xb[i]])
            else:
                s.tt(xs, xs, bk[:, :], ALU.add, [s.Xb[i], bb], [s.Xb[i]])


def preload_mla(s, l):
    k = s.k
    W0, W0b, W0s = s.wslot()
    W0v = W0[:, 0:8 * 416].rearrange("p (k c) -> p k c", k=8)
    k.dma("pool", W0v, s.w_in[l].rearrange("(k p) c -> p k c", p=128)[:, :, 0:416], W0s, writes=[W0b])
    W1, W1b, W1s = s.wslot()
    Wuq = W1[:, 0:1536].rearrange("p (c n) -> p c n", c=2)
    k.dma("pool", Wuq, s.w_uq[l].rearrange("(c p) n -> p c n", p=128), W1s, writes=[W1b])
    Wkv = W1[:, 1536:2560]
    k.dma("pool", Wkv, s.w_ukv[l], W1s, writes=[W1b])
    s.pre["mla"] = (W0v, W0b, Wuq, Wkv, W1b)


def preload_mlstm_a(s, l):
    Wa, Wab, Was = s.wslot()
    Wav = Wa[:, 0:8 * 512].rearrange("p (k c) -> p k c", k=8)
    wsrc = s.w_in[l].rearrange("(k p) c -> p k c", p=128)
    s.k.dma("pool", Wav, wsrc[:, :, 416:928], Was, writes=[Wab])
    s.pre["mlstm_a"] = (Wav, Wab)


def preload_ssd_x(s, l):
    Wx, Wxb, Wxs = s.wslot()
    Wxv = Wx[:, 0:6144].rearrange("p (k c) -> p k c", k=8)
    wsrc = s.w_in[l].rearrange("(k p) c -> p k c", p=128)
    s.k.dma("pool", Wxv, wsrc[:, :, 1712:2480], Wxs, writes=[Wxb])
    s.pre["ssd_x"] = (Wxv, Wxb)


def phase_mla(st, l):
    s = NS(st) if isinstance(st, dict) else st
    k, ar = s.k, s.arena
    ar.reset()
    PA = s.pool([0, 1, 2, 3])
    PB = s.pool([4, 5])
    PC = s.pool([6, 7])
    if "mla" not in s.pre:
        preload_mla(s, l)
    W0v, W0b, Wuq, Wkv, W1b = s.pre.pop("mla")
    for c in range(2):
        s.ts(Wuq[:, c, :], Wuq[:, c, :], s.PRM[:, P_GQ + c:P_GQ + c + 1], None, ALU.mult, None,
             [s.PRMb, W1b], [W1b])
    s.ts(Wkv, Wkv, s.PRM[:, P_GKV:P_GKV + 1], None, ALU.mult, None, [s.PRMb, W1b], [W1b])

    if s.stop_after == "mla_w":
        return
    cT = ar.alloc([3, S], BF16, "cT"); cTb = Buf("cT")
    YA = ar.alloc([4, S], BF16, "YA"); YAb = Buf("YA")
    krr = ar.alloc([NT, 32], F32, "krr"); krrb = Buf("krr")
    krb = ar.alloc([NT, 32], BF16, "krb"); krbb = Buf("krb")
    sq = ar.alloc([384], F32, "sq"); sqb = Buf("sq")
    s.sc_reset()
    ssq = s.sc_alloc(NT); sskv = s.sc_alloc(NT); ssb = Buf("ss")
    rq = s.sc_alloc(NT); rkv = s.sc_alloc(NT); rb_ = Buf("r")
    ta = ar.alloc([NT, 16], F32, "ta"); tb2 = ar.alloc([NT, 16], F32, "tb")
    qhb = ar.alloc([NT, 96], BF16, "qhb"); qhbb = Buf("qhb")
    qr = ar.alloc([NT, 32], F32, "qr"); qrb = Buf("qr")
    khb = ar.alloc([NT, 96], BF16, "khb"); khbb = Buf("khb")
    VA = [ar.alloc([NT, 65], BF16, "VA%d" % i) for i in range(2)]; VAb = [Buf("VA0"), Buf("VA1")]
    QT = [ar.alloc([S], BF16, "QT%d" % i) for i in range(2)]; QTb = [Buf("QT0"), Buf("QT1")]
    KT = [ar.alloc([S], BF16, "KT%d" % i) for i in range(2)]; KTb = [Buf("KT0"), Buf("KT1")]
    PTs = [(ar.alloc([512], BF16, "PT%d" % i), Buf("PT%d" % i)) for i in range(5)]
    PTr = Rot(PTs)
    OSs = [(ar.alloc([512], F32, "OS%d" % i), Buf("OS%d" % i)) for i in range(2)]
    OSr = Rot(OSs)

    for i in range(NT):
        bk, bb = PA()
        for kk in range(8):
            s.mm(bk[:, 0:416], s.XT[:, kk, i * 128:(i + 1) * 128], W0v[:, kk, :], kk == 0, kk == 7,
                 [s.XTb[i // 4], W0b], [bb], kk == 7)
        s.act(sq[:, 0:384], bk[:, 0:384], AF.Square, [bb], [sqb])
        s.red(ssq[:, i:i + 1], sq[:, 0:256], ALU.add, [sqb], [ssb])
        s.red(sskv[:, i:i + 1], sq[:, 256:384], ALU.add, [sqb], [ssb])
        s.cp(krr[:, i, :], bk[:, 384:416], [bb], [krrb])
    if s.stop_after == "mla_tm":
        return
    for j in range(3):
        for g in range(4):
            bk, bb = PA()
            for kk in range(8):
                s.mm(bk[:, :], W0v[:, kk, j * 128:(j + 1) * 128], s.XT[:, kk, g * 512:(g + 1) * 512],
                     kk == 0, kk == 7, [s.XTb[g], W0b], [bb], kk == 7)
            s.act(cT[:, j, g * 512:(g + 1) * 512], bk[:, :], AF.Copy, [bb], [cTb])
    if s.stop_after == "mla_fm":
        return
    s.act(rq, ssq, AF.Sqrt, [ssb], [rb_], scale=96.0 / 256.0, bias=96e-6)
    s.act(rkv, sskv, AF.Sqrt, [ssb], [rb_], scale=1.0 / 128.0, bias=1e-6)
    s.recip(rq, rq, [rb_], [rb_])
    s.recip(rkv, rkv, [rb_], [rb_])
    rope_tm(s, krr, krb, ta, tb2, krrb, krbb)
    for v_ in range(2):
        s.memset(VA[v_][:, :, 64:65], 1.0, [VAb[v_]])

    if s.stop_after == "mla_r":
        return

    def prep_chunks(h):
        p = h % 2
        ch = []

        def q_group(i4):
            bk, bb = PC()
            for j in range(4):
                i = i4 * 4 + j
                for c in range(2):
                    s.mm(bk[:, j * 96:(j + 1) * 96], cT[:, c, i * 128:(i + 1) * 128],
                         Wuq[:, c, h * 96:(h + 1) * 96], c == 0, c == 1, [cTb, W1b], [bb],
                         j == 3 and c == 1)
            bkv = bk[:, 0:384].rearrange("p (a b) -> p a b", a=4)
            rq4 = rq[:, i4 * 4:(i4 + 1) * 4].rearrange("p (a b) -> p a b", b=1)
            s.tt(qhb[:, i4 * 4:(i4 + 1) * 4, 0:64], bkv[:, :, 0:64], bc(rq4, [128, 4, 64]), ALU.mult,
                 [bb, rb_], [qhbb])
            s.tt(qr[:, i4 * 4:(i4 + 1) * 4, :], bkv[:, :, 64:96], bc(rq4, [128, 4, 32]), ALU.mult,
                 [bb, rb_], [qrb])

        def kv_group(i4):
            bk, bb = PC()
            for j in range(4):
                i = i4 * 4 + j
                s.mm(bk[:, j * 128:(j + 1) * 128], cT[:, 2, i * 128:(i + 1) * 128],
                     Wkv[:, h * 128:(h + 1) * 128], True, True, [cTb, W1b], [bb], j == 3)
            bkv = bk[:, :].rearrange("p (a b) -> p a b", a=4)
            r4 = rkv[:, i4 * 4:(i4 + 1) * 4].rearrange("p (a b) -> p a b", b=1)
            s.tt(khb[:, i4 * 4:(i4 + 1) * 4, 0:64], bkv[:, :, 0:64], bc(r4, [128, 4, 64]), ALU.mult,
                 [bb, rb_], [khbb])
            s.tt(VA[p][:, i4 * 4:(i4 + 1) * 4, 0:64], bkv[:, :, 64:128], bc(r4, [128, 4, 64]), ALU.mult,
                 [bb, rb_], [VAb[p]])

        def tr_group(src, srcb, dst, dstb, i4):
            bk, bb = PC()
            pb = bk[:, 0:256].bitcast(BF16)
            for j in range(4):
                i = i4 * 4 + j
                s.tr(pb[0:96, j * 128:(j + 1) * 128], src[:, i, :], s.identB, [srcb, s.CBb], [bb], j == 3)
            s.cp(dst[0:96, i4 * 512:(i4 + 1) * 512], pb[0:96, :], [bb], [dstb])

        for i4 in range(4):
            ch.append(lambda i4=i4: q_group(i4))
        ch.append(lambda: rope_tm(s, qr, qhb[:, :, 64:96], ta, tb2, qrb, qhbb))
        for i4 in range(4):
            ch.append(lambda i4=i4: kv_group(i4))
        ch.append(lambda: s.cp(khb[:, :, 64:96], krb, [krbb], [khbb]))
        for i4 in range(4):
            ch.append(lambda i4=i4: tr_group(qhb, qhbb, QT[p], QTb[p], i4))
        for i4 in range(4):
            ch.append(lambda i4=i4: tr_group(khb, khbb, KT[p], KTb[p], i4))
        return ch

    LOOK = 3
    pend = []

    fin_q = []

    def fin_tick(flush=False):
        for it in fin_q:
            it[0] -= 1
        while fin_q and (flush or fin_q[0][0] <= 0):
            fin_q.pop(0)[1]()

    def attn_finish(h, qc, ob, obb):
        os_, osb = OSr()
        s.cp(os_[0:65, :], ob[0:65, :], [obb], [osb])
        s.act(os_[64:65, :], os_[64:65, :], AF.Ln, [osb], [osb])
        s.act(os_[64:65, :], os_[64:65, :], AF.Exp, [osb], [osb], scale=-1.0)
        fin_q.append([5, lambda: attn_finish2(h, qc, os_, osb)])

    def attn_finish2(h, qc, os_, osb):
        rbk, rbb = PC()
        s.mm(rbk[0:64, :], s.CF[64:65, C_ONE:C_ONE + 64], os_[64:65, :], True, True,
             [osb, s.CFb], [rbb], True)
        r0 = (h % 2) * 64
        s.tt(YA[r0:r0 + 64, h // 2, qc * 512:(qc + 1) * 512], os_[0:64, :], rbk[0:64, :], ALU.mult,
             [osb, rbb], [YAb])

    def pv_step(item):
        (h, qc, kt, pt, ptb, ob, obb) = item
        p = h % 2
        s.mm(ob[0:65, :], VA[p][:, kt, :], pt, kt == 0, kt == 15, [ptb, VAb[p]], [obb], kt == 15)
        if kt == 15:
            attn_finish(h, qc, ob, obb)

    def attn(h, chunks):
        p = h % 2
        n = 0
        for qc in range(4):
            ob, obb = PB()
            for kt in range(16):
                n += 1
                if chunks and n >= 5 and n % 2 == 0:
                    chunks.pop(0)()
                sb_, sbb = PA()
                s.mm(sb_[:, :], KT[p][0:96, kt * 128:(kt + 1) * 128], QT[p][0:96, qc * 512:(qc + 1) * 512],
                     True, True, [KTb[p], QTb[p]], [sbb], True)
                pt, ptb = PTr()
                s.act(pt, sb_[:, :], AF.Exp, [sbb], [ptb])
                pend.append((h, qc, kt, pt, ptb, ob, obb))
                if len(pend) > LOOK:
                    pv_step(pend.pop(0))
                fin_tick()

    for c_ in prep_chunks(0):
        c_()
    wo_pre = outproj_load(s, l, 4, 0)
    for h in range(8):
        chunks = prep_chunks(h + 1) if h + 1 < 8 else []
        attn(h, chunks)
        while chunks:
            chunks.pop(0)()
    while pend:
        pv_step(pend.pop(0))
    fin_tick(flush=True)
    preload_mlstm_a(s, l)
    s.tap("ya", YA, [YAb])
    outproj_partial(s, l, YA, YAb, 4, 0, True, pre=wo_pre)


def token_decay_arrays(s, a_all, u_all, ab):
    ar = s.arena
    cs = ar.alloc([NT, 2, 4], F32, "cs")
    nb = ar.alloc([NT, 2, 4], F32, "nb")
    ecs = ar.alloc([NT, 2, 4], F32, "ecs")
    ws = ar.alloc([NT, 2, 4], F32, "ws")
    gst = ar.alloc([NT, 2, 4], F32, "gst")
    P1 = s.pool([0, 1])
    U = s.CF[:, C_U:C_U + 128]
    L = s.CF[:, C_L:C_L + 128]
    ones = s.CF[:, C_ONE:C_ONE + 128]
    bk, bb = P1()
    for d, M in ((0, U), (1, L)):
        s.mm(bk[:, d * 64:(d + 1) * 64], M, a_all[:, :, d, :], True, True, [ab, s.CFb], [bb], d == 1)
    for d in range(2):
        s.cp(cs[:, :, d, :], bk[:, d * 64:(d + 1) * 64].rearrange("p (a b) -> p a b", a=NT), [bb], [ab])
    bk2, bb2 = P1()
    s.mm(bk2[:, 0:128], ones, a_all.rearrange("p a b c -> p (a b c)"), True, True, [ab, s.CFb], [bb2], True)
    tot = bk2[:, 0:128].rearrange("p (a b c) -> p a b c", a=NT, b=2)
    s.act(gst, tot, AF.Exp, [bb2], [ab])
    s.tt(nb, u_all, cs, ALU.subtract, [ab], [ab])
    s.tt(ws, tot, nb, ALU.add, [bb2, ab], [ab])
    s.act(ws, ws, AF.Exp, [ab], [ab])
    s.act(ecs, cs, AF.Exp, [ab], [ab])
    return dict(cs=cs, nb=nb, ecs=ecs, ws=ws, gst=gst)


def decay_E(s, i, a_all, nb, ab, Ebuf, Ebb, banks2):
    U = s.CF[:, C_U:C_U + 128]
    L = s.CF[:, C_L:C_L + 128]
    for d in range(2):
        bk, bb = banks2[d]
        M = U if d == 0 else L
        mk = s.CB[:, C_MF:C_MF + 128] if d == 0 else s.CB[:, C_MB:C_MB + 128]
        for h in range(4):
            s.mm(bk[:, h * 128:(h + 1) * 128], bc(a_all[:, i, d, h:h + 1], [128, 128]), M, True, False,
                 [ab, s.CFb], [bb], False)
            s.mm(bk[:, h * 128:(h + 1) * 128], s.identB, mk, False, True, [s.CBb], [bb], h == 3)
        for h in range(4):
            s.act(Ebuf[:, d, h, :], bk[:, h * 128:(h + 1) * 128], AF.Exp, [bb, ab], [Ebb],
                  bias=nb[:, i, d, h:h + 1])


def phase_mlstm(st, l):
    s = NS(st) if isinstance(st, dict) else st
    k, ar = s.k, s.arena
    k.barrier()
    ar.reset()
    s.sc_reset()
    PA = s.pool([0, 1, 2, 3])
    wsrc = s.w_in[l].rearrange("(k p) c -> p k c", p=128)
    if "mlstm_a" not in s.pre:
        preload_mlstm_a(s, l)
    Wav, Wab = s.pre.pop("mlstm_a")
    Wb, Wbb, Wbs = s.wslot()
    Wbv = Wb[:, 0:8 * 528].rearrange("p (k c) -> p k c", k=8)
    k.dma("pool", Wbv, wsrc[:, :, 928:1456], Wbs, writes=[Wbb])
    mqT = ar.alloc([2, S], BF16, "mqT"); mkT = ar.alloc([2, S], BF16, "mkT"); fmb = Buf("mfm")
    mkTM = ar.alloc([NT, 4, 64], BF16, "mkTM"); mvA = ar.alloc([NT, 4, 65], BF16, "mvA"); tmb = Buf("mtm")
    YB = ar.alloc([2, S], BF16, "YB"); YBb = Buf("YB")
    gts = ar.alloc([NT, 2, 2, 4], F32, "gts")
    a_all = ar.alloc([NT, 2, 4], F32, "a_all"); u_all = ar.alloc([NT, 2, 4], F32, "u_all"); ab = Buf("mtok")
    Fst = ar.alloc([NT, 2, 2, 65], BF16, "F"); Fb = Buf("F")
    Sst = ar.alloc([2, 2, 65], F32, "S"); Sb = Buf("S")
    Kw = [ar.alloc([4, 64], BF16, "Kw%d" % i) for i in range(2)]; Kwb = [Buf("Kw0"), Buf("Kw1")]
    Es = [ar.alloc([2, 4, 128], BF16, "E%d" % i) for i in range(2)]; Ebs = [Buf("E0"), Buf("E1")]
    MTs = [ar.alloc([2, 4, 128], BF16, "MT%d" % i) for i in range(2)]; MTbs = [Buf("MT0"), Buf("MT1")]
    Rall = ar.alloc([2, 4, 65], F32, "Rall"); Rt = [Rall[:, 0, :, :], Rall[:, 1, :, :]]; Rb = Buf("R")
    prod = ar.alloc([2, 4, 64], F32, "prod")
    cen, sq_ = prod[:, 0, :, :], prod[:, 1, :, :]
    tmp = ar.alloc([4, 65], F32, "tmp")
    den = s.sc_alloc(8); hs = ar.alloc([4, 64], F32, "hs"); hb_ = Buf("hs")

    st4 = s.sc_alloc(4); st4b = s.sc_alloc(4)
    sgos = [ar.alloc([256], F32, "sgo%d" % i) for i in range(2)]; sgobs = [Buf("sgo0"), Buf("sgo1")]
    ybs = [ar.alloc([256], BF16, "yb%d" % i) for i in range(3)]; ybbs = [Buf("yb%d" % i) for i in range(3)]
    s.memset(mvA[:, :, :, 64:65], 1.0, [tmb])
    for i in range(NT):
        bk, bb = PA()
        for kk in range(8):
            s.mm(bk[:, 0:256], s.XT[:, kk, i * 128:(i + 1) * 128], Wav[:, kk, 256:512], kk == 0, kk == 7,
                 [s.XTb[i // 4], Wab], [bb], kk == 7)
        s.act(mkTM[:, i, :, :], bk[:, 0:256].rearrange("p (a b) -> p a b", a=4), AF.Copy, [bb], [tmb], scale=0.125)
        bk, bb = PA()
        for kk in range(8):
            s.mm(bk[:, 0:256], s.XT[:, kk, i * 128:(i + 1) * 128], Wbv[:, kk, 0:256], kk == 0, kk == 7,
                 [s.XTb[i // 4], Wbb], [bb], False)
        for kk in range(8):
            s.mm(bk[:, 256:272], s.XT[:, kk, i * 128:(i + 1) * 128], Wbv[:, kk, 512:528], kk == 0, kk == 7,
                 [s.XTb[i // 4], Wbb], [bb], kk == 7)
        s.act(mvA[:, i, :, 0:64], bk[:, 0:256].rearrange("p (a b) -> p a b", a=4), AF.Copy, [bb], [tmb])
        s.tt(gts[:, i, :, :, :].rearrange("p a b c -> p (a b c)"), bk[:, 256:272], s.PRM[:, P_GB:P_GB + 16],
             ALU.add, [bb, s.PRMb], [ab])
    for j in range(4):
        for g in range(4):
            bk, bb = PA()
            for kk in range(8):
                s.mm(bk[:, :], Wav[:, kk, j * 128:(j + 1) * 128], s.XT[:, kk, g * 512:(g + 1) * 512],
                     kk == 0, kk == 7, [s.XTb[g], Wab], [bb], kk == 7)
            if j < 2:
                s.act(mqT[:, j, g * 512:(g + 1) * 512], bk[:, :], AF.Copy, [bb], [fmb])
            else:
                s.act(mkT[:, j - 2, g * 512:(g + 1) * 512], bk[:, :], AF.Copy, [bb], [fmb], scale=0.125)
    wo_pre = outproj_load(s, l, 2, 512)
    s.cp(u_all, gts[:, :, :, 0, :], [ab], [ab])
    s.act(a_all, gts[:, :, :, 1, :], AF.Exp, [ab], [ab], scale=-1.0)
    s.act(a_all, a_all, AF.Ln, [ab], [ab], bias=1.0)
    s.ts(a_all, a_all, -1.0, None, ALU.mult, None, [ab], [ab])
    if s.stop_after == "mlstm_a":
        return
    T = token_decay_arrays(s, a_all, u_all, ab)
    cs, nb, ecs, ws, gst = T["cs"], T["nb"], T["ecs"], T["ws"], T["gst"]
    if s.stop_after == "mlstm_b":
        return
    s.memset(Sst, 0.0, [Sb])
    P23 = [s.pool([2, 4]), s.pool([3, 5])]
    for c in range(NT):
        for d in range(2):
            t_ = c if d == 0 else NT - 1 - c
            s.tt(Kw[d], mkTM[:, t_, :, :], bc(ws[:, t_, d, :].rearrange("p (a b) -> p a b", b=1), [128, 4, 64]),
                 ALU.mult, [tmb, ab], [Kwb[d]])
            bk, bb = P23[d]()
            for c2 in range(2):
                s.mm(bk[:, c2 * 130:(c2 + 1) * 130], Kw[d][:, 2 * c2:2 * c2 + 2, :].rearrange("p a b -> p (a b)"),
                     mvA[:, t_, 2 * c2:2 * c2 + 2, :].rearrange("p a b -> p (a b)"), True, True,
                     [Kwb[d], tmb], [bb], c2 == 1)
            s.act(Fst[:, t_, d, :, :], Sst[:, d, :, :], AF.Copy, [Sb], [Fb])
            bv = bk[:, 0:260].rearrange("p (a b) -> p a b", a=2)
            for hp in range(2):
                r0, r1 = hp * 64, hp * 64 + 64
                gsel = gst[r0:r1, t_, d, :].rearrange("p (a b) -> p a b", b=2)[:, :, hp:hp + 1]
                s.tt(Sst[r0:r1, d, :, :], Sst[r0:r1, d, :, :], bc(gsel, [64, 2, 65]), ALU.mult, [Sb, ab], [Sb])
                s.tt(Sst[r0:r1, d, :, :], Sst[r0:r1, d, :, :], bv[r0:r1, :, hp * 65:(hp + 1) * 65], ALU.add,
                     [Sb, bb], [Sb])
    if s.stop_after == "mlstm_c":
        return
    bE = [(s.pool([0])()), (s.pool([1])())]
    bS = s.pool([2])()
    bI = [s.pool([3])(), s.pool([4])()]
    bJ = [s.pool([5])(), s.pool([6])()]
    bT = s.pool([7])()
    def stage_a(i):
        sl = slice(i * 128, (i + 1) * 128)
        E, Eb, MT, MTb, sgo, sgob = Es[i % 2], Ebs[i % 2], MTs[i % 2], MTbs[i % 2], sgos[i % 2], sgobs[i % 2]
        decay_E(s, i, a_all, nb, ab, E, Eb, bE)
        for h in range(4):
            r0 = (h % 2) * 64
            s.mm(bS[0][:, h * 128:(h + 1) * 128], mkT[r0:r0 + 64, h // 2, sl], mqT[r0:r0 + 64, h // 2, sl],
                 True, True, [fmb], [bS[1]], True, serial=True)
        for d in range(2):
            s.tt(MT[:, d, :, :], bS[0][:, :].rearrange("p (a b) -> p a b", a=4), E[:, d, :, :], ALU.mult,
                 [bS[1], Eb], [MTb])
        bk, bb = bT
        for kk in range(8):
            s.mm(bk[:, 0:256], s.XT[:, kk, sl], Wbv[:, kk, 256:512], kk == 0, kk == 7,
                 [s.XTb[i // 4], Wbb], [bb], kk == 7)
        s.act(sgo, bk[:, 0:256], AF.Sigmoid, [bb], [sgob])

    def stage_b(i):
        sl = slice(i * 128, (i + 1) * 128)
        yb, ybb = ybs[i % 3], ybbs[i % 3]
        E, Eb, MT, MTb, sgo, sgob = Es[i % 2], Ebs[i % 2], MTs[i % 2], MTbs[i % 2], sgos[i % 2], sgobs[i % 2]
        for d in range(2):
            for h in range(4):
                r0 = (h % 2) * 64
                s.mm(bJ[d][0][:, h * 65:(h + 1) * 65], mqT[r0:r0 + 64, h // 2, sl], Fst[r0:r0 + 64, i, d, h // 2, :],
                     True, True, [fmb, Fb], [bJ[d][1]], True, serial=True)
            for h in range(4):
                s.mm(bI[d][0][:, h * 65:(h + 1) * 65], MT[:, d, h, :], mvA[:, i, h, :], True, True,
                     [MTb, tmb], [bI[d][1]], h == 3)
            s.tt(tmp, bJ[d][0][:, 0:260].rearrange("p (a b) -> p a b", a=4),
                 bc(ecs[:, i, d, :].rearrange("p (a b) -> p a b", b=1), [128, 4, 65]), ALU.mult,
                 [bJ[d][1], ab], [Rb])
            s.tt(Rt[d], bI[d][0][:, 0:260].rearrange("p (a b) -> p a b", a=4), tmp, ALU.add, [bI[d][1], Rb], [Rb])
        d8 = den.rearrange("p (a b) -> p a b", a=2)
        s.ts(d8, Rall[:, :, :, 64], -1.0, None, ALU.mult, None, [Rb], [Rb])
        s.tt(d8, d8, Rall[:, :, :, 64], ALU.max, [Rb], [Rb])
        s.ts(d8, d8, 1.0, None, ALU.max, None, [Rb], [Rb])
        s.recip(den, den, [Rb], [Rb])
        s.tt(prod, Rall[:, :, :, 0:64], bc(den.rearrange("p (a b c) -> p a b c", a=2, c=1), [128, 2, 4, 64]),
             ALU.mult, [Rb], [hb_])
        s.tt(hs, prod[:, 0, :, :], prod[:, 1, :, :], ALU.add, [hb_], [hb_])
        s.red(st4, hs, ALU.add, [hb_], [hb_])
        s.ts(st4, st4, 1.0 / 64.0, None, ALU.mult, None, [hb_], [hb_])
        s.tt(cen, hs, bc(st4.rearrange("p (a b) -> p a b", b=1), [128, 4, 64]), ALU.subtract, [hb_], [hb_])
        s.tt(sq_, cen, cen, ALU.mult, [hb_], [hb_])
        s.red(st4b, sq_, ALU.add, [hb_], [hb_])
        s.act(st4b, st4b, AF.Ln, [hb_], [hb_], scale=1.0 / 64.0, bias=1e-5)
        s.act(st4b, st4b, AF.Exp, [hb_], [hb_], scale=-0.5)
        s.tt(cen, cen, bc(st4b.rearrange("p (a b) -> p a b", b=1), [128, 4, 64]), ALU.mult, [hb_], [hb_])
        cf = cen.rearrange("p a b -> p (a b)")
        s.tt(cf, cf, s.PRM[:, P_MN:P_MN + 256], ALU.mult, [hb_, s.PRMb], [hb_])
        s.tt(yb, cf, sgo, ALU.mult, [hb_, sgob], [ybb])

    def stage_c(i):
        sl = slice(i * 128, (i + 1) * 128)
        yb, ybb = ybs[i % 3], ybbs[i % 3]
        bk, bb = bT
        pb = bk[:, 0:256].bitcast(BF16)
        for c in range(2):
            s.tr(pb[:, c * 128:(c + 1) * 128], yb[:, c * 128:(c + 1) * 128], s.identB, [ybb, s.CBb], [bb], c == 1)
        s.act(YB[:, :, sl], pb[:, 0:256].rearrange("p (a b) -> p a b", a=2), AF.Copy, [bb], [YBb])

    stage_a(0)
    for i in range(NT + 1):
        if i + 1 < NT:
            stage_a(i + 1)
        if i < NT:
            stage_b(i)
        if i >= 1:
            stage_c(i - 1)
    s.tap("yb", YB, [YBb])
    preload_ssd_x(s, l)
    outproj_partial(s, l, YB, YBb, 2, 512, False, pre=wo_pre)


def phase_ssd(st, l):
    s = NS(st) if isinstance(st, dict) else st
    k, ar = s.k, s.arena
    k.barrier()
    ar.reset()
    s.sc_reset()
    PA = s.pool([0, 1, 2, 3])
    PT = s.pool([6, 7])
    wsrc = s.w_in[l].rearrange("(k p) c -> p k c", p=128)
    if "ssd_x" not in s.pre:
        preload_ssd_x(s, l)
    Wxv, Wxb = s.pre.pop("ssd_x")
    Wz, Wzb, Wzs = s.wslot()
    Wzv = Wz[:, 0:8 * 264].rearrange("p (k c) -> p k c", k=8)
    k.dma("pool", Wzv[:, :, 0:256], wsrc[:, :, 1456:1712], Wzs, writes=[Wzb])
    k.dma("pool", Wzv[:, :, 256:264], wsrc[:, :, 2480:2488], Wzs, writes=[Wzb])
    BCT = ar.alloc([4, S], BF16, "BCT"); bcb = Buf("BCT")
    BTM = ar.alloc([NT, 2, 128], BF16, "BTM"); btb = Buf("BTM")
    xTM = ar.alloc([NT, 4, 64], BF16, "xTM"); tmb = Buf("stm")
    YC = BTM.rearrange("p a b c -> p (a b c)").rearrange("p (a b) -> p a b", a=2); YCb = btb
    dtr = ar.alloc([NT, 2, 4], F32, "dtr")
    a_all = ar.alloc([NT, 2, 4], F32, "a_all"); u_all = ar.alloc([NT, 2, 4], F32, "u_all"); ab = Buf("stok")
    aneg = s.sc_alloc(8)
    T = None
    mark = ar.off
    bk, bb = PA()
    for i in range(NT):
        for kk in range(8):
            s.mm(bk[:, i * 8:(i + 1) * 8], s.XT[:, kk, i * 128:(i + 1) * 128], Wzv[:, kk, 256:264], kk == 0, kk == 7,
                 [s.XTb[i // 4], Wzb], [bb], i == NT - 1 and kk == 7)
    dflat = dtr.rearrange("p a b c -> p a (b c)")
    s.tt(dflat, bk[:, 0:128].rearrange("p (a b) -> p a b", a=NT),
         bc(s.PRM[:, P_DTB:P_DTB + 8].rearrange("p (a b) -> p a b", a=1), [128, NT, 8]), ALU.add,
         [bb, s.PRMb], [ab])
    s.act(dtr, dtr, AF.Exp, [ab], [ab])
    s.act(dtr, dtr, AF.Ln, [ab], [ab], bias=1.0)
    s.act(u_all, dtr, AF.Ln, [ab], [ab])
    s.act(aneg, s.PRM[:, P_AL:P_AL + 8], AF.Exp, [s.PRMb], [ab])
    s.ts(aneg, aneg, -1.0, None, ALU.mult, None, [ab], [ab])
    s.tt(a_all.rearrange("p a b c -> p a (b c)"), dflat,
         bc(aneg.rearrange("p (a b) -> p a b", a=1), [128, NT, 8]), ALU.mult, [ab], [ab])
    cin = [ar.alloc([2052], BF16, "cin%d" % i) for i in range(2)]; cinb = [Buf("cin0"), Buf("cin1")]
    acc = [ar.alloc([512], F32, "acc%d" % i) for i in range(2)]; accb = [Buf("acc0"), Buf("acc1")]
    xfm = [ar.alloc([S], BF16, "xfm%d" % i) for i in range(2)]; xfmb = [Buf("xfm0"), Buf("xfm1")]
    for p_ in range(2):
        s.memset(cin[p_][:, 0:2], 0.0, [cinb[p_]])
        s.memset(cin[p_][:, 2050:2052], 0.0, [cinb[p_]])
    accr = Rot([0, 1])
    for j in range(6):
        p_ = j % 2
        for g in range(4):
            bk, bb = PA()
            for kk in range(8):
                s.mm(bk[:, :], Wxv[:, kk, j * 128:(j + 1) * 128], s.XT[:, kk, g * 512:(g + 1) * 512],
                     kk == 0, kk == 7, [s.XTb[g], Wxb], [bb], kk == 7)
            s.act(cin[p_][:, 2 + g * 512:2 + (g + 1) * 512], bk[:, :], AF.Copy, [bb], [cinb[p_]])
        if j < 2:
            dst, dstb = xfm[p_], xfmb[p_]
        else:
            dst, dstb = BCT[:, j - 2, :], bcb
        for g in range(4):
            a_ = accr()
            cw = lambda jj: s.PRM[:, P_CW + j * 5 + jj:P_CW + j * 5 + jj + 1]
            s.ts(acc[a_], cin[p_][:, g * 512:g * 512 + 512], cw(0), None, ALU.mult, None,
                 [cinb[p_], s.PRMb], [accb[a_]])
            for jj in range(1, 5):
                s.stt(acc[a_], cin[p_][:, g * 512 + jj:g * 512 + jj + 512], cw(jj), acc[a_], ALU.mult, ALU.add,
                      [cinb[p_], s.PRMb, accb[a_]], [accb[a_]])
            s.act(dst[:, g * 512:(g + 1) * 512], acc[a_], AF.Silu, [accb[a_], s.PRMb], [dstb],
                  bias=s.PRM[:, P_CB + j:P_CB + j + 1])
        if j < 4:
            src = xfm[p_] if j < 2 else BCT[:, j - 2, :]
            srcb = xfmb[p_] if j < 2 else bcb
            for i4 in range(4):
                bk, bb = PT()
                pb = bk[:, 0:256].bitcast(BF16)
                for q in range(4):
                    i = i4 * 4 + q
                    s.tr(pb[:, q * 128:(q + 1) * 128], src[:, i * 128:(i + 1) * 128], s.identB, [srcb, s.CBb], [bb], q == 3)
                if j < 2:
                    s.act(xTM[:, i4 * 4:(i4 + 1) * 4, 2 * j:2 * j + 2, :],
                          pb[:, :].rearrange("p (a b c) -> p a b c", a=4, b=2), AF.Copy, [bb], [tmb])
                else:
                    s.act(BTM[:, i4 * 4:(i4 + 1) * 4, j - 2, :], pb[:, :].rearrange("p (a b) -> p a b", a=4),
                          AF.Copy, [bb], [btb])
    s.tap("bct", BCT, [bcb])
    wo_pre = outproj_load(s, l, 2, 768)
    k.barrier()
    ar.off = mark
    T = token_decay_arrays(s, a_all, u_all, ab)
    cs, nb, ecs, ws, gst = T["cs"], T["nb"], T["ecs"], T["ws"], T["gst"]
    Fst = ar.alloc([NT, 2, 4, 64], BF16, "F"); Fb = Buf("F")
    Sst = ar.alloc([2, 4, 64], F32, "S"); Sb = Buf("S")
    Kw = [ar.alloc([4, 128], BF16, "Kw%d" % i) for i in range(2)]; Kwb = [Buf("Kw0"), Buf("Kw1")]
    Es = [ar.alloc([2, 4, 128], BF16, "E%d" % i) for i in range(2)]; Ebs = [Buf("E0"), Buf("E1")]
    MTs = [ar.alloc([2, 4, 128], BF16, "MT%d" % i) for i in range(2)]; MTbs = [Buf("MT0"), Buf("MT1")]
    Rt = [ar.alloc([4, 64], F32, "R%d" % i) for i in range(2)]; Rb = Buf("R")
    tmp = ar.alloc([4, 64], F32, "tmp")
    yt = ar.alloc([4, 64], F32, "yt"); ytb = Buf("yt")
    sq_ = tmp
    zss = [ar.alloc([256], F32, "zs%d" % i) for i in range(2)]; zsbs = [Buf("zs0"), Buf("zs1")]
    ybs = [ar.alloc([256], BF16, "yb%d" % i) for i in range(3)]; ybbs = [Buf("yb%d" % i) for i in range(3)]
    ss2 = s.sc_alloc(2)
    s.memset(Sst, 0.0, [Sb])
    P23 = [s.pool([2, 4]), s.pool([3, 5])]
    for c in range(NT):
        for d in range(2):
            t_ = c if d == 0 else NT - 1 - c
            s.tt(Kw[d].rearrange("p (g e) n -> p g e n", g=2),
                 bc(BTM[:, t_, :, :].rearrange("p g (o n) -> p g o n", o=1), [128, 2, 2, 128]),
                 bc(ws[:, t_, d, :].rearrange("p (g e o) -> p g e o", g=2, o=1), [128, 2, 2, 128]),
                 ALU.mult, [btb, ab], [Kwb[d]])
            bk, bb = P23[d]()
            for h in range(4):
                s.mm(bk[:, h * 64:(h + 1) * 64], Kw[d][:, h, :], xTM[:, t_, h, :], True, True,
                     [Kwb[d], tmb], [bb], h == 3)
            s.act(Fst[:, t_, d, :, :], Sst[:, d, :, :], AF.Copy, [Sb], [Fb])
            s.tt(Sst[:, d, :, :], Sst[:, d, :, :],
                 bc(gst[:, t_, d, :].rearrange("p (a b) -> p a b", b=1), [128, 4, 64]), ALU.mult, [Sb, ab], [Sb])
            s.tt(Sst[:, d, :, :], Sst[:, d, :, :], bk[:, 0:256].rearrange("p (a b) -> p a b", a=4), ALU.add,
                 [Sb, bb], [Sb])
    bE = [(s.pool([0])()), (s.pool([1])())]
    bS = s.pool([2])()
    bI = [s.pool([3])(), s.pool([4])()]
    bJ = [s.pool([5])(), s.pool([6])()]
    bT = s.pool([7])()
    def stage_a(i):
        sl = slice(i * 128, (i + 1) * 128)
        E, Eb, MT, MTb, zs, zsb = Es[i % 2], Ebs[i % 2], MTs[i % 2], MTbs[i % 2], zss[i % 2], zsbs[i % 2]
        decay_E(s, i, a_all, nb, ab, E, Eb, bE)
        for g in range(2):
            s.mm(bS[0][:, g * 128:(g + 1) * 128], BCT[:, g, sl], BCT[:, 2 + g, sl], True, True, [bcb], [bS[1]], g == 1)
        for d in range(2):
            s.tt(MT[:, d, :, :].rearrange("p (g e) n -> p g e n", g=2),
                 bc(bS[0][:, 0:256].rearrange("p (g o n) -> p g o n", g=2, o=1), [128, 2, 2, 128]),
                 E[:, d, :, :].rearrange("p (g e) n -> p g e n", g=2), ALU.mult, [bS[1], Eb], [MTb])
        bk, bb = bT
        for kk in range(8):
            s.mm(bk[:, 0:256], s.XT[:, kk, sl], Wzv[:, kk, 0:256], kk == 0, kk == 7,
                 [s.XTb[i // 4], Wzb], [bb], kk == 7)
        s.act(zs, bk[:, 0:256], AF.Silu, [bb], [zsb])

    def stage_b(i):
        sl = slice(i * 128, (i + 1) * 128)
        yb, ybb = ybs[i % 3], ybbs[i % 3]
        E, Eb, MT, MTb, zs, zsb = Es[i % 2], Ebs[i % 2], MTs[i % 2], MTbs[i % 2], zss[i % 2], zsbs[i % 2]
        for d in range(2):
            for h in range(4):
                s.mm(bJ[d][0][:, h * 64:(h + 1) * 64], BCT[:, 2 + h // 2, sl], Fst[:, i, d, h, :], True, True,
                     [bcb, Fb], [bJ[d][1]], h == 3)
            for h in range(4):
                s.mm(bI[d][0][:, h * 64:(h + 1) * 64], MT[:, d, h, :], xTM[:, i, h, :], True, True,
                     [MTb, tmb], [bI[d][1]], h == 3)
            s.tt(tmp, bJ[d][0][:, 0:256].rearrange("p (a b) -> p a b", a=4),
                 bc(ecs[:, i, d, :].rearrange("p (a b) -> p a b", b=1), [128, 4, 64]), ALU.mult,
                 [bJ[d][1], ab], [Rb])
            s.tt(Rt[d], bI[d][0][:, 0:256].rearrange("p (a b) -> p a b", a=4), tmp, ALU.add, [bI[d][1], Rb], [Rb])
        s.tt(yt, Rt[0], Rt[1], ALU.add, [Rb], [ytb])
        s.tt(sq_, xTM[:, i, :, :], bc(s.PRM[:, P_SD:P_SD + 4].rearrange("p (a b) -> p a b", b=1), [128, 4, 64]),
             ALU.mult, [tmb, s.PRMb], [ytb, Rb])
        s.tt(yt, yt, sq_, ALU.add, [ytb, Rb], [ytb])
        yf = yt.rearrange("p a b -> p (a b)")
        s.tt(yf, yf, zs, ALU.mult, [ytb, zsb], [ytb])
        s.tt(sq_, yt, yt, ALU.mult, [ytb], [ytb, Rb])
        s.red(ss2, sq_.rearrange("p (g e) n -> p g (e n)", g=2), ALU.add, [ytb, Rb], [ytb])
        s.act(ss2, ss2, AF.Ln, [ytb], [ytb], scale=1.0 / 128.0, bias=1e-6)
        s.act(ss2, ss2, AF.Exp, [ytb], [ytb], scale=-0.5)
        s.tt(yt.rearrange("p (g e) n -> p g (e n)", g=2), yt.rearrange("p (g e) n -> p g (e n)", g=2),
             bc(ss2.rearrange("p (a b) -> p a b", b=1), [128, 2, 128]), ALU.mult, [ytb], [ytb])
        s.tt(yb, yf, s.PRM[:, P_SN:P_SN + 256], ALU.mult, [ytb, s.PRMb], [ybb])

    def stage_c(i):
        sl = slice(i * 128, (i + 1) * 128)
        yb, ybb = ybs[i % 3], ybbs[i % 3]
        bk, bb = bT
        pb = bk[:, 0:256].bitcast(BF16)
        for c in range(2):
            s.tr(pb[:, c * 128:(c + 1) * 128], yb[:, c * 128:(c + 1) * 128], s.identB, [ybb, s.CBb], [bb], c == 1)
        s.act(YC[:, :, sl], pb[:, 0:256].rearrange("p (a b) -> p a b", a=2), AF.Copy, [bb], [YCb])

    stage_a(0)
    for i in range(NT + 1):
        if i + 1 < NT:
            stage_a(i + 1)
        if i < NT:
            stage_b(i)
        if i >= 1:
            stage_c(i - 1)
    s.tap("yc", YC, [YCb])
    outproj_partial(s, l, YC, YCb, 2, 768, False, pre=wo_pre)


def layernorm_tiles(s, l, which, T1s, T1bs, LNP, LNPb, post):
    st6 = s.arena.alloc([2, 6], F32, "st6")
    mv = s.sc_alloc(2)
    sd = s.sc_alloc(1)
    lb = Buf("lnstat")
    def front(i):
        T1, T1b = T1s[i % 2], T1bs[i % 2]
        for hf in range(2):
            s.k.op("dve", lambda h: h.bn_stats(out=st6[:, hf, :], in_=s.X[:, i, hf * 512:(hf + 1) * 512]),
                   reads=[s.Xb[i]], writes=[lb])
        s.k.op("dve", lambda h: h.bn_aggr(out=mv, in_=st6.rearrange("p a b -> p (a b)")), reads=[lb], writes=[lb])
        s.act(sd, mv[:, 1:2], AF.Sqrt, [lb], [lb], scale=1.0, bias=1e-5)
        s.recip(sd, sd, [lb], [lb])
        s.ts(T1, s.X[:, i, :], mv[:, 0:1], sd, ALU.subtract, ALU.mult, [s.Xb[i], lb], [T1b])
        s.tt(T1, T1, LNP[:, 0, :], ALU.mult, [T1b, LNPb], [T1b])
        s.tt(T1, T1, LNP[:, 1, :], ALU.add, [T1b, LNPb], [T1b])

    front(0)
    for i in range(NT):
        if i + 1 < NT:
            front(i + 1)
        post(i, T1s[i % 2], T1bs[i % 2])


def phase_ln_router_moe(st, l, last):
    s = NS(st) if isinstance(st, dict) else st
    k, ar = s.k, s.arena
    k.barrier()
    ar.reset()
    s.sc_reset()
    s_l = s.s_l
    comb = ar.alloc([NT, 4, 4], F32, "comb"); combb = Buf("comb")
    mark = ar.off

    def load_gu(e):
        W, Wb, Ws = s.wslot()
        Wv = W[:, 0:4096].rearrange("p (k c) -> p k c", k=8)
        k.dma("pool", Wv[:, :, 0:256], s.d_wg[l, e].rearrange("(k p) f -> p k f", p=128), Ws, writes=[Wb])
        k.dma("pool", Wv[:, :, 256:512], s.d_wu[l, e].rearrange("(k p) f -> p k f", p=128), Ws, writes=[Wb])
        return Wv, Wb

    e0_pre = load_gu(0)
    LNP = ar.alloc([2, D], F32, "LNP"); LNPb = Buf("LNP")
    for q in range(2):
        k.dma("sp", LNP[:, q, :], s.dlnp[l, q:q + 1, :].to_broadcast([128, D]), s_l, writes=[LNPb])
    T1s = [ar.alloc([D], F32, "T1%d" % i) for i in range(2)]; T1bs = [Buf("T10"), Buf("T11")]
    x32s = [ar.alloc([8, 128], F32, "x32%d" % i) for i in range(2)]; x32bs = [Buf("x320"), Buf("x321")]
    RL = ar.alloc([NT, 20], F32, "RL"); RLb = Buf("RL")
    PR = s.pool([4, 5])

    def post1(i, T1, T1b):
        x32, x32b = x32s[i % 2], x32bs[i % 2]
        s.transpose_x_tile(i, T1, [T1b], dst32=x32, dst32b=x32b)
        s.act(s.X[:, i, :], T1, AF.Copy, [T1b], [s.Xb[i]], scale=ALPHA)
        bk, bb = PR()
        for c in range(8):
            s.mm(bk[:, 0:20], x32[:, c, :], s.PRM[:, P_RW + c * 20:P_RW + (c + 1) * 20], c == 0, c == 7,
                 [x32b, s.PRMb], [bb], c == 7)
        s.tt(RL[:, i, :], bk[:, 0:20], s.PRM[:, P_RB:P_RB + 20], ALU.add, [bb, s.PRMb], [RLb])

    layernorm_tiles(s, l, 0, T1s, T1bs, LNP, LNPb, post1)
    s.tap("x1", s.X[:, :, :], s.Xb)
    gl = RL[:, :, 0:4]
    el = RL[:, :, 4:20].rearrange("p t (g e) -> p t g e", g=4)
    A1 = lambda n: ar.alloc([NT, n], F32, "r%d" % n)
    gmax, gsum, gp, emax1, emax2, ssum, rg = A1(1), A1(1), A1(1), A1(1), A1(1), A1(1), A1(1)
    gsh, ohg, esel, esel2, m1, m2, ee = A1(4), A1(4), A1(4), A1(4), A1(4), A1(4), A1(4)
    t16 = ar.alloc([NT, 4, 4], F32, "t16")
    rb_ = Buf("route")
    b4 = lambda a: bc(a, [128, NT, 4])
    R_, W_ = [RLb, rb_], [rb_]
    s.red(gmax, gl, ALU.max, R_, W_)
    s.tt(gsh, gl, b4(gmax), ALU.subtract, R_, W_)
    s.act(gsh, gsh, AF.Exp, R_, W_)
    s.red(gsum, gsh, ALU.add, R_, W_)
    s.recip(gp, gsum, R_, W_)
    s.tt(ohg, gl, b4(gmax), ALU.is_equal, R_, W_)
    s.tt(t16, el, bc(ohg.rearrange("p t (g o) -> p t g o", o=1), [128, NT, 4, 4]), ALU.mult, R_, W_)
    s.red(esel, t16.rearrange("p t g e -> p t e g"), ALU.add, R_, W_)
    s.red(emax1, esel, ALU.max, R_, W_)
    s.tt(m1, esel, b4(emax1), ALU.is_equal, R_, W_)
    s.stt(esel2, m1, -1e30, esel, ALU.mult, ALU.add, R_, W_)
    s.red(emax2, esel2, ALU.max, R_, W_)
    s.tt(m2, esel2, b4(emax2), ALU.is_equal, R_, W_)
    s.tt(m1, m1, m2, ALU.add, R_, W_)
    s.tt(ee, esel, b4(emax1), ALU.subtract, R_, W_)
    s.act(ee, ee, AF.Exp, R_, W_)
    s.tt(ee, ee, m1, ALU.mult, R_, W_)
    s.red(ssum, ee, ALU.add, R_, W_)
    s.recip(rg, ssum, R_, W_)
    s.tt(rg, rg, gp, ALU.mult, R_, W_)
    s.tt(ee, ee, b4(rg), ALU.mult, R_, W_)
    s.tt(comb, bc(ohg.rearrange("p t (g o) -> p t g o", o=1), [128, NT, 4, 4]),
         bc(ee.rearrange("p t (o e) -> p t o e", o=1), [128, NT, 4, 4]), ALU.mult, R_, [combb])
    s.tap("comb", comb, [combb])
    if s.stop_after == "ln1":
        return
    k.barrier()
    ar.off = mark
    hT = [ar.alloc([2, S], BF16, "hT%d" % i) for i in range(4)]; hTb = [Buf("hT%d" % i) for i in range(4)]
    WD = [[ar.alloc([2, D], BF16, "WD%d_%d" % (g_, e_)) for e_ in range(4)] for g_ in range(2)]
    WDb = [[Buf("WD") for _ in range(4)] for _ in range(2)]
    WDs = s.wd_sems
    NB_ = 4
    sgs = [ar.alloc([256], F32, "sg%d" % i) for i in range(NB_)]; sgb = [Buf("sg%d" % i) for i in range(NB_)]
    hms = [ar.alloc([256], BF16, "hm%d" % i) for i in range(NB_)]; hmb = [Buf("hm%d" % i) for i in range(NB_)]
    trq = []
    PG = s.pool([0, 1, 2])
    PTr = s.pool([3, 4])
    PD = s.pool([5, 6, 7])
    slots = {}

    def load_expert(e):
        if e == 0:
            Wv, Wb = e0_pre
        else:
            Wv, Wb = load_gu(e)
        g_, e_ = (e // 4) % 2, e % 4
        k.dma("pool", WD[g_][e_], s.d_wd[l, e].rearrange("(c p) n -> p c n", p=128), WDs[g_ * 4 + e_],
              writes=[WDb[g_][e_]])
        slots[e] = (Wv, Wb)

    load_expert(0)
    cnt = 0
    for e in range(16):
        if e + 1 < 16:
            load_expert(e + 1)
        Wv, Wb = slots.pop(e)
        e4 = e % 4
        def moe_tr(item):
            (i_, hm_, hmb2, e4_) = item
            sl_ = slice(i_ * 128, (i_ + 1) * 128)
            tk_, tb_ = PTr()
            pb = tk_[:, 0:128].bitcast(BF16)
            for c in range(2):
                s.tr(pb[:, c * 128:(c + 1) * 128], hm_[:, c * 128:(c + 1) * 128], s.identB, [hmb2, s.CBb], [tb_], c == 1)
            s.act(hT[e4_][:, :, sl_], pb[:, 0:256].rearrange("p (a b) -> p a b", a=2), AF.Copy, [tb_], [hTb[e4_]])

        for i in range(NT):
            sl = slice(i * 128, (i + 1) * 128)
            bk, bb = PG()
            for kk in range(8):
                s.mm(bk[:, :], s.XT[:, kk, sl], Wv[:, kk, :], kk == 0, kk == 7, [s.XTb[i // 4], Wb], [bb], kk == 7)
            sg, sgb_ = sgs[cnt % NB_], sgb[cnt % NB_]
            hm, hmb_ = hms[cnt % NB_], hmb[cnt % NB_]
            cnt += 1
            s.act(sg, bk[:, 0:256], AF.Silu, [bb], [sgb_])
            s.stt(hm, bk[:, 256:512], comb[:, i, e // 4, e4:e4 + 1], sg, ALU.mult, ALU.mult,
                  [bb, combb, sgb_], [hmb_])
            trq.append((i, hm, hmb_, e4))
            if len(trq) > 2:
                moe_tr(trq.pop(0))
        if e4 == 3:
            while trq:
                moe_tr(trq.pop(0))
        if e4 == 3:
            g_ = (e // 4) % 2
            for i in range(NT):
                sl = slice(i * 128, (i + 1) * 128)
                for hf in range(2):
                    bk, bb = PD()
                    n = 0
                    for ee_ in range(4):
                        for c in range(2):
                            s.mm(bk[:, :], hT[ee_][:, c, sl], WD[g_][ee_][:, c, hf * 512:(hf + 1) * 512],
                                 n == 0, n == 7, [hTb[ee_], WDb[g_][ee_]], [bb], n == 7)
                            n += 1
                    xs = s.X[:, i, hf * 512:(hf + 1) * 512]
                    s.tt(xs, xs, bk[:, :], ALU.add, [s.Xb[i], bb], [s.Xb[i]])
    if s.stop_after == "moe":
        return
    if not last:
        preload_mla(s, l + 1)
    k.barrier()
    ar.off = mark
    LNP = ar.alloc([2, D], F32, "LNP2"); LNPb = Buf("LNP2")
    for q in range(2):
        k.dma("sp", LNP[:, q, :], s.dlnp[l, 2 + q:3 + q, :].to_broadcast([128, D]), s_l, writes=[LNPb])
    T1s = [ar.alloc([D], F32, "T2%d" % i) for i in range(2)]; T1bs = [Buf("T20"), Buf("T21")]

    def post2(i, T1, T1b):
        if not last:
            s.transpose_x_tile(i, T1, [T1b])
        s.act(s.X[:, i, :], T1, AF.Copy, [T1b], [s.Xb[i]])

    layernorm_tiles(s, l, 1, T1s, T1bs, LNP, LNPb, post2)
    k.barrier()


def _consts():
    c = np.zeros((128, CW), np.float32)
    r = np.arange(128)
    c[:, C_ID:C_ID + 128] = np.eye(128, dtype=np.float32)
    c[:, C_U:C_U + 128] = (r[:, None] <= r[None, :]).astype(np.float32)
    c[:, C_L:C_L + 128] = (r[:, None] >= r[None, :]).astype(np.float32)
    c[:, C_MF:C_MF + 128] = np.where(r[:, None] <= r[None, :], 0.0, NEG).astype(np.float32)
    c[:, C_MB:C_MB + 128] = np.where(r[:, None] >= r[None, :], 0.0, NEG).astype(np.float32)
    c[:, C_ONE:C_ONE + 128] = 1.0
    inv = (10000.0 ** (-np.arange(0, 32, 2, dtype=np.float32) / np.float32(32))).astype(np.float32)
    c[:, C_IF:C_IF + 16] = inv[None, :]
    return c


def _pack_prm(inp, l):
    p = np.zeros((128, PW), np.float32)
    rep = lambda v: np.broadcast_to(np.asarray(v, np.float32).reshape(1, -1), (128, np.asarray(v).size))
    p[:, P_GQ:P_GQ + 2] = inp["mla_q_norm"][l].reshape(2, 128).T
    p[:, P_GKV] = inp["mla_kv_norm"][l]
    p[:, P_GB:P_GB + 16] = rep(inp["mlstm_gate_bias"][l])
    p[:, P_MN:P_MN + 256] = rep(inp["mlstm_norm"][l])
    cw = inp["ssd_conv_w"][l]
    p[:, P_CW:P_CW + 30] = cw.reshape(5, 6, 128).transpose(2, 1, 0).reshape(128, 30)
    p[:, P_CB:P_CB + 6] = inp["ssd_conv_b"][l].reshape(6, 128).T
    p[:, P_DTB:P_DTB + 8] = rep(inp["ssd_dt_bias"][l])
    p[:, P_AL:P_AL + 8] = rep(inp["ssd_a_log"][l])
    p[:, P_SD:P_SD + 4] = rep(inp["ssd_d"][l])
    p[:, P_SN:P_SN + 256] = rep(inp["ssd_norm"][l])
    rb = np.concatenate([inp["router_group_b"][l].reshape(-1), inp["router_expert_b"][l].reshape(-1)])
    p[:, P_RB:P_RB + 20] = rep(rb)
    wr = np.concatenate([inp["router_group_w"][l],
                         inp["router_expert_w"][l].transpose(1, 0, 2).reshape(1024, 16)], axis=1)
    p[:, P_RW:P_RW + 160] = wr.reshape(8, 128, 20).transpose(1, 0, 2).reshape(128, 160)
    return p


def make_in_maps(inp, layers):
    inp = {k_: np.asarray(v) for k_, v in inp.items()}
    L = list(layers)
    sl = lambda a: np.ascontiguousarray(a[L])
    shared = {
        "cst": _consts(),
        "prm": np.stack([_pack_prm(inp, l) for l in L]),
        "w_in": sl(inp["w_in"]), "w_uq": sl(inp["mla_w_uq"]), "w_ukv": sl(inp["mla_w_ukv"]),
        "w_out": sl(inp["w_out"]),
        "lnp": np.stack([np.stack([inp["ln1_g"][l], inp["ln1_b"][l], inp["ln2_g"][l], inp["ln2_b"][l]]) for l in L]),
        "wg": sl(inp["expert_w_gate"]), "wu": sl(inp["expert_w_up"]), "wd": sl(inp["expert_w_down"]),
    }
    return shared


_CACHE = {}


def _prog(NL):
    if NL not in _CACHE:
        _CACHE[NL] = build(NL)[0]
    return _CACHE[NL]


FUSED = True


def kernel(**inputs):
    x = np.asarray(inputs["x"], np.float32)
    pos = np.asarray(inputs["positions"]).astype(np.int32)
    B = x.shape[0]
    posl = [np.ascontiguousarray(pos[b].reshape(NT, 128).T) for b in range(B)]
    groups = [list(range(DEPTH))] if FUSED else [[l] for l in range(DEPTH)]
    cur = [np.ascontiguousarray(x[b]) for b in range(B)]
    for L in groups:
        nc = _prog(len(L))
        shared = make_in_maps(inputs, L)
        in_maps = []
        for b in range(B):
            m = dict(shared)
            m["x"] = cur[b]
            m["pos"] = posl[b]
            in_maps.append(m)
        res = run_bass_kernel_spmd(nc, in_maps, core_ids=list(range(B)))
        cur = [np.ascontiguousarray(np.asarray(r["y"], dtype=np.float32)) for r in res.results]
    return np.stack(cur).astype(np.float32)
```

```python
import math
import numpy as np
import concourse.bass as bass
import concourse.mybir as mybir
from concourse.bass_utils import run_bass_kernel_spmd

F32 = mybir.dt.float32
BF16 = mybir.dt.bfloat16
I32 = mybir.dt.int32
AF = mybir.ActivationFunctionType
ALU = mybir.AluOpType
AX = mybir.AxisListType

S = 2048
D = 1024
NT = 16
DEPTH = 4
ALPHA = (2 * DEPTH) ** 0.25
IN_W = 2488
PI = math.pi
NEG = -30000.0

P_GQ, P_GKV, P_GB, P_MN, P_CW, P_CB, P_DTB, P_AL, P_SD, P_SN, P_RB, P_RW = (
    0, 2, 3, 19, 275, 305, 311, 319, 327, 331, 587, 607)
PW = 767
C_ID, C_U, C_L, C_MF, C_MB, C_ONE, C_IF = 0, 128, 256, 384, 512, 640, 768
CW = 784

WSLOT = 6144
NSLOT = 2
ARENA_BYTES = 74 * 1024


class Sem:
    def __init__(self, nc, name, dma=False):
        self.h = nc.alloc_semaphore(name)
        self.val = 0
        self.dma = dma
        self.name = name


class Buf:
    __slots__ = ("name", "w", "r", "excl")

    def __init__(self, name, excl=False):
        self.name = name
        self.w = None
        self.r = {}
        self.excl = excl


class K:
    def __init__(self, nc):
        self.nc = nc
        self.eng = {}
        for n, a in (("pe", "tensor"), ("dve", "vector"), ("act", "scalar"),
                     ("pool", "gpsimd"), ("sp", "sync")):
            self.eng[n] = (getattr(nc, a), Sem(nc, "e_" + n))
        self.known = {n: {} for n in self.eng}
        self.dsems = []
        self.ninst = 0

    def dsem(self, name):
        s = Sem(self.nc, name, dma=True)
        self.dsems.append(s)
        return s

    def _wait(self, e, tickets):
        h, own = self.eng[e]
        kn = self.known[e]
        need = {}
        for (s, v) in tickets:
            if s.dma:
                v = s.val
            if v > kn.get(s, 0) and v > need.get(s, 0):
                need[s] = v
        for s, v in need.items():
            assert s.val >= v, "wait on unsignalled ticket %s %d>%d (eng %s)" % (s.name, v, s.val, e)
            h.wait_ge(s.h, v)
            kn[s] = v
            self.ninst += 1

    def _tickets(self, reads, writes):
        tk = []
        for b in reads:
            if b.w is not None:
                tk.append(b.w)
        for b in writes:
            if b.w is not None:
                tk.append(b.w)
            for s, v in b.r.items():
                tk.append((s, v))
        return tk

    def _commit(self, t, reads, writes):
        for b in writes:
            b.w = t
            b.r = {}
        for b in reads:
            if b not in writes:
                if t[1] > b.r.get(t[0], 0):
                    b.r[t[0]] = t[1]

    def op(self, e, fn, reads=(), writes=(), signal=True, keep_self=False):
        h, sem = self.eng[e]
        ex = [b for b in reads if b.excl and b not in writes]
        if ex:
            writes = list(writes) + ex
            reads = [b for b in reads if not b.excl]
        tk = self._tickets(reads, writes)
        if e == "pe" and not keep_self:
            tk = [(s, v) for (s, v) in tk if s is not sem]
        self._wait(e, tk)
        inst = fn(h)
        self.ninst += 1
        if signal:
            sem.val += 1
            inst.then_inc(sem.h, 1)
            t = (sem, sem.val)
        else:
            t = (sem, sem.val + 1)
        self._commit(t, reads, writes)
        return inst

    def dma(self, q, out, in_, dsem, reads=(), writes=(), **kw):
        h, _ = self.eng[q]
        self._wait(q, self._tickets(reads, writes))
        inst = h.dma_start(out=out, in_=in_, **kw)
        self.ninst += 1
        dsem.val += 16
        inst.then_inc(dsem.h, 16)
        self._commit((dsem, dsem.val), reads, writes)

    def barrier(self):
        allt = [(s, s.val) for (_, s) in self.eng.values() if s.val > 0]
        allt += [(s, s.val) for s in self.dsems if s.val > 0]
        for e in self.eng:
            self._wait(e, allt)


class Rot:
    def __init__(self, items):
        self.items = list(items)
        self.i = 0

    def __call__(self):
        x = self.items[self.i % len(self.items)]
        self.i += 1
        return x


class Arena:
    def __init__(self, nc, nbytes):
        self.t = nc.alloc_sbuf_tensor("arena", [128, nbytes // 2], BF16)
        self.nbytes = nbytes
        self.off = 0
        self.peak = 0

    def reset(self):
        self.off = 0

    def alloc(self, shape, dtype, name="a"):
        esz = 4 if dtype in (F32, I32) else 2
        n = 1
        for s in shape:
            n *= s
        nb = (n * esz + 63) // 64 * 64
        assert self.off + nb <= self.nbytes, "arena overflow %s need %d have %d" % (
            name, nb, self.nbytes - self.off)
        ap = self.t[:, self.off // 2:(self.off + n * esz) // 2]
        if esz == 4:
            ap = ap.bitcast(dtype)
        if len(shape) == 2:
            ap = ap.rearrange("p (a b) -> p a b", a=shape[0])
        elif len(shape) == 3:
            ap = ap.rearrange("p (a b c) -> p a b c", a=shape[0], b=shape[1])
        elif len(shape) == 4:
            ap = ap.rearrange("p (a b c d) -> p a b c d", a=shape[0], b=shape[1], c=shape[2])
        self.off += nb
        self.peak = max(self.peak, self.off)
        return ap


def bc(ap, shape):
    return ap.to_broadcast(list(shape))


def build(NL, taps=(), stop_after=None):
    nc = bass.Bass("TRN2", target_bir_lowering=False)
    k = K(nc)

    def din(name, shape, dt=F32):
        return nc.dram_tensor(name, list(shape), dt, kind="ExternalInput").ap()

    dx = din("x", [S, D])
    dpos = din("pos", [128, NT], I32)
    dcst = din("cst", [128, CW])
    dprm = din("prm", [NL, 128, PW])
    w_in = din("w_in", [NL, D, IN_W])
    w_uq = din("w_uq", [NL, 256, 768])
    w_ukv = din("w_ukv", [NL, 128, 1024])
    w_out = din("w_out", [NL, D, D])
    dlnp = din("lnp", [NL, 4, D])
    d_wg = din("wg", [NL, 16, D, 256])
    d_wu = din("wu", [NL, 16, D, 256])
    d_wd = din("wd", [NL, 16, 256, D])
    dy = nc.dram_tensor("y", [S, D], F32, kind="ExternalOutput").ap()
    tap_out = {}

    X = nc.alloc_sbuf_tensor("X", [128, NT, D], F32)
    Xb = [Buf("X%d" % i) for i in range(NT)]
    XT = nc.alloc_sbuf_tensor("XT", [128, 8, S], BF16)
    XTb = [Buf("XT%d" % g) for g in range(4)]
    WS = [nc.alloc_sbuf_tensor("ws%d" % i, [128, WSLOT], BF16) for i in range(NSLOT)]
    WSb = [Buf("ws%d" % i) for i in range(NSLOT)]
    WSs = [k.dsem("ws%d" % i) for i in range(NSLOT)]
    CF = nc.alloc_sbuf_tensor("cf", [128, CW], F32)
    CFb = Buf("cf")
    CB = nc.alloc_sbuf_tensor("cb", [128, CW], BF16)
    CBb = Buf("cb")
    PRM = nc.alloc_sbuf_tensor("prm_s", [128, PW], F32)
    PRMb = Buf("prm")
    SCT = nc.alloc_sbuf_tensor("sc", [128, 1024], F32)
    sc_off = [0]

    def sc_reset():
        sc_off[0] = 0

    def sc_alloc(n):
        o = sc_off[0]
        assert o + n <= 1024, "scalar pool overflow"
        sc_off[0] = o + n
        return SCT[:, o:o + n]
    COS = nc.alloc_sbuf_tensor("cos", [128, NT, 16], F32)
    SIN = nc.alloc_sbuf_tensor("sin", [128, NT, 16], F32)
    CSb = Buf("cossin")
    arena = Arena(nc, ARENA_BYTES)
    banks = [nc.alloc_psum_tensor("pb%d" % i, [128, 512], F32) for i in range(8)]
    bankb = [Buf("pb%d" % i, excl=True) for i in range(8)]

    def pool(idx):
        return Rot([(banks[i], bankb[i]) for i in idx])

    s_c = k.dsem("cst")
    s_x = k.dsem("xld")
    s_p = k.dsem("prm")
    s_l = k.dsem("lnp")
    s_y = k.dsem("yst")
    s_t = k.dsem("tap")

    wd_sems = [k.dsem("wd%d" % i) for i in range(8)]
    identF = CF[:, C_ID:C_ID + 128]
    identB = CB[:, C_ID:C_ID + 128]
    slot_ctr = [0]

    def wslot():
        i = slot_ctr[0] % NSLOT
        slot_ctr[0] += 1
        return WS[i], WSb[i], WSs[i]

    def tap(name, ap, reads):
        if name not in taps:
            return
        shp = list(ap.shape)
        n = 1
        for s_ in shp[1:]:
            n *= s_
        cnt = sum(1 for t_ in tap_out if t_.startswith(name))
        nm = name if cnt == 0 else "%s_%d" % (name, cnt)
        dt_ = nc.dram_tensor("tap_" + nm, shp, ap.dtype, kind="ExternalOutput").ap()
        tap_out[nm] = shp
        k.dma("sp", dt_, ap, s_t, reads=reads)

    def mm(out, lhsT, rhs, start, stop, reads, writes, signal, serial=False):
        k.op("pe", lambda h: h.matmul(out, lhsT, rhs, start=start, stop=stop),
             reads=reads, writes=writes, signal=(signal or serial), keep_self=serial)

    def act(out, in_, func, reads, writes, **kw):
        k.op("act", lambda h: h.activation(out=out, in_=in_, func=func, **kw), reads=reads, writes=writes)

    def tt(out, in0, in1, op, reads, writes, e="dve"):
        k.op(e, lambda h: h.tensor_tensor(out=out, in0=in0, in1=in1, op=op), reads=reads, writes=writes)

    def ts(out, in0, s1, s2, op0, op1, reads, writes, e="dve"):
        if s2 is None:
            k.op(e, lambda h: h.tensor_scalar(out=out, in0=in0, scalar1=s1, scalar2=None, op0=op0),
                 reads=reads, writes=writes)
        else:
            k.op(e, lambda h: h.tensor_scalar(out=out, in0=in0, scalar1=s1, scalar2=s2, op0=op0, op1=op1),
                 reads=reads, writes=writes)

    def stt(out, in0, scalar, in1, op0, op1, reads, writes):
        k.op("dve", lambda h: h.scalar_tensor_tensor(out=out, in0=in0, scalar=scalar, in1=in1, op0=op0, op1=op1),
             reads=reads, writes=writes)

    def cp(out, in_, reads, writes, e="dve"):
        k.op(e, lambda h: h.tensor_copy(out=out, in_=in_), reads=reads, writes=writes)

    def red(out, in_, op, reads, writes):
        k.op("dve", lambda h: h.tensor_reduce(out=out, in_=in_, axis=AX.X, op=op), reads=reads, writes=writes)

    def recip(out, in_, reads, writes):
        k.op("dve", lambda h: h.reciprocal(out=out, in_=in_), reads=reads, writes=writes)

    def memset(ap, val, writes, e="dve"):
        k.op(e, lambda h: h.memset(ap, val), writes=writes)

    def tr(out, in_, ident, reads, writes, signal):
        k.op("pe", lambda h: h.transpose(out, in_, ident), reads=reads, writes=writes, signal=signal)

    k.dma("sp", CF[:, :], dcst[:, :], s_c, writes=[CFb])
    s_cb = k.dsem("cstb")
    k.dma("pool", CB[:, :], dcst[:, :], s_cb, writes=[CBb])
    dxv = dx.rearrange("(i p) d -> p i d", p=128)
    for q in range(4):
        k.dma("sp", X[:, 4 * q:4 * q + 4, :], dxv[:, 4 * q:4 * q + 4, :], s_x, writes=Xb[4 * q:4 * q + 4])

    arena.reset()
    tb = Buf("setup_tmp")
    posi = arena.alloc([NT, 1], I32, "posi")
    posf = arena.alloc([NT, 1], F32, "posf")
    ang = arena.alloc([NT, 16], F32, "ang")
    kf = arena.alloc([NT, 16], F32, "kf")
    ki = arena.alloc([NT, 16], I32, "ki")
    mt_ = arena.alloc([NT, 16], F32, "mt")
    k.dma("sp", posi[:, :, 0], dpos[:, :], s_c, writes=[tb])
    cp(posf, posi, [tb], [tb])
    invf = CF[:, C_IF:C_IF + 16].rearrange("p (a b) -> p a b", a=1)
    tt(ang, bc(posf, [128, NT, 16]), bc(invf, [128, NT, 16]), ALU.mult, [tb, CFb], [tb])
    ts(kf, ang, 1.0 / (2 * PI), None, ALU.mult, None, [tb], [tb])
    cp(ki, kf, [tb], [tb])
    cp(kf, ki, [tb], [tb])
    stt(ang, kf, -2 * PI, ang, ALU.mult, ALU.add, [tb], [tb])

    def wrap(r):
        ts(mt_, r, PI, -2 * PI, ALU.is_gt, ALU.mult, [tb], [tb])
        tt(r, r, mt_, ALU.add, [tb], [tb])
        ts(mt_, r, -PI, 2 * PI, ALU.is_lt, ALU.mult, [tb], [tb])
        tt(r, r, mt_, ALU.add, [tb], [tb])

    wrap(ang)
    act(SIN[:, :, :], ang, AF.Sin, [tb], [CSb])
    ts(ang, ang, PI / 2, None, ALU.add, None, [tb, CSb], [tb])
    wrap(ang)
    act(COS[:, :, :], ang, AF.Sin, [tb], [CSb])

    PT4 = pool([0, 1, 2, 3, 6, 7])

    def transpose_x_tile(i, src_tile_ap, src_reads, dst32=None, dst32b=None):
        for c0 in (0, 4):
            bk, bb = PT4()
            for j in range(4):
                c = c0 + j
                tr(bk[:, j * 128:(j + 1) * 128], src_tile_ap[:, c * 128:(c + 1) * 128], identF,
                   src_reads + [CFb], [bb], j == 3)
            bv = bk[:, :].rearrange("p (a b) -> p a b", a=4)
            act(XT[:, c0:c0 + 4, i * 128:(i + 1) * 128], bv, AF.Copy, [bb], [XTb[i // 4]])
            if dst32 is not None:
                cp(dst32[:, c0:c0 + 4, :], bv, [bb], [dst32b])

    for i in range(NT):
        transpose_x_tile(i, X[:, i, :], [Xb[i]])
    k.barrier()

    st = dict(nc=nc, k=k, arena=arena, X=X, Xb=Xb, XT=XT, XTb=XTb, CF=CF, CFb=CFb, CB=CB, CBb=CBb,
              PRM=PRM, PRMb=PRMb, sc_alloc=sc_alloc, sc_reset=sc_reset, COS=COS, SIN=SIN, CSb=CSb, pool=pool,
              wslot=wslot, tap=tap, mm=mm, act=act, tt=tt, ts=ts, stt=stt, cp=cp, red=red, recip=recip,
              memset=memset, tr=tr, identF=identF, identB=identB, w_in=w_in, w_uq=w_uq, w_ukv=w_ukv,
              w_out=w_out, dlnp=dlnp, d_wg=d_wg, d_wu=d_wu, d_wd=d_wd, dprm=dprm, s_p=s_p, s_l=s_l,
              transpose_x_tile=transpose_x_tile, stop_after=stop_after, wd_sems=wd_sems, pre={})

    st = NS(st)
    for l in range(NL):
        k.dma("sp", PRM[:, :], dprm[l], s_p, writes=[PRMb])
        if stop_after == "setup":
            break
        phase_mla(st, l)
        if stop_after is not None and stop_after.startswith("mla"):
            break
        phase_mlstm(st, l)
        if stop_after is not None and stop_after.startswith("mlstm"):
            break
        phase_ssd(st, l)
        if stop_after is not None and stop_after.startswith("ssd"):
            break
        phase_ln_router_moe(st, l, last=(l == NL - 1))

    k.barrier()
    dyv = dy.rearrange("(i p) d -> p i d", p=128)
    for q in range(4):
        k.dma("sp", dyv[:, 4 * q:4 * q + 4, :], X[:, 4 * q:4 * q + 4, :], s_y, reads=Xb[4 * q:4 * q + 4])
    k._wait("sp", [(s_y, s_y.val), (s_t, s_t.val)] if s_t.val else [(s_y, s_y.val)])
    return nc, tap_out, k


class NS:
    def __init__(self, d):
        self.__dict__.update(d)


def rope_tm(st, src, dst, tmpa, tmpb, sb, db):
    s = st
    t1, t2 = src[:, :, 0:16], src[:, :, 16:32]
    C, Sn = s.COS[:, :, :], s.SIN[:, :, :]
    tb_ = Buf("rope_tmp")
    s.tt(tmpa, t1, C, ALU.mult, [sb, s.CSb], [tb_])
    s.tt(tmpb, t2, Sn, ALU.mult, [sb, s.CSb, tb_], [tb_])
    s.tt(dst[:, :, 0:16], tmpa, tmpb, ALU.subtract, [tb_], [db])
    s.tt(tmpa, t2, C, ALU.mult, [sb, s.CSb, db], [tb_])
    s.tt(tmpb, t1, Sn, ALU.mult, [sb, s.CSb, tb_], [tb_])
    s.tt(dst[:, :, 16:32], tmpa, tmpb, ALU.add, [tb_], [db])


def outproj_load(st, l, nchunk, row0):
    s = st
    W, Wb, Wsm = s.wslot()
    Wv = W[:, 0:nchunk * 1024].rearrange("p (c n) -> p c n", c=nchunk)
    src = s.w_out[l, row0:row0 + nchunk * 128, :].rearrange("(c p) n -> p c n", p=128)
    s.k.dma("pool", Wv, src, Wsm, writes=[Wb])
    return Wv, Wb


def outproj_partial(st, l, YT, YTb, nchunk, row0, first, pre=None):
    s = st
    Wv, Wb = pre if pre is not None else outproj_load(s, l, nchunk, row0)
    PA = s.pool([0, 1, 2, 3])
    for i in range(NT):
        for hf in range(2):
            bk, bb = PA()
            for c in range(nchunk):
                s.mm(bk[:, :], YT[:, c, i * 128:(i + 1) * 128], Wv[:, c, hf * 512:(hf + 1) * 512],
                     c == 0, c == nchunk - 1, [YTb, Wb], [bb], c == nchunk - 1)
            xs = s.X[:, i, hf * 512:(hf + 1) * 512]
            if first:
                s.stt(xs, xs, ALPHA, bk[:, :], ALU.mult, ALU.add, [s.Xb[i], bb], [s.Xb[i]])
            else:
                s.tt(xs, xs, bk[:, :], ALU.add, [s.Xb[i], bb], [s.Xb[i]])


def preload_mla(s, l):
    k = s.k
    W0, W0b, W0s = s.wslot()
    W0v = W0[:, 0:8 * 416].rearrange("p (k c) -> p k c", k=8)
    k.dma("pool", W0v, s.w_in[l].rearrange("(k p) c -> p k c", p=128)[:, :, 0:416], W0s, writes=[W0b])
    W1, W1b, W1s = s.wslot()
    Wuq = W1[:, 0:1536].rearrange("p (c n) -> p c n", c=2)
    k.dma("pool", Wuq, s.w_uq[l].rearrange("(c p) n -> p c n", p=128), W1s, writes=[W1b])
    Wkv = W1[:, 1536:2560]
    k.dma("pool", Wkv, s.w_ukv[l], W1s, writes=[W1b])
    s.pre["mla"] = (W0v, W0b, Wuq, Wkv, W1b)


def preload_mlstm_a(s, l):
    Wa, Wab, Was = s.wslot()
    Wav = Wa[:, 0:8 * 512].rearrange("p (k c) -> p k c", k=8)
    wsrc = s.w_in[l].rearrange("(k p) c -> p k c", p=128)
    s.k.dma("pool", Wav, wsrc[:, :, 416:928], Was, writes=[Wab])
    s.pre["mlstm_a"] = (Wav, Wab)


def preload_ssd_x(s, l):
    Wx, Wxb, Wxs = s.wslot()
    Wxv = Wx[:, 0:6144].rearrange("p (k c) -> p k c", k=8)
    wsrc = s.w_in[l].rearrange("(k p) c -> p k c", p=128)
    s.k.dma("pool", Wxv, wsrc[:, :, 1712:2480], Wxs, writes=[Wxb])
    s.pre["ssd_x"] = (Wxv, Wxb)


def phase_mla(st, l):
    s = NS(st) if isinstance(st, dict) else st
    k, ar = s.k, s.arena
    ar.reset()
    PA = s.pool([0, 1, 2, 3])
    PB = s.pool([4, 5])
    PC = s.pool([6, 7])
    if "mla" not in s.pre:
        preload_mla(s, l)
    W0v, W0b, Wuq, Wkv, W1b = s.pre.pop("mla")
    for c in range(2):
        s.ts(Wuq[:, c, :], Wuq[:, c, :], s.PRM[:, P_GQ + c:P_GQ + c + 1], None, ALU.mult, None,
             [s.PRMb, W1b], [W1b])
    s.ts(Wkv, Wkv, s.PRM[:, P_GKV:P_GKV + 1], None, ALU.mult, None, [s.PRMb, W1b], [W1b])

    if s.stop_after == "mla_w":
        return
    cT = ar.alloc([3, S], BF16, "cT"); cTb = Buf("cT")
    YA = ar.alloc([4, S], BF16, "YA"); YAb = Buf("YA")
    krr = ar.alloc([NT, 32], F32, "krr"); krrb = Buf("krr")
    krb = ar.alloc([NT, 32], BF16, "krb"); krbb = Buf("krb")
    sq = ar.alloc([384], F32, "sq"); sqb = Buf("sq")
    s.sc_reset()
    ssq = s.sc_alloc(NT); sskv = s.sc_alloc(NT); ssb = Buf("ss")
    rq = s.sc_alloc(NT); rkv = s.sc_alloc(NT); rb_ = Buf("r")
    ta = ar.alloc([NT, 16], F32, "ta"); tb2 = ar.alloc([NT, 16], F32, "tb")
    qhb = ar.alloc([NT, 96], BF16, "qhb"); qhbb = Buf("qhb")
    qr = ar.alloc([NT, 32], F32, "qr"); qrb = Buf("qr")
    khb = ar.alloc([NT, 96], BF16, "khb"); khbb = Buf("khb")
    VA = [ar.alloc([NT, 65], BF16, "VA%d" % i) for i in range(2)]; VAb = [Buf("VA0"), Buf("VA1")]
    QT = [ar.alloc([S], BF16, "QT%d" % i) for i in range(2)]; QTb = [Buf("QT0"), Buf("QT1")]
    KT = [ar.alloc([S], BF16, "KT%d" % i) for i in range(2)]; KTb = [Buf("KT0"), Buf("KT1")]
    PTs = [(ar.alloc([512], BF16, "PT%d" % i), Buf("PT%d" % i)) for i in range(5)]
    PTr = Rot(PTs)
    OSs = [(ar.alloc([512], F32, "OS%d" % i), Buf("OS%d" % i)) for i in range(2)]
    OSr = Rot(OSs)

    for i in range(NT):
        bk, bb = PA()
        for kk in range(8):
            s.mm(bk[:, 0:416], s.XT[:, kk, i * 128:(i + 1) * 128], W0v[:, kk, :], kk == 0, kk == 7,
                 [s.XTb[i // 4], W0b], [bb], kk == 7)
        s.act(sq[:, 0:384], bk[:, 0:384], AF.Square, [bb], [sqb])
        s.red(ssq[:, i:i + 1], sq[:, 0:256], ALU.add, [sqb], [ssb])
        s.red(sskv[:, i:i + 1], sq[:, 256:384], ALU.add, [sqb], [ssb])
        s.cp(krr[:, i, :], bk[:, 384:416], [bb], [krrb])
    if s.stop_after == "mla_tm":
        return
    for j in range(3):
        for g in range(4):
            bk, bb = PA()
            for kk in range(8):
                s.mm(bk[:, :], W0v[:, kk, j * 128:(j + 1) * 128], s.XT[:, kk, g * 512:(g + 1) * 512],
                     kk == 0, kk == 7, [s.XTb[g], W0b], [bb], kk == 7)
            s.act(cT[:, j, g * 512:(g + 1) * 512], bk[:, :], AF.Copy, [bb], [cTb])
    if s.stop_after == "mla_fm":
        return
    s.act(rq, ssq, AF.Sqrt, [ssb], [rb_], scale=96.0 / 256.0, bias=96e-6)
    s.act(rkv, sskv, AF.Sqrt, [ssb], [rb_], scale=1.0 / 128.0, bias=1e-6)
    s.recip(rq, rq, [rb_], [rb_])
    s.recip(rkv, rkv, [rb_], [rb_])
    rope_tm(s, krr, krb, ta, tb2, krrb, krbb)
    for v_ in range(2):
        s.memset(VA[v_][:, :, 64:65], 1.0, [VAb[v_]])

    if s.stop_after == "mla_r":
        return

    def prep_chunks(h):
        p = h % 2
        ch = []

        def q_group(i4):
            bk, bb = PC()
            for j in range(4):
                i = i4 * 4 + j
                for c in range(2):
                    s.mm(bk[:, j * 96:(j + 1) * 96], cT[:, c, i * 128:(i + 1) * 128],
                         Wuq[:, c, h * 96:(h + 1) * 96], c == 0, c == 1, [cTb, W1b], [bb],
                         j == 3 and c == 1)
            bkv = bk[:, 0:384].rearrange("p (a b) -> p a b", a=4)
            rq4 = rq[:, i4 * 4:(i4 + 1) * 4].rearrange("p (a b) -> p a b", b=1)
            s.tt(qhb[:, i4 * 4:(i4 + 1) * 4, 0:64], bkv[:, :, 0:64], bc(rq4, [128, 4, 64]), ALU.mult,
                 [bb, rb_], [qhbb])
            s.tt(qr[:, i4 * 4:(i4 + 1) * 4, :], bkv[:, :, 64:96], bc(rq4, [128, 4, 32]), ALU.mult,
                 [bb, rb_], [qrb])

        def kv_group(i4):
            bk, bb = PC()
            for j in range(4):
                i = i4 * 4 + j
                s.mm(bk[:, j * 128:(j + 1) * 128], cT[:, 2, i * 128:(i + 1) * 128],
                     Wkv[:, h * 128:(h + 1) * 128], True, True, [cTb, W1b], [bb], j == 3)
            bkv = bk[:, :].rearrange("p (a b) -> p a b", a=4)
            r4 = rkv[:, i4 * 4:(i4 + 1) * 4].rearrange("p (a b) -> p a b", b=1)
            s.tt(khb[:, i4 * 4:(i4 + 1) * 4, 0:64], bkv[:, :, 0:64], bc(r4, [128, 4, 64]), ALU.mult,
                 [bb, rb_], [khbb])
            s.tt(VA[p][:, i4 * 4:(i4 + 1) * 4, 0:64], bkv[:, :, 64:128], bc(r4, [128, 4, 64]), ALU.mult,
                 [bb, rb_], [VAb[p]])

        def tr_group(src, srcb, dst, dstb, i4):
            bk, bb = PC()
            pb = bk[:, 0:256].bitcast(BF16)
            for j in range(4):
                i = i4 * 4 + j
                s.tr(pb[0:96, j * 128:(j + 1) * 128], src[:, i, :], s.identB, [srcb, s.CBb], [bb], j == 3)
            s.cp(dst[0:96, i4 * 512:(i4 + 1) * 512], pb[0:96, :], [bb], [dstb])

        for i4 in range(4):
            ch.append(lambda i4=i4: q_group(i4))
        ch.append(lambda: rope_tm(s, qr, qhb[:, :, 64:96], ta, tb2, qrb, qhbb))
        for i4 in range(4):
            ch.append(lambda i4=i4: kv_group(i4))
        ch.append(lambda: s.cp(khb[:, :, 64:96], krb, [krbb], [khbb]))
        for i4 in range(4):
            ch.append(lambda i4=i4: tr_group(qhb, qhbb, QT[p], QTb[p], i4))
        for i4 in range(4):
            ch.append(lambda i4=i4: tr_group(khb, khbb, KT[p], KTb[p], i4))
        return ch

    LOOK = 3
    pend = []

    fin_q = []

    def fin_tick(flush=False):
        for it in fin_q:
            it[0] -= 1
        while fin_q and (flush or fin_q[0][0] <= 0):
            fin_q.pop(0)[1]()

    def attn_finish(h, qc, ob, obb):
        os_, osb = OSr()
        s.cp(os_[0:65, :], ob[0:65, :], [obb], [osb])
        s.act(os_[64:65, :], os_[64:65, :], AF.Ln, [osb], [osb])
        s.act(os_[64:65, :], os_[64:65, :], AF.Exp, [osb], [osb], scale=-1.0)
        fin_q.append([5, lambda: attn_finish2(h, qc, os_, osb)])

    def attn_finish2(h, qc, os_, osb):
        rbk, rbb = PC()
        s.mm(rbk[0:64, :], s.CF[64:65, C_ONE:C_ONE + 64], os_[64:65, :], True, True,
             [osb, s.CFb], [rbb], True)
        r0 = (h % 2) * 64
        s.tt(YA[r0:r0 + 64, h // 2, qc * 512:(qc + 1) * 512], os_[0:64, :], rbk[0:64, :], ALU.mult,
             [osb, rbb], [YAb])

    def pv_step(item):
        (h, qc, kt, pt, ptb, ob, obb) = item
        p = h % 2
        s.mm(ob[0:65, :], VA[p][:, kt, :], pt, kt == 0, kt == 15, [ptb, VAb[p]], [obb], kt == 15)
        if kt == 15:
            attn_finish(h, qc, ob, obb)

    def attn(h, chunks):
        p = h % 2
        n = 0
        for qc in range(4):
            ob, obb = PB()
            for kt in range(16):
                n += 1
                if chunks and n >= 5 and n % 2 == 0:
                    chunks.pop(0)()
                sb_, sbb = PA()
                s.mm(sb_[:, :], KT[p][0:96, kt * 128:(kt + 1) * 128], QT[p][0:96, qc * 512:(qc + 1) * 512],
                     True, True, [KTb[p], QTb[p]], [sbb], True)
                pt, ptb = PTr()
                s.act(pt, sb_[:, :], AF.Exp, [sbb], [ptb])
                pend.append((h, qc, kt, pt, ptb, ob, obb))
                if len(pend) > LOOK:
                    pv_step(pend.pop(0))
                fin_tick()

    for c_ in prep_chunks(0):
        c_()
    wo_pre = outproj_load(s, l, 4, 0)
    for h in range(8):
        chunks = prep_chunks(h + 1) if h + 1 < 8 else []
        attn(h, chunks)
        while chunks:
            chunks.pop(0)()
    while pend:
        pv_step(pend.pop(0))
    fin_tick(flush=True)
    preload_mlstm_a(s, l)
    s.tap("ya", YA, [YAb])
    outproj_partial(s, l, YA, YAb, 4, 0, True, pre=wo_pre)


def token_decay_arrays(s, a_all, u_all, ab):
    ar = s.arena
    cs = ar.alloc([NT, 2, 4], F32, "cs")
    nb = ar.alloc([NT, 2, 4], F32, "nb")
    ecs = ar.alloc([NT, 2, 4], F32, "ecs")
    ws = ar.alloc([NT, 2, 4], F32, "ws")
    gst = ar.alloc([NT, 2, 4], F32, "gst")
    P1 = s.pool([0, 1])
    U = s.CF[:, C_U:C_U + 128]
    L = s.CF[:, C_L:C_L + 128]
    ones = s.CF[:, C_ONE:C_ONE + 128]
    bk, bb = P1()
    for d, M in ((0, U), (1, L)):
        s.mm(bk[:, d * 64:(d + 1) * 64], M, a_all[:, :, d, :], True, True, [ab, s.CFb], [bb], d == 1)
    for d in range(2):
        s.cp(cs[:, :, d, :], bk[:, d * 64:(d + 1) * 64].rearrange("p (a b) -> p a b", a=NT), [bb], [ab])
    bk2, bb2 = P1()
    s.mm(bk2[:, 0:128], ones, a_all.rearrange("p a b c -> p (a b c)"), True, True, [ab, s.CFb], [bb2], True)
    tot = bk2[:, 0:128].rearrange("p (a b c) -> p a b c", a=NT, b=2)
    s.act(gst, tot, AF.Exp, [bb2], [ab])
    s.tt(nb, u_all, cs, ALU.subtract, [ab], [ab])
    s.tt(ws, tot, nb, ALU.add, [bb2, ab], [ab])
    s.act(ws, ws, AF.Exp, [ab], [ab])
    s.act(ecs, cs, AF.Exp, [ab], [ab])
    return dict(cs=cs, nb=nb, ecs=ecs, ws=ws, gst=gst)


def decay_E(s, i, a_all, nb, ab, Ebuf, Ebb, banks2):
    U = s.CF[:, C_U:C_U + 128]
    L = s.CF[:, C_L:C_L + 128]
    for d in range(2):
        bk, bb = banks2[d]
        M = U if d == 0 else L
        mk = s.CB[:, C_MF:C_MF + 128] if d == 0 else s.CB[:, C_MB:C_MB + 128]
        for h in range(4):
            s.mm(bk[:, h * 128:(h + 1) * 128], bc(a_all[:, i, d, h:h + 1], [128, 128]), M, True, False,
                 [ab, s.CFb], [bb], False)
            s.mm(bk[:, h * 128:(h + 1) * 128], s.identB, mk, False, True, [s.CBb], [bb], h == 3)
        for h in range(4):
            s.act(Ebuf[:, d, h, :], bk[:, h * 128:(h + 1) * 128], AF.Exp, [bb, ab], [Ebb],
                  bias=nb[:, i, d, h:h + 1])


def phase_mlstm(st, l):
    s = NS(st) if isinstance(st, dict) else st
    k, ar = s.k, s.arena
    k.barrier()
    ar.reset()
    s.sc_reset()
    PA = s.pool([0, 1, 2, 3])
    wsrc = s.w_in[l].rearrange("(k p) c -> p k c", p=128)
    if "mlstm_a" not in s.pre:
        preload_mlstm_a(s, l)
    Wav, Wab = s.pre.pop("mlstm_a")
    Wb, Wbb, Wbs = s.wslot()
    Wbv = Wb[:, 0:8 * 528].rearrange("p (k c) -> p k c", k=8)
    k.dma("pool", Wbv, wsrc[:, :, 928:1456], Wbs, writes=[Wbb])
    mqT = ar.alloc([2, S], BF16, "mqT"); mkT = ar.alloc([2, S], BF16, "mkT"); fmb = Buf("mfm")
    mkTM = ar.alloc([NT, 4, 64], BF16, "mkTM"); mvA = ar.alloc([NT, 4, 65], BF16, "mvA"); tmb = Buf("mtm")
    YB = ar.alloc([2, S], BF16, "YB"); YBb = Buf("YB")
    gts = ar.alloc([NT, 2, 2, 4], F32, "gts")
    a_all = ar.alloc([NT, 2, 4], F32, "a_all"); u_all = ar.alloc([NT, 2, 4], F32, "u_all"); ab = Buf("mtok")
    Fst = ar.alloc([NT, 2, 2, 65], BF16, "F"); Fb = Buf("F")
    Sst = ar.alloc([2, 2, 65], F32, "S"); Sb = Buf("S")
    Kw = [ar.alloc([4, 64], BF16, "Kw%d" % i) for i in range(2)]; Kwb = [Buf("Kw0"), Buf("Kw1")]
    Es = [ar.alloc([2, 4, 128], BF16, "E%d" % i) for i in range(2)]; Ebs = [Buf("E0"), Buf("E1")]
    MTs = [ar.alloc([2, 4, 128], BF16, "MT%d" % i) for i in range(2)]; MTbs = [Buf("MT0"), Buf("MT1")]
    Rall = ar.alloc([2, 4, 65], F32, "Rall"); Rt = [Rall[:, 0, :, :], Rall[:, 1, :, :]]; Rb = Buf("R")
    prod = ar.alloc([2, 4, 64], F32, "prod")
    cen, sq_ = prod[:, 0, :, :], prod[:, 1, :, :]
    tmp = ar.alloc([4, 65], F32, "tmp")
    den = s.sc_alloc(8); hs = ar.alloc([4, 64], F32, "hs"); hb_ = Buf("hs")

    st4 = s.sc_alloc(4); st4b = s.sc_alloc(4)
    sgos = [ar.alloc([256], F32, "sgo%d" % i) for i in range(2)]; sgobs = [Buf("sgo0"), Buf("sgo1")]
    ybs = [ar.alloc([256], BF16, "yb%d" % i) for i in range(3)]; ybbs = [Buf("yb%d" % i) for i in range(3)]
    s.memset(mvA[:, :, :, 64:65], 1.0, [tmb])
    for i in range(NT):
        bk, bb = PA()
        for kk in range(8):
            s.mm(bk[:, 0:256], s.XT[:, kk, i * 128:(i + 1) * 128], Wav[:, kk, 256:512], kk == 0, kk == 7,
                 [s.XTb[i // 4], Wab], [bb], kk == 7)
        s.act(mkTM[:, i, :, :], bk[:, 0:256].rearrange("p (a b) -> p a b", a=4), AF.Copy, [bb], [tmb], scale=0.125)
        bk, bb = PA()
        for kk in range(8):
            s.mm(bk[:, 0:256], s.XT[:, kk, i * 128:(i + 1) * 128], Wbv[:, kk, 0:256], kk == 0, kk == 7,
                 [s.XTb[i // 4], Wbb], [bb], False)
        for kk in range(8):
            s.mm(bk[:, 256:272], s.XT[:, kk, i * 128:(i + 1) * 128], Wbv[:, kk, 512:528], kk == 0, kk == 7,
                 [s.XTb[i // 4], Wbb], [bb], kk == 7)
        s.act(mvA[:, i, :, 0:64], bk[:, 0:256].rearrange("p (a b) -> p a b", a=4), AF.Copy, [bb], [tmb])
        s.tt(gts[:, i, :, :, :].rearrange("p a b c -> p (a b c)"), bk[:, 256:272], s.PRM[:, P_GB:P_GB + 16],
             ALU.add, [bb, s.PRMb], [ab])
    for j in range(4):
        for g in range(4):
            bk, bb = PA()
            for kk in range(8):
                s.mm(bk[:, :], Wav[:, kk, j * 128:(j + 1) * 128], s.XT[:, kk, g * 512:(g + 1) * 512],
                     kk == 0, kk == 7, [s.XTb[g], Wab], [bb], kk == 7)
            if j < 2:
                s.act(mqT[:, j, g * 512:(g + 1) * 512], bk[:, :], AF.Copy, [bb], [fmb])
            else:
                s.act(mkT[:, j - 2, g * 512:(g + 1) * 512], bk[:, :], AF.Copy, [bb], [fmb], scale=0.125)
    wo_pre = outproj_load(s, l, 2, 512)
    s.cp(u_all, gts[:, :, :, 0, :], [ab], [ab])
    s.act(a_all, gts[:, :, :, 1, :], AF.Exp, [ab], [ab], scale=-1.0)
    s.act(a_all, a_all, AF.Ln, [ab], [ab], bias=1.0)
    s.ts(a_all, a_all, -1.0, None, ALU.mult, None, [ab], [ab])
    if s.stop_after == "mlstm_a":
        return
    T = token_decay_arrays(s, a_all, u_all, ab)
    cs, nb, ecs, ws, gst = T["cs"], T["nb"], T["ecs"], T["ws"], T["gst"]
    if s.stop_after == "mlstm_b":
        return
    s.memset(Sst, 0.0, [Sb])
    P23 = [s.pool([2, 4]), s.pool([3, 5])]
    for c in range(NT):
        for d in range(2):
            t_ = c if d == 0 else NT - 1 - c
            s.tt(Kw[d], mkTM[:, t_, :, :], bc(ws[:, t_, d, :].rearrange("p (a b) -> p a b", b=1), [128, 4, 64]),
                 ALU.mult, [tmb, ab], [Kwb[d]])
            bk, bb = P23[d]()
            for c2 in range(2):
                s.mm(bk[:, c2 * 130:(c2 + 1) * 130], Kw[d][:, 2 * c2:2 * c2 + 2, :].rearrange("p a b -> p (a b)"),
                     mvA[:, t_, 2 * c2:2 * c2 + 2, :].rearrange("p a b -> p (a b)"), True, True,
                     [Kwb[d], tmb], [bb], c2 == 1)
            s.act(Fst[:, t_, d, :, :], Sst[:, d, :, :], AF.Copy, [Sb], [Fb])
            bv = bk[:, 0:260].rearrange("p (a b) -> p a b", a=2)
            for hp in range(2):
                r0, r1 = hp * 64, hp * 64 + 64
                gsel = gst[r0:r1, t_, d, :].rearrange("p (a b) -> p a b", b=2)[:, :, hp:hp + 1]
                s.tt(Sst[r0:r1, d, :, :], Sst[r0:r1, d, :, :], bc(gsel, [64, 2, 65]), ALU.mult, [Sb, ab], [Sb])
                s.tt(Sst[r0:r1, d, :, :], Sst[r0:r1, d, :, :], bv[r0:r1, :, hp * 65:(hp + 1) * 65], ALU.add,
                     [Sb, bb], [Sb])
    if s.stop_after == "mlstm_c":
        return
    bE = [(s.pool([0])()), (s.pool([1])())]
    bS = s.pool([2])()
    bI = [s.pool([3])(), s.pool([4])()]
    bJ = [s.pool([5])(), s.pool([6])()]
    bT = s.pool([7])()
    def stage_a(i):
        sl = slice(i * 128, (i + 1) * 128)
        E, Eb, MT, MTb, sgo, sgob = Es[i % 2], Ebs[i % 2], MTs[i % 2], MTbs[i % 2], sgos[i % 2], sgobs[i % 2]
        decay_E(s, i, a_all, nb, ab, E, Eb, bE)
        for h in range(4):
            r0 = (h % 2) * 64
            s.mm(bS[0][:, h * 128:(h + 1) * 128], mkT[r0:r0 + 64, h // 2, sl], mqT[r0:r0 + 64, h // 2, sl],
                 True, True, [fmb], [bS[1]], True, serial=True)
        for d in range(2):
            s.tt(MT[:, d, :, :], bS[0][:, :].rearrange("p (a b) -> p a b", a=4), E[:, d, :, :], ALU.mult,
                 [bS[1], Eb], [MTb])
        bk, bb = bT
        for kk in range(8):
            s.mm(bk[:, 0:256], s.XT[:, kk, sl], Wbv[:, kk, 256:512], kk == 0, kk == 7,
                 [s.XTb[i // 4], Wbb], [bb], kk == 7)
        s.act(sgo, bk[:, 0:256], AF.Sigmoid, [bb], [sgob])

    def stage_b(i):
        sl = slice(i * 128, (i + 1) * 128)
        yb, ybb = ybs[i % 3], ybbs[i % 3]
        E, Eb, MT, MTb, sgo, sgob = Es[i % 2], Ebs[i % 2], MTs[i % 2], MTbs[i % 2], sgos[i % 2], sgobs[i % 2]
        for d in range(2):
            for h in range(4):
                r0 = (h % 2) * 64
                s.mm(bJ[d][0][:, h * 65:(h + 1) * 65], mqT[r0:r0 + 64, h // 2, sl], Fst[r0:r0 + 64, i, d, h // 2, :],
                     True, True, [fmb, Fb], [bJ[d][1]], True, serial=True)
            for h in range(4):
                s.mm(bI[d][0][:, h * 65:(h + 1) * 65], MT[:, d, h, :], mvA[:, i, h, :], True, True,
                     [MTb, tmb], [bI[d][1]], h == 3)
            s.tt(tmp, bJ[d][0][:, 0:260].rearrange("p (a b) -> p a b", a=4),
                 bc(ecs[:, i, d, :].rearrange("p (a b) -> p a b", b=1), [128, 4, 65]), ALU.mult,
                 [bJ[d][1], ab], [Rb])
            s.tt(Rt[d], bI[d][0][:, 0:260].rearrange("p (a b) -> p a b", a=4), tmp, ALU.add, [bI[d][1], Rb], [Rb])
        d8 = den.rearrange("p (a b) -> p a b", a=2)
        s.ts(d8, Rall[:, :, :, 64], -1.0, None, ALU.mult, None, [Rb], [Rb])
        s.tt(d8, d8, Rall[:, :, :, 64], ALU.max, [Rb], [Rb])
        s.ts(d8, d8, 1.0, None, ALU.max, None, [Rb], [Rb])
        s.recip(den, den, [Rb], [Rb])
        s.tt(prod, Rall[:, :, :, 0:64], bc(den.rearrange("p (a b c) -> p a b c", a=2, c=1), [128, 2, 4, 64]),
             ALU.mult, [Rb], [hb_])
        s.tt(hs, prod[:, 0, :, :], prod[:, 1, :, :], ALU.add, [hb_], [hb_])
        s.red(st4, hs, ALU.add, [hb_], [hb_])
        s.ts(st4, st4, 1.0 / 64.0, None, ALU.mult, None, [hb_], [hb_])
        s.tt(cen, hs, bc(st4.rearrange("p (a b) -> p a b", b=1), [128, 4, 64]), ALU.subtract, [hb_], [hb_])
        s.tt(sq_, cen, cen, ALU.mult, [hb_], [hb_])
        s.red(st4b, sq_, ALU.add, [hb_], [hb_])
        s.act(st4b, st4b, AF.Ln, [hb_], [hb_], scale=1.0 / 64.0, bias=1e-5)
        s.act(st4b, st4b, AF.Exp, [hb_], [hb_], scale=-0.5)
        s.tt(cen, cen, bc(st4b.rearrange("p (a b) -> p a b", b=1), [128, 4, 64]), ALU.mult, [hb_], [hb_])
        cf = cen.rearrange("p a b -> p (a b)")
        s.tt(cf, cf, s.PRM[:, P_MN:P_MN + 256], ALU.mult, [hb_, s.PRMb], [hb_])
        s.tt(yb, cf, sgo, ALU.mult, [hb_, sgob], [ybb])

    def stage_c(i):
        sl = slice(i * 128, (i + 1) * 128)
        yb, ybb = ybs[i % 3], ybbs[i % 3]
        bk, bb = bT
        pb = bk[:, 0:256].bitcast(BF16)
        for c in range(2):
            s.tr(pb[:, c * 128:(c + 1) * 128], yb[:, c * 128:(c + 1) * 128], s.identB, [ybb, s.CBb], [bb], c == 1)
        s.act(YB[:, :, sl], pb[:, 0:256].rearrange("p (a b) -> p a b", a=2), AF.Copy, [bb], [YBb])

    stage_a(0)
    for i in range(NT + 1):
        if i + 1 < NT:
            stage_a(i + 1)
        if i < NT:
            stage_b(i)
        if i >= 1:
            stage_c(i - 1)
    s.tap("yb", YB, [YBb])
    preload_ssd_x(s, l)
    outproj_partial(s, l, YB, YBb, 2, 512, False, pre=wo_pre)


def phase_ssd(st, l):
    s = NS(st) if isinstance(st, dict) else st
    k, ar = s.k, s.arena
    k.barrier()
    ar.reset()
    s.sc_reset()
    PA = s.pool([0, 1, 2, 3])
    PT = s.pool([4, 5, 6, 7])
    wsrc = s.w_in[l].rearrange("(k p) c -> p k c", p=128)
    if "ssd_x" not in s.pre:
        preload_ssd_x(s, l)
    Wxv, Wxb = s.pre.pop("ssd_x")
    Wz, Wzb, Wzs = s.wslot()
    Wzv = Wz[:, 0:8 * 264].rearrange("p (k c) -> p k c", k=8)
    k.dma("pool", Wzv[:, :, 0:256], wsrc[:, :, 1456:1712], Wzs, writes=[Wzb])
    k.dma("pool", Wzv[:, :, 256:264], wsrc[:, :, 2480:2488], Wzs, writes=[Wzb])
    BCT = ar.alloc([4, S], BF16, "BCT"); bcb = Buf("BCT")
    BTM = ar.alloc([NT, 2, 128], BF16, "BTM"); btb = Buf("BTM")
    xTM = ar.alloc([NT, 4, 64], BF16, "xTM"); tmb = Buf("stm")
    YC = BTM.rearrange("p a b c -> p (a b c)").rearrange("p (a b) -> p a b", a=2); YCb = btb
    dtr = ar.alloc([NT, 2, 4], F32, "dtr")
    a_all = ar.alloc([NT, 2, 4], F32, "a_all"); u_all = ar.alloc([NT, 2, 4], F32, "u_all"); ab = Buf("stok")
    aneg = s.sc_alloc(8)
    T = None
    mark = ar.off
    bk, bb = PA()
    for i in range(NT):
        for kk in range(8):
            s.mm(bk[:, i * 8:(i + 1) * 8], s.XT[:, kk, i * 128:(i + 1) * 128], Wzv[:, kk, 256:264], kk == 0, kk == 7,
                 [s.XTb[i // 4], Wzb], [bb], i == NT - 1 and kk == 7)
    dflat = dtr.rearrange("p a b c -> p a (b c)")
    s.tt(dflat, bk[:, 0:128].rearrange("p (a b) -> p a b", a=NT),
         bc(s.PRM[:, P_DTB:P_DTB + 8].rearrange("p (a b) -> p a b", a=1), [128, NT, 8]), ALU.add,
         [bb, s.PRMb], [ab])
    s.act(dtr, dtr, AF.Exp, [ab], [ab])
    s.act(dtr, dtr, AF.Ln, [ab], [ab], bias=1.0)
    s.act(u_all, dtr, AF.Ln, [ab], [ab])
    s.act(aneg, s.PRM[:, P_AL:P_AL + 8], AF.Exp, [s.PRMb], [ab])
    s.ts(aneg, aneg, -1.0, None, ALU.mult, None, [ab], [ab])
    s.tt(a_all.rearrange("p a b c -> p a (b c)"), dflat,
         bc(aneg.rearrange("p (a b) -> p a b", a=1), [128, NT, 8]), ALU.mult, [ab], [ab])
    cin = [ar.alloc([2052], BF16, "cin%d" % i) for i in range(2)]; cinb = [Buf("cin0"), Buf("cin1")]
    acc = [ar.alloc([512], F32, "acc%d" % i) for i in range(2)]; accb = [Buf("acc0"), Buf("acc1")]
    xfm = [ar.alloc([S], BF16, "xfm%d" % i) for i in range(2)]; xfmb = [Buf("xfm0"), Buf("xfm1")]
    for p_ in range(2):
        s.memset(cin[p_][:, 0:2], 0.0, [cinb[p_]])
        s.memset(cin[p_][:, 2050:2052], 0.0, [cinb[p_]])
    accr = Rot([0, 1])
    for j in range(6):
        p_ = j % 2
        for g in range(4):
            bk, bb = PA()
            for kk in range(8):
                s.mm(bk[:, :], Wxv[:, kk, j * 128:(j + 1) * 128], s.XT[:, kk, g * 512:(g + 1) * 512],
                     kk == 0, kk == 7, [s.XTb[g], Wxb], [bb], kk == 7)
            s.act(cin[p_][:, 2 + g * 512:2 + (g + 1) * 512], bk[:, :], AF.Copy, [bb], [cinb[p_]])
        if j < 2:
            dst, dstb = xfm[p_], xfmb[p_]
        else:
            dst, dstb = BCT[:, j - 2, :], bcb
        for g in range(4):
            a_ = accr()
            cw = lambda jj: s.PRM[:, P_CW + j * 5 + jj:P_CW + j * 5 + jj + 1]
            s.ts(acc[a_], cin[p_][:, g * 512:g * 512 + 512], cw(0), None, ALU.mult, None,
                 [cinb[p_], s.PRMb], [accb[a_]])
            for jj in range(1, 5):
                s.stt(acc[a_], cin[p_][:, g * 512 + jj:g * 512 + jj + 512], cw(jj), acc[a_], ALU.mult, ALU.add,
                      [cinb[p_], s.PRMb, accb[a_]], [accb[a_]])
            s.act(dst[:, g * 512:(g + 1) * 512], acc[a_], AF.Silu, [accb[a_], s.PRMb], [dstb],
                  bias=s.PRM[:, P_CB + j:P_CB + j + 1])
        if j < 4:
            src = xfm[p_] if j < 2 else BCT[:, j - 2, :]
            srcb = xfmb[p_] if j < 2 else bcb
            for i4 in range(4):
                bk, bb = PT()
                pb = bk[:, 0:256].bitcast(BF16)
                for q in range(4):
                    i = i4 * 4 + q
                    s.tr(pb[:, q * 128:(q + 1) * 128], src[:, i * 128:(i + 1) * 128], s.identB, [srcb, s.CBb], [bb], q == 3)
                if j < 2:
                    s.act(xTM[:, i4 * 4:(i4 + 1) * 4, 2 * j:2 * j + 2, :],
                          pb[:, :].rearrange("p (a b c) -> p a b c", a=4, b=2), AF.Copy, [bb], [tmb])
                else:
                    s.act(BTM[:, i4 * 4:(i4 + 1) * 4, j - 2, :], pb[:, :].rearrange("p (a b) -> p a b", a=4),
                          AF.Copy, [bb], [btb])
    s.tap("bct", BCT, [bcb])
    wo_pre = outproj_load(s, l, 2, 768)
    k.barrier()
    ar.off = mark
    T = token_decay_arrays(s, a_all, u_all, ab)
    cs, nb, ecs, ws, gst = T["cs"], T["nb"], T["ecs"], T["ws"], T["gst"]
    Fst = ar.alloc([NT, 2, 4, 64], BF16, "F"); Fb = Buf("F")
    Sst = ar.alloc([2, 4, 64], F32, "S"); Sb = Buf("S")
    Kw = [ar.alloc([4, 128], BF16, "Kw%d" % i) for i in range(2)]; Kwb = [Buf("Kw0"), Buf("Kw1")]
    Es = [ar.alloc([2, 4, 128], BF16, "E%d" % i) for i in range(2)]; Ebs = [Buf("E0"), Buf("E1")]
    MTs = [ar.alloc([2, 4, 128], BF16, "MT%d" % i) for i in range(2)]; MTbs = [Buf("MT0"), Buf("MT1")]
    Rt = [ar.alloc([4, 64], F32, "R%d" % i) for i in range(2)]; Rb = Buf("R")
    tmp = ar.alloc([4, 64], F32, "tmp")
    yt = ar.alloc([4, 64], F32, "yt"); ytb = Buf("yt")
    sq_ = tmp
    zss = [ar.alloc([256], F32, "zs%d" % i) for i in range(2)]; zsbs = [Buf("zs0"), Buf("zs1")]
    ybs = [ar.alloc([256], BF16, "yb%d" % i) for i in range(3)]; ybbs = [Buf("yb%d" % i) for i in range(3)]
    ss2 = s.sc_alloc(2)
    s.memset(Sst, 0.0, [Sb])
    P23 = [s.pool([2, 4]), s.pool([3, 5])]
    for c in range(NT):
        for d in range(2):
            t_ = c if d == 0 else NT - 1 - c
            s.tt(Kw[d].rearrange("p (g e) n -> p g e n", g=2),
                 bc(BTM[:, t_, :, :].rearrange("p g (o n) -> p g o n", o=1), [128, 2, 2, 128]),
                 bc(ws[:, t_, d, :].rearrange("p (g e o) -> p g e o", g=2, o=1), [128, 2, 2, 128]),
                 ALU.mult, [btb, ab], [Kwb[d]])
            bk, bb = P23[d]()
            for h in range(4):
                s.mm(bk[:, h * 64:(h + 1) * 64], Kw[d][:, h, :], xTM[:, t_, h, :], True, True,
                     [Kwb[d], tmb], [bb], h == 3)
            s.act(Fst[:, t_, d, :, :], Sst[:, d, :, :], AF.Copy, [Sb], [Fb])
            s.tt(Sst[:, d, :, :], Sst[:, d, :, :],
                 bc(gst[:, t_, d, :].rearrange("p (a b) -> p a b", b=1), [128, 4, 64]), ALU.mult, [Sb, ab], [Sb])
            s.tt(Sst[:, d, :, :], Sst[:, d, :, :], bk[:, 0:256].rearrange("p (a b) -> p a b", a=4), ALU.add,
                 [Sb, bb], [Sb])
    bE = [(s.pool([0])()), (s.pool([1])())]
    bS = s.pool([2])()
    bI = [s.pool([3])(), s.pool([4])()]
    bJ = [s.pool([5])(), s.pool([6])()]
    bT = s.pool([7])()
    def stage_a(i):
        sl = slice(i * 128, (i + 1) * 128)
        E, Eb, MT, MTb, zs, zsb = Es[i % 2], Ebs[i % 2], MTs[i % 2], MTbs[i % 2], zss[i % 2], zsbs[i % 2]
        decay_E(s, i, a_all, nb, ab, E, Eb, bE)
        for g in range(2):
            s.mm(bS[0][:, g * 128:(g + 1) * 128], BCT[:, g, sl], BCT[:, 2 + g, sl], True, True, [bcb], [bS[1]], g == 1)
        for d in range(2):
            s.tt(MT[:, d, :, :].rearrange("p (g e) n -> p g e n", g=2),
                 bc(bS[0][:, 0:256].rearrange("p (g o n) -> p g o n", g=2, o=1), [128, 2, 2, 128]),
                 E[:, d, :, :].rearrange("p (g e) n -> p g e n", g=2), ALU.mult, [bS[1], Eb], [MTb])
        bk, bb = bT
        for kk in range(8):
            s.mm(bk[:, 0:256], s.XT[:, kk, sl], Wzv[:, kk, 0:256], kk == 0, kk == 7,
                 [s.XTb[i // 4], Wzb], [bb], kk == 7)
        s.act(zs, bk[:, 0:256], AF.Silu, [bb], [zsb])

    def stage_b(i):
        sl = slice(i * 128, (i + 1) * 128)
        yb, ybb = ybs[i % 3], ybbs[i % 3]
        E, Eb, MT, MTb, zs, zsb = Es[i % 2], Ebs[i % 2], MTs[i % 2], MTbs[i % 2], zss[i % 2], zsbs[i % 2]
        for d in range(2):
            for h in range(4):
                s.mm(bJ[d][0][:, h * 64:(h + 1) * 64], BCT[:, 2 + h // 2, sl], Fst[:, i, d, h, :], True, True,
                     [bcb, Fb], [bJ[d][1]], h == 3)
            for h in range(4):
                s.mm(bI[d][0][:, h * 64:(h + 1) * 64], MT[:, d, h, :], xTM[:, i, h, :], True, True,
                     [MTb, tmb], [bI[d][1]], h == 3)
            s.tt(tmp, bJ[d][0][:, 0:256].rearrange("p (a b) -> p a b", a=4),
                 bc(ecs[:, i, d, :].rearrange("p (a b) -> p a b", b=1), [128, 4, 64]), ALU.mult,
                 [bJ[d][1], ab], [Rb])
            s.tt(Rt[d], bI[d][0][:, 0:256].rearrange("p (a b) -> p a b", a=4), tmp, ALU.add, [bI[d][1], Rb], [Rb])
        s.tt(yt, Rt[0], Rt[1], ALU.add, [Rb], [ytb])
        s.tt(sq_, xTM[:, i, :, :], bc(s.PRM[:, P_SD:P_SD + 4].rearrange("p (a b) -> p a b", b=1), [128, 4, 64]),
             ALU.mult, [tmb, s.PRMb], [ytb, Rb])
        s.tt(yt, yt, sq_, ALU.add, [ytb, Rb], [ytb])
        yf = yt.rearrange("p a b -> p (a b)")
        s.tt(yf, yf, zs, ALU.mult, [ytb, zsb], [ytb])
        s.tt(sq_, yt, yt, ALU.mult, [ytb], [ytb, Rb])
        s.red(ss2, sq_.rearrange("p (g e) n -> p g (e n)", g=2), ALU.add, [ytb, Rb], [ytb])
        s.act(ss2, ss2, AF.Ln, [ytb], [ytb], scale=1.0 / 128.0, bias=1e-6)
        s.act(ss2, ss2, AF.Exp, [ytb], [ytb], scale=-0.5)
        s.tt(yt.rearrange("p (g e) n -> p g (e n)", g=2), yt.rearrange("p (g e) n -> p g (e n)", g=2),
             bc(ss2.rearrange("p (a b) -> p a b", b=1), [128, 2, 128]), ALU.mult, [ytb], [ytb])
        s.tt(yb, yf, s.PRM[:, P_SN:P_SN + 256], ALU.mult, [ytb, s.PRMb], [ybb])

    def stage_c(i):
        sl = slice(i * 128, (i + 1) * 128)
        yb, ybb = ybs[i % 3], ybbs[i % 3]
        bk, bb = bT
        pb = bk[:, 0:256].bitcast(BF16)
        for c in range(2):
            s.tr(pb[:, c * 128:(c + 1) * 128], yb[:, c * 128:(c + 1) * 128], s.identB, [ybb, s.CBb], [bb], c == 1)
        s.act(YC[:, :, sl], pb[:, 0:256].rearrange("p (a b) -> p a b", a=2), AF.Copy, [bb], [YCb])

    stage_a(0)
    for i in range(NT + 1):
        if i + 1 < NT:
            stage_a(i + 1)
        if i < NT:
            stage_b(i)
        if i >= 1:
            stage_c(i - 1)
    s.tap("yc", YC, [YCb])
    outproj_partial(s, l, YC, YCb, 2, 768, False, pre=wo_pre)


def layernorm_tiles(s, l, which, T1s, T1bs, LNP, LNPb, post):
    st6 = s.arena.alloc([2, 6], F32, "st6")
    mv = s.sc_alloc(2)
    sd = s.sc_alloc(1)
    lb = Buf("lnstat")
    def front(i):
        T1, T1b = T1s[i % 2], T1bs[i % 2]
        for hf in range(2):
            s.k.op("dve", lambda h: h.bn_stats(out=st6[:, hf, :], in_=s.X[:, i, hf * 512:(hf + 1) * 512]),
                   reads=[s.Xb[i]], writes=[lb])
        s.k.op("dve", lambda h: h.bn_aggr(out=mv, in_=st6.rearrange("p a b -> p (a b)")), reads=[lb], writes=[lb])
        s.act(sd, mv[:, 1:2], AF.Sqrt, [lb], [lb], scale=1.0, bias=1e-5)
        s.recip(sd, sd, [lb], [lb])
        s.ts(T1, s.X[:, i, :], mv[:, 0:1], sd, ALU.subtract, ALU.mult, [s.Xb[i], lb], [T1b])
        s.tt(T1, T1, LNP[:, 0, :], ALU.mult, [T1b, LNPb], [T1b])
        s.tt(T1, T1, LNP[:, 1, :], ALU.add, [T1b, LNPb], [T1b])

    front(0)
    for i in range(NT):
        if i + 1 < NT:
            front(i + 1)
        post(i, T1s[i % 2], T1bs[i % 2])


def phase_ln_router_moe(st, l, last):
    s = NS(st) if isinstance(st, dict) else st
    k, ar = s.k, s.arena
    k.barrier()
    ar.reset()
    s.sc_reset()
    s_l = s.s_l
    comb = ar.alloc([NT, 4, 4], F32, "comb"); combb = Buf("comb")
    mark = ar.off

    def load_gu(e):
        W, Wb, Ws = s.wslot()
        Wv = W[:, 0:4096].rearrange("p (k c) -> p k c", k=8)
        k.dma("pool", Wv[:, :, 0:256], s.d_wg[l, e].rearrange("(k p) f -> p k f", p=128), Ws, writes=[Wb])
        k.dma("pool", Wv[:, :, 256:512], s.d_wu[l, e].rearrange("(k p) f -> p k f", p=128), Ws, writes=[Wb])
        return Wv, Wb

    e0_pre = load_gu(0)
    LNP = ar.alloc([2, D], F32, "LNP"); LNPb = Buf("LNP")
    for q in range(2):
        k.dma("sp", LNP[:, q, :], s.dlnp[l, q:q + 1, :].to_broadcast([128, D]), s_l, writes=[LNPb])
    T1s = [ar.alloc([D], F32, "T1%d" % i) for i in range(2)]; T1bs = [Buf("T10"), Buf("T11")]
    x32s = [ar.alloc([8, 128], F32, "x32%d" % i) for i in range(2)]; x32bs = [Buf("x320"), Buf("x321")]
    RL = ar.alloc([NT, 20], F32, "RL"); RLb = Buf("RL")
    PR = s.pool([4, 5])

    def post1(i, T1, T1b):
        x32, x32b = x32s[i % 2], x32bs[i % 2]
        s.transpose_x_tile(i, T1, [T1b], dst32=x32, dst32b=x32b)
        s.act(s.X[:, i, :], T1, AF.Copy, [T1b], [s.Xb[i]], scale=ALPHA)
        bk, bb = PR()
        for c in range(8):
            s.mm(bk[:, 0:20], x32[:, c, :], s.PRM[:, P_RW + c * 20:P_RW + (c + 1) * 20], c == 0, c == 7,
                 [x32b, s.PRMb], [bb], c == 7)
        s.tt(RL[:, i, :], bk[:, 0:20], s.PRM[:, P_RB:P_RB + 20], ALU.add, [bb, s.PRMb], [RLb])

    layernorm_tiles(s, l, 0, T1s, T1bs, LNP, LNPb, post1)
    s.tap("x1", s.X[:, :, :], s.Xb)
    gl = RL[:, :, 0:4]
    el = RL[:, :, 4:20].rearrange("p t (g e) -> p t g e", g=4)
    A1 = lambda n: ar.alloc([NT, n], F32, "r%d" % n)
    gmax, gsum, gp, emax1, emax2, ssum, rg = A1(1), A1(1), A1(1), A1(1), A1(1), A1(1), A1(1)
    gsh, ohg, esel, esel2, m1, m2, ee = A1(4), A1(4), A1(4), A1(4), A1(4), A1(4), A1(4)
    t16 = ar.alloc([NT, 4, 4], F32, "t16")
    rb_ = Buf("route")
    b4 = lambda a: bc(a, [128, NT, 4])
    R_, W_ = [RLb, rb_], [rb_]
    s.red(gmax, gl, ALU.max, R_, W_)
    s.tt(gsh, gl, b4(gmax), ALU.subtract, R_, W_)
    s.act(gsh, gsh, AF.Exp, R_, W_)
    s.red(gsum, gsh, ALU.add, R_, W_)
    s.recip(gp, gsum, R_, W_)
    s.tt(ohg, gl, b4(gmax), ALU.is_equal, R_, W_)
    s.tt(t16, el, bc(ohg.rearrange("p t (g o) -> p t g o", o=1), [128, NT, 4, 4]), ALU.mult, R_, W_)
    s.red(esel, t16.rearrange("p t g e -> p t e g"), ALU.add, R_, W_)
    s.red(emax1, esel, ALU.max, R_, W_)
    s.tt(m1, esel, b4(emax1), ALU.is_equal, R_, W_)
    s.stt(esel2, m1, -1e30, esel, ALU.mult, ALU.add, R_, W_)
    s.red(emax2, esel2, ALU.max, R_, W_)
    s.tt(m2, esel2, b4(emax2), ALU.is_equal, R_, W_)
    s.tt(m1, m1, m2, ALU.add, R_, W_)
    s.tt(ee, esel, b4(emax1), ALU.subtract, R_, W_)
    s.act(ee, ee, AF.Exp, R_, W_)
    s.tt(ee, ee, m1, ALU.mult, R_, W_)
    s.red(ssum, ee, ALU.add, R_, W_)
    s.recip(rg, ssum, R_, W_)
    s.tt(rg, rg, gp, ALU.mult, R_, W_)
    s.tt(ee, ee, b4(rg), ALU.mult, R_, W_)
    s.tt(comb, bc(ohg.rearrange("p t (g o) -> p t g o", o=1), [128, NT, 4, 4]),
         bc(ee.rearrange("p t (o e) -> p t o e", o=1), [128, NT, 4, 4]), ALU.mult, R_, [combb])
    s.tap("comb", comb, [combb])
    if s.stop_after == "ln1":
        return
    k.barrier()
    ar.off = mark
    hT = [ar.alloc([2, S], BF16, "hT%d" % i) for i in range(4)]; hTb = [Buf("hT%d" % i) for i in range(4)]
    WD = [[ar.alloc([2, D], BF16, "WD%d_%d" % (g_, e_)) for e_ in range(4)] for g_ in range(2)]
    WDb = [[Buf("WD") for _ in range(4)] for _ in range(2)]
    WDs = s.wd_sems
    NB_ = 4
    sgs = [ar.alloc([256], F32, "sg%d" % i) for i in range(NB_)]; sgb = [Buf("sg%d" % i) for i in range(NB_)]
    hms = [ar.alloc([256], BF16, "hm%d" % i) for i in range(NB_)]; hmb = [Buf("hm%d" % i) for i in range(NB_)]
    trq = []
    PG = s.pool([0, 1, 2])
    PTr = s.pool([3, 4])
    PD = s.pool([5, 6, 7])
    slots = {}

    def load_expert(e):
        if e == 0:
            Wv, Wb = e0_pre
        else:
            Wv, Wb = load_gu(e)
        g_, e_ = (e // 4) % 2, e % 4
        k.dma("pool", WD[g_][e_], s.d_wd[l, e].rearrange("(c p) n -> p c n", p=128), WDs[g_ * 4 + e_],
              writes=[WDb[g_][e_]])
        slots[e] = (Wv, Wb)

    load_expert(0)
    cnt = 0
    for e in range(16):
        if e + 1 < 16:
            load_expert(e + 1)
        Wv, Wb = slots.pop(e)
        e4 = e % 4
        def moe_tr(item):
            (i_, hm_, hmb2, e4_) = item
            sl_ = slice(i_ * 128, (i_ + 1) * 128)
            tk_, tb_ = PTr()
            pb = tk_[:, 0:128].bitcast(BF16)
            for c in range(2):
                s.tr(pb[:, c * 128:(c + 1) * 128], hm_[:, c * 128:(c + 1) * 128], s.identB, [hmb2, s.CBb], [tb_], c == 1)
            s.act(hT[e4_][:, :, sl_], pb[:, 0:256].rearrange("p (a b) -> p a b", a=2), AF.Copy, [tb_], [hTb[e4_]])

        for i in range(NT):
            sl = slice(i * 128, (i + 1) * 128)
            bk, bb = PG()
            for kk in range(8):
                s.mm(bk[:, :], s.XT[:, kk, sl], Wv[:, kk, :], kk == 0, kk == 7, [s.XTb[i // 4], Wb], [bb], kk == 7)
            sg, sgb_ = sgs[cnt % NB_], sgb[cnt % NB_]
            hm, hmb_ = hms[cnt % NB_], hmb[cnt % NB_]
            cnt += 1
            s.act(sg, bk[:, 0:256], AF.Silu, [bb], [sgb_])
            s.stt(hm, bk[:, 256:512], comb[:, i, e // 4, e4:e4 + 1], sg, ALU.mult, ALU.mult,
                  [bb, combb, sgb_], [hmb_])
            trq.append((i, hm, hmb_, e4))
            if len(trq) > 2:
                moe_tr(trq.pop(0))
        if e4 == 3:
            while trq:
                moe_tr(trq.pop(0))
        if e4 == 3:
            g_ = (e // 4) % 2
            for i in range(NT):
                sl = slice(i * 128, (i + 1) * 128)
                for hf in range(2):
                    bk, bb = PD()
                    n = 0
                    for ee_ in range(4):
                        for c in range(2):
                            s.mm(bk[:, :], hT[ee_][:, c, sl], WD[g_][ee_][:, c, hf * 512:(hf + 1) * 512],
                                 n == 0, n == 7, [hTb[ee_], WDb[g_][ee_]], [bb], n == 7)
                            n += 1
                    xs = s.X[:, i, hf * 512:(hf + 1) * 512]
                    s.tt(xs, xs, bk[:, :], ALU.add, [s.Xb[i], bb], [s.Xb[i]])
    if s.stop_after == "moe":
        return
    if not last:
        preload_mla(s, l + 1)
    k.barrier()
    ar.off = mark
    LNP = ar.alloc([2, D], F32, "LNP2"); LNPb = Buf("LNP2")
    for q in range(2):
        k.dma("sp", LNP[:, q, :], s.dlnp[l, 2 + q:3 + q, :].to_broadcast([128, D]), s_l, writes=[LNPb])
    T1s = [ar.alloc([D], F32, "T2%d" % i) for i in range(2)]; T1bs = [Buf("T20"), Buf("T21")]

    def post2(i, T1, T1b):
        if not last:
            s.transpose_x_tile(i, T1, [T1b])
        s.act(s.X[:, i, :], T1, AF.Copy, [T1b], [s.Xb[i]])

    layernorm_tiles(s, l, 1, T1s, T1bs, LNP, LNPb, post2)
    k.barrier()


def _consts():
    c = np.zeros((128, CW), np.float32)
    r = np.arange(128)
    c[:, C_ID:C_ID + 128] = np.eye(128, dtype=np.float32)
    c[:, C_U:C_U + 128] = (r[:, None] <= r[None, :]).astype(np.float32)
    c[:, C_L:C_L + 128] = (r[:, None] >= r[None, :]).astype(np.float32)
    c[:, C_MF:C_MF + 128] = np.where(r[:, None] <= r[None, :], 0.0, NEG).astype(np.float32)
    c[:, C_MB:C_MB + 128] = np.where(r[:, None] >= r[None, :], 0.0, NEG).astype(np.float32)
    c[:, C_ONE:C_ONE + 128] = 1.0
    inv = (10000.0 ** (-np.arange(0, 32, 2, dtype=np.float32) / np.float32(32))).astype(np.float32)
    c[:, C_IF:C_IF + 16] = inv[None, :]
    return c


def _pack_prm(inp, l):
    p = np.zeros((128, PW), np.float32)
    rep = lambda v: np.broadcast_to(np.asarray(v, np.float32).reshape(1, -1), (128, np.asarray(v).size))
    p[:, P_GQ:P_GQ + 2] = inp["mla_q_norm"][l].reshape(2, 128).T
    p[:, P_GKV] = inp["mla_kv_norm"][l]
    p[:, P_GB:P_GB + 16] = rep(inp["mlstm_gate_bias"][l])
    p[:, P_MN:P_MN + 256] = rep(inp["mlstm_norm"][l])
    cw = inp["ssd_conv_w"][l]
    p[:, P_CW:P_CW + 30] = cw.reshape(5, 6, 128).transpose(2, 1, 0).reshape(128, 30)
    p[:, P_CB:P_CB + 6] = inp["ssd_conv_b"][l].reshape(6, 128).T
    p[:, P_DTB:P_DTB + 8] = rep(inp["ssd_dt_bias"][l])
    p[:, P_AL:P_AL + 8] = rep(inp["ssd_a_log"][l])
    p[:, P_SD:P_SD + 4] = rep(inp["ssd_d"][l])
    p[:, P_SN:P_SN + 256] = rep(inp["ssd_norm"][l])
    rb = np.concatenate([inp["router_group_b"][l].reshape(-1), inp["router_expert_b"][l].reshape(-1)])
    p[:, P_RB:P_RB + 20] = rep(rb)
    wr = np.concatenate([inp["router_group_w"][l],
                         inp["router_expert_w"][l].transpose(1, 0, 2).reshape(1024, 16)], axis=1)
    p[:, P_RW:P_RW + 160] = wr.reshape(8, 128, 20).transpose(1, 0, 2).reshape(128, 160)
    return p


def make_in_maps(inp, layers):
    inp = {k_: np.asarray(v) for k_, v in inp.items()}
    L = list(layers)
    sl = lambda a: np.ascontiguousarray(a[L])
    shared = {
        "cst": _consts(),
        "prm": np.stack([_pack_prm(inp, l) for l in L]),
        "w_in": sl(inp["w_in"]), "w_uq": sl(inp["mla_w_uq"]), "w_ukv": sl(inp["mla_w_ukv"]),
        "w_out": sl(inp["w_out"]),
        "lnp": np.stack([np.stack([inp["ln1_g"][l], inp["ln1_b"][l], inp["ln2_g"][l], inp["ln2_b"][l]]) for l in L]),
        "wg": sl(inp["expert_w_gate"]), "wu": sl(inp["expert_w_up"]), "wd": sl(inp["expert_w_down"]),
    }
    return shared


_CACHE = {}


def _prog(NL):
    if NL not in _CACHE:
        _CACHE[NL] = build(NL)[0]
    return _CACHE[NL]


FUSED = True


def kernel(**inputs):
    x = np.asarray(inputs["x"], np.float32)
    pos = np.asarray(inputs["positions"]).astype(np.int32)
    B = x.shape[0]
    posl = [np.ascontiguousarray(pos[b].reshape(NT, 128).T) for b in range(B)]
    groups = [list(range(DEPTH))] if FUSED else [[l] for l in range(DEPTH)]
    cur = [np.ascontiguousarray(x[b]) for b in range(B)]
    for L in groups:
        nc = _prog(len(L))
        shared = make_in_maps(inputs, L)
        in_maps = []
        for b in range(B):
            m = dict(shared)
            m["x"] = cur[b]
            m["pos"] = posl[b]
            in_maps.append(m)
        res = run_bass_kernel_spmd(nc, in_maps, core_ids=list(range(B)))
        cur = [np.ascontiguousarray(np.asarray(r["y"], dtype=np.float32)) for r in res.results]
    return np.stack(cur).astype(np.float32)
```
